# Optimizing a Trainium2 kernel written in Bass

```python
import jax, jax.numpy as jnp
from jax import lax
import numpy as np

D_MODEL = 1024
BATCH = 16
SEQ = 2048
DEPTH = 1

N_HEADS = 8
N_KV_HEADS = 2
HEAD_DIM = 64
WINDOW = 128
ATTN_BLOCK = 128
GMLP_WIDTH = 512
GMLP_GROUPS = 4
GMLP_CHUNK = 128
N_EXPERTS = 32
TOP_K = 4
D_FF_EXPERT = D_MODEL
SWIGLU_LIMIT = 7.0
SWIGLU_ALPHA = 1.702
MOE_BLOCK = 128
PLE_DIM = 256

RMS_EPS = 1e-6
LN_EPS = 1e-5

Q_WIDTH = N_HEADS * HEAD_DIM
KV_WIDTH = N_KV_HEADS * HEAD_DIM
IN_WIDTH = Q_WIDTH + 2 * KV_WIDTH + 2 * GMLP_WIDTH + 2 * D_MODEL
IN_SPLITS = list(np.cumsum([Q_WIDTH, KV_WIDTH, KV_WIDTH, GMLP_WIDTH, GMLP_WIDTH, D_MODEL]).tolist())

kernel_name = "hybrid_swa_gmlp_moe_block"


def rmsnorm(x, g):
    xf = x.astype(jnp.float32)
    y = xf * lax.rsqrt(jnp.mean(xf * xf, axis=-1, keepdims=True) + RMS_EPS) * g.astype(jnp.float32)
    return y.astype(x.dtype)


def layernorm(x, g, b):
    xf = x.astype(jnp.float32)
    mu = jnp.mean(xf, axis=-1, keepdims=True)
    xc = xf - mu
    var = jnp.mean(xc * xc, axis=-1, keepdims=True)
    y = xc * lax.rsqrt(var + LN_EPS) * g.astype(jnp.float32) + b.astype(jnp.float32)
    return y.astype(x.dtype)


def alibi_slopes(n_heads):
    return jnp.asarray(2.0 ** (-8.0 * (np.arange(n_heads, dtype=np.float32) + 1.0) / n_heads), dtype=jnp.float32)


def sliding_window_attention(q, k, v, sinks):
    B, S = q.shape[0], q.shape[1]
    nb = S // ATTN_BLOCK
    G = N_HEADS // N_KV_HEADS
    qb = q.reshape(B, nb, ATTN_BLOCK, N_KV_HEADS, G, HEAD_DIM)
    kb = k.reshape(B, nb, ATTN_BLOCK, N_KV_HEADS, HEAD_DIM)
    vb = v.reshape(B, nb, ATTN_BLOCK, N_KV_HEADS, HEAD_DIM)
    pad = ((0, 0), (1, 0), (0, 0), (0, 0), (0, 0))
    kk = jnp.concatenate([jnp.pad(kb, pad)[:, :-1], kb], axis=2)
    vv = jnp.concatenate([jnp.pad(vb, pad)[:, :-1], vb], axis=2)
    scores = jnp.einsum('bnqkgd,bnskd->bnkgqs', qb, kk).astype(jnp.float32) * (HEAD_DIM ** -0.5)
    qi = jnp.arange(ATTN_BLOCK)[:, None]
    sj = jnp.arange(2 * ATTN_BLOCK)[None, :]
    dist = (qi + ATTN_BLOCK - sj).astype(jnp.float32)
    in_window = (dist >= 0) & (dist < WINDOW)
    key_pos = (jnp.arange(nb)[:, None] - 1) * ATTN_BLOCK + jnp.arange(2 * ATTN_BLOCK)[None, :]
    mask = in_window[None, :, :] & (key_pos >= 0)[:, None, :]
    slopes = alibi_slopes(N_HEADS).reshape(N_KV_HEADS, G)
    scores = scores - slopes[:, :, None, None] * dist[None, None, :, :]
    scores = jnp.where(mask[None, :, None, None, :, :], scores, -jnp.inf)
    sink_col = jnp.broadcast_to(sinks.astype(jnp.float32).reshape(1, 1, N_KV_HEADS, G, 1, 1),
                                scores.shape[:-1] + (1,))
    probs = jax.nn.softmax(jnp.concatenate([scores, sink_col], axis=-1), axis=-1)[..., :-1]
    out = jnp.einsum('bnkgqs,bnskd->bnqkgd', probs.astype(v.dtype), vv)
    return out.reshape(B, S, Q_WIDTH)


def chunked_spatial_gating(u, v, w_spatial, b_spatial, g_ln, b_ln):
    B, S = u.shape[0], u.shape[1]
    nc = S // GMLP_CHUNK
    cg = GMLP_WIDTH // GMLP_GROUPS
    v = layernorm(v, g_ln, b_ln)
    vc = v.reshape(B, nc, GMLP_CHUNK, GMLP_GROUPS, cg)
    causal = jnp.tril(jnp.ones((GMLP_CHUNK, GMLP_CHUNK), dtype=w_spatial.dtype))
    w = w_spatial * causal[None]
    mixed = jnp.einsum('gts,bnsgc->bntgc', w, vc) + b_spatial.T[None, None, :, :, None]
    return u * mixed.reshape(B, S, GMLP_WIDTH)


def clamped_swiglu(a, b):
    a = jnp.minimum(a, SWIGLU_LIMIT)
    b = jnp.clip(b, -SWIGLU_LIMIT, SWIGLU_LIMIT)
    return (b + 1.0) * (a * jax.nn.sigmoid(SWIGLU_ALPHA * a))


def moe(h, w_router, b_router, w_gate, b_gate, w_up, b_up, w_down, b_down):
    B, S, D = h.shape
    T = B * S
    hf = h.reshape(T, D)
    logits = (hf @ w_router + b_router).astype(jnp.float32)
    top_val, top_idx = lax.top_k(logits, TOP_K)
    gates = jax.nn.softmax(top_val, axis=-1)
    flat_e = top_idx.reshape(-1).astype(jnp.int32)
    flat_g = gates.reshape(-1)
    order = jnp.argsort(flat_e)
    sorted_e = flat_e[order]
    sorted_tok = (order // TOP_K).astype(jnp.int32)
    sorted_g = flat_g[order]
    counts = jnp.bincount(flat_e, length=N_EXPERTS).astype(jnp.int32)
    padded = (counts + MOE_BLOCK - 1) // MOE_BLOCK * MOE_BLOCK
    pend = jnp.cumsum(padded)
    pstart = pend - padded
    ustart = jnp.cumsum(counts) - counts
    dest = pstart[sorted_e] + jnp.arange(T * TOP_K, dtype=jnp.int32) - ustart[sorted_e]
    capacity = T * TOP_K + N_EXPERTS * MOE_BLOCK
    n_blocks = capacity // MOE_BLOCK
    row_tok = jnp.full((capacity,), T, jnp.int32).at[dest].set(sorted_tok)
    row_g = jnp.zeros((capacity,), jnp.float32).at[dest].set(sorted_g)
    block_e = jnp.minimum(jnp.searchsorted(pend, jnp.arange(n_blocks, dtype=jnp.int32) * MOE_BLOCK, side='right'),
                          N_EXPERTS - 1).astype(jnp.int32)
    h_pad = jnp.concatenate([hf, jnp.zeros((1, D), hf.dtype)], axis=0)

    def expert_block(args):
        tok, g, e = args
        xb = h_pad[tok]
        a = xb @ w_gate[e] + b_gate[e]
        b = xb @ w_up[e] + b_up[e]
        y = clamped_swiglu(a, b) @ w_down[e] + b_down[e]
        return (y * g[:, None]).astype(h.dtype)

    ys = lax.map(expert_block, (row_tok.reshape(n_blocks, MOE_BLOCK), row_g.reshape(n_blocks, MOE_BLOCK), block_e))
    out = jnp.zeros((T + 1, D), h.dtype).at[row_tok].add(ys.reshape(capacity, D))
    return out[:T].reshape(B, S, D)


def setup_inputs(seed: int = 0) -> dict:
    key = jax.random.key(seed)
    ks = jax.random.split(key, 32)
    f32 = jnp.float32
    L, D, E, F = DEPTH, D_MODEL, N_EXPERTS, D_FF_EXPERT

    def nrm(k, shape, scale):
        return jax.random.normal(k, shape, f32) * scale

    return {
        "x": nrm(ks[0], (BATCH, SEQ, D), 1.0),
        "p": nrm(ks[1], (DEPTH, BATCH, SEQ, PLE_DIM), 1.0),
        "g_mix": 1.0 + nrm(ks[2], (L, D), 0.02),
        "w_in": nrm(ks[3], (L, D, IN_WIDTH), D ** -0.5),
        "attn_sinks": nrm(ks[4], (L, N_HEADS), 1.0),
        "g_sgu": 1.0 + nrm(ks[5], (L, GMLP_WIDTH), 0.02),
        "b_sgu": nrm(ks[6], (L, GMLP_WIDTH), 0.02),
        "w_spatial": nrm(ks[7], (L, GMLP_GROUPS, GMLP_CHUNK, GMLP_CHUNK), GMLP_CHUNK ** -0.5),
        "b_spatial": nrm(ks[8], (L, GMLP_GROUPS, GMLP_CHUNK), 0.02),
        "w_attn_proj": nrm(ks[9], (L, Q_WIDTH, D), Q_WIDTH ** -0.5),
        "w_sgu_proj": nrm(ks[10], (L, GMLP_WIDTH, D), GMLP_WIDTH ** -0.5),
        "w_out": nrm(ks[11], (L, D, D), D ** -0.5),
        "g_ffn": 1.0 + nrm(ks[12], (L, D), 0.02),
        "w_router": nrm(ks[13], (L, D, E), D ** -0.5),
        "b_router": nrm(ks[14], (L, E), 0.01),
        "w_gate": nrm(ks[15], (L, E, D, F), D ** -0.5),
        "b_gate": nrm(ks[16], (L, E, F), 0.01),
        "w_up": nrm(ks[17], (L, E, D, F), D ** -0.5),
        "b_up": nrm(ks[18], (L, E, F), 0.01),
        "w_down": nrm(ks[19], (L, E, F, D), F ** -0.5),
        "b_down": nrm(ks[20], (L, E, D), 0.01),
        "g_ple": 1.0 + nrm(ks[21], (L, D), 0.02),
        "w_ple_gate": nrm(ks[22], (L, D, D), D ** -0.5),
        "w_ple_proj": nrm(ks[23], (L, PLE_DIM, D), PLE_DIM ** -0.5),
        "g_final": 1.0 + nrm(ks[24], (D,), 0.02),
    }


def reference(x, p, g_mix, w_in, attn_sinks, g_sgu, b_sgu, w_spatial, b_spatial, w_attn_proj, w_sgu_proj,
              w_out, g_ffn, w_router, b_router, w_gate, b_gate, w_up, b_up, w_down, b_down,
              g_ple, w_ple_gate, w_ple_proj, g_final):
    B, S = x.shape[0], x.shape[1]
    for i in range(DEPTH):
        h = rmsnorm(x, g_mix[i])
        z = h @ w_in[i]
        q, k, v, gu, gv, ga, gb = jnp.split(z, IN_SPLITS, axis=-1)
        attn = sliding_window_attention(q.reshape(B, S, N_HEADS, HEAD_DIM),
                                        k.reshape(B, S, N_KV_HEADS, HEAD_DIM),
                                        v.reshape(B, S, N_KV_HEADS, HEAD_DIM),
                                        attn_sinks[i])
        sgu = chunked_spatial_gating(jax.nn.gelu(gu), jax.nn.gelu(gv), w_spatial[i], b_spatial[i],
                                     g_sgu[i], b_sgu[i])
        merged = jax.nn.sigmoid(ga) * (attn @ w_attn_proj[i]) + jax.nn.sigmoid(gb) * (sgu @ w_sgu_proj[i])
        x = x + merged @ w_out[i]
        x = x + moe(rmsnorm(x, g_ffn[i]), w_router[i], b_router[i], w_gate[i], b_gate[i],
                    w_up[i], b_up[i], w_down[i], b_down[i])
        hp = rmsnorm(x, g_ple[i])
        x = x + jax.nn.sigmoid(hp @ w_ple_gate[i]) * (p[i] @ w_ple_proj[i])
    return rmsnorm(x, g_final)
```

```python
from contextlib import ExitStack
import numpy as np
import concourse.bass as bass
import concourse.mybir as mybir
from concourse.bass_utils import run_bass_kernel_spmd

F32 = mybir.dt.float32
BF16 = mybir.dt.bfloat16
I32 = mybir.dt.int32
U32 = mybir.dt.uint32
AF = mybir.ActivationFunctionType
ALU = mybir.AluOpType

NCORES = 8
D = 1024
T = 4096
NT = 32
TPS = 16
NG = 8
NE = 32
CAP = 640
NB = CAP // 128
NSLOT = NE * CAP
BIGF = 1.0e6
QO, KO, VO, GUO, GVO, GAO, GBO = 0, 512, 640, 768, 1280, 1792, 2816
INW = 3840
RMS_EPS = 1e-6
LN_EPS = 1e-5
CSTW = 128 * 3 + 32 + 256 + 1024 + 256


class Buf:
    __slots__ = ("name", "w", "r")

    def __init__(self, name=""):
        self.name = name
        self.w = None
        self.r = []


class Sched:
    def __init__(self, nc, n_dma_sems=32):
        self.nc = nc
        self.engs = {"pe": nc.tensor, "act": nc.scalar, "dve": nc.vector, "pool": nc.gpsimd, "sp": nc.sync}
        self.sems = {}
        self.cnt = {}
        self._ctx = []
        for k in list(self.engs) + ["d%d" % i for i in range(n_dma_sems)]:
            cm = nc.semaphore("s_" + k)
            self.sems[k] = cm.__enter__()
            self._ctx.append(cm)
            self.cnt[k] = 0
        half = n_dma_sems // 2
        self.dma_keys = {"sp": ["d%d" % i for i in range(half)], "pool": ["d%d" % i for i in range(half, n_dma_sems)]}
        self.dma_rr = {"sp": 0, "pool": 0}
        self.seen = {e: {} for e in self.engs}
        self.pe_pending = False

    def close(self):
        for cm in reversed(self._ctx):
            cm.__exit__(None, None, None)

    def _wait(self, e, deps):
        need = {}
        for d in deps:
            if d is None:
                continue
            k, v = d
            if k == "pe" and e == "pe":
                continue
            if v > need.get(k, 0):
                need[k] = v
        for k, v in need.items():
            if self.seen[e].get(k, 0) >= v:
                continue
            assert v <= self.cnt[k], (e, k, v, self.cnt[k])
            self.engs[e].wait_ge(self.sems[k], v)
            self.seen[e][k] = v

    @staticmethod
    def _deps(reads, writes):
        deps = []
        for b in reads:
            deps.append(b.w)
        for b in writes:
            deps.append(b.w)
            deps.extend(b.r)
        return deps

    def _record(self, tok, reads, writes):
        for b in reads:
            b.r.append(tok)
            if len(b.r) > 64:
                best = {}
                for k, v in b.r:
                    if v > best.get(k, 0):
                        best[k] = v
                b.r = list(best.items())
        for b in writes:
            b.w = tok
            b.r = []

    def op(self, e, fn, reads=(), writes=(), inc=True):
        self._wait(e, self._deps(reads, writes))
        ins = fn(self.engs[e])
        if inc:
            self.cnt[e] += 1
            ins.then_inc(self.sems[e], 1)
            tok = (e, self.cnt[e])
            if e == "pe":
                self.pe_pending = False
        else:
            assert e == "pe"
            tok = (e, self.cnt[e] + 1)
            self.pe_pending = True
        self._record(tok, reads, writes)
        return tok

    def dma(self, q, fn, reads=(), writes=(), track=None):
        keys = self.dma_keys[q]
        k = keys[self.dma_rr[q] % len(keys)]
        self.dma_rr[q] += 1
        deps = self._deps(reads, writes)
        if self.cnt[k] > 0:
            deps.append((k, self.cnt[k]))
        self._wait(q, deps)
        ins = fn(self.engs[q])
        self.cnt[k] += 16
        ins.then_inc(self.sems[k], 16)
        tok = (k, self.cnt[k])
        self._record(tok, reads, writes)
        if track is not None:
            track.append(tok)
        return tok

    def barrier(self):
        assert not self.pe_pending
        for e in self.engs:
            self._wait(e, [(k, v) for k, v in self.cnt.items() if v > 0])


class _Stop(Exception):
    pass


def build_nc(stop=None, dumps=()):
    nc = bass.Bass("TRN2", target_bir_lowering=False)

    def din(name, shape, dt=F32):
        return nc.dram_tensor(name, list(shape), dt, kind="ExternalInput").ap()

    x_d = din("x", [T, D])
    p_d = din("p", [T, 256])
    win_d = din("w_in", [D, INW])
    wa_d = din("w_attn_proj", [512, D])
    wb_d = din("w_sgu_proj", [512, D])
    wout_d = din("w_out", [D, D])
    wr_d = din("w_router", [D, NE])
    wg_d = din("w_gate", [NE, D, D])
    wu_d = din("w_up", [NE, D, D])
    wd_d = din("w_down", [NE, D, D])
    bd_d = din("b_down", [NE, D])
    wpg_d = din("w_ple_gate", [D, D])
    wpp_d = din("w_ple_proj", [256, D])
    ws_d = din("w_spatial", [4, 128, 128])
    cvec_d = din("cvec", [128, 32])
    bgT_d = din("bgT", [128, NE * 8])
    buT_d = din("buT", [128, NE * 8])
    rowp_d = din("rowp", [128, 32 + 4 + 512 + 1024])
    cst_d = din("cst", [128, CSTW])
    out_d = nc.dram_tensor("out", [T, D], F32, kind="ExternalOutput").ap()
    x1_d = nc.dram_tensor("x1_scr", [T, D], F32, kind="Internal").ap()
    xs_d = nc.dram_tensor("xs_scr", [NSLOT + 128, D], BF16, kind="Internal").ap()
    ys_d = nc.dram_tensor("ys_scr", [NSLOT + 128, D], F32, kind="Internal").ap()

    S = Sched(nc)
    uid = [0]

    def alloc(es, shape, dt, name="t"):
        uid[0] += 1
        t = es.enter_context(nc.sbuf_tensor("%s_%d" % (name, uid[0]), list(shape), dt))
        return t, Buf(name)

    def palloc(es, shape, dt, name="p"):
        uid[0] += 1
        t = es.enter_context(nc.psum_tensor("%s_%d" % (name, uid[0]), list(shape), dt))
        return t, Buf(name)

    class Ring:
        def __init__(self, items):
            self.items = items
            self.i = 0

        def next(self):
            it = self.items[self.i % len(self.items)]
            self.i += 1
            return it

    def pe(fn, r=(), w=(), inc=True):
        return S.op("pe", fn, r, w, inc=inc)

    def act(fn, r=(), w=()):
        return S.op("act", fn, r, w)

    def dve(fn, r=(), w=()):
        return S.op("dve", fn, r, w)

    def pool(fn, r=(), w=()):
        return S.op("pool", fn, r, w)

    def mmgroup(out_ap, outB, pairs, rB, first=True, last=True):
        n = len(pairs)
        for i, (l, r) in enumerate(pairs):
            st = first and i == 0
            sp_ = last and i == n - 1
            pe(lambda e: e.matmul(out_ap, lhsT=l, rhs=r, start=st, stop=sp_), rB, [outB], inc=(i == n - 1))

    out_tokens = []
    dump_names = []

    def dump(name, ap, B, cols):
        if name not in dumps:
            return
        dd = nc.dram_tensor("dbg_" + name, [128, cols], F32, kind="ExternalOutput").ap()
        dump_names.append(name)
        with nc.sbuf_tensor("dbgst_" + name, [128, cols], F32) as stg:
            sB = Buf("stg")
            dve(lambda e: e.tensor_copy(out=stg[:], in_=ap), [B], [sB])
            S.dma("sp", lambda e: e.dma_start(out=dd, in_=stg[:]), [sB], ())
            S.barrier()

    def check_stop(tag):
        if stop == tag:
            raise _Stop()

    try:
        _body(locals())
    except _Stop:
        pass
    S.barrier()
    S.close()
    return nc


def _body(L):
    globals_ = L
    nc = L["nc"]; S = L["S"]; alloc = L["alloc"]; palloc = L["palloc"]; Ring = L["Ring"]
    pe = L["pe"]; act = L["act"]; dve = L["dve"]; pool = L["pool"]; mmgroup = L["mmgroup"]
    out_tokens = L["out_tokens"]; dump = L["dump"]; check_stop = L["check_stop"]
    x_d = L["x_d"]; p_d = L["p_d"]; win_d = L["win_d"]; wa_d = L["wa_d"]; wb_d = L["wb_d"]; wout_d = L["wout_d"]
    wr_d = L["wr_d"]; wg_d = L["wg_d"]; wu_d = L["wu_d"]; wd_d = L["wd_d"]; bd_d = L["bd_d"]; wpg_d = L["wpg_d"]
    wpp_d = L["wpp_d"]; ws_d = L["ws_d"]; cvec_d = L["cvec_d"]; bgT_d = L["bgT_d"]; buT_d = L["buT_d"]
    rowp_d = L["rowp_d"]; cst_d = L["cst_d"]; out_d = L["out_d"]; x1_d = L["x1_d"]; xs_d = L["xs_d"]; ys_d = L["ys_d"]

    with ExitStack() as glob:
        ident16, ident16B = alloc(glob, [128, 128], BF16, "ident16")
        ident32, ident32B = alloc(glob, [128, 128], F32, "ident32")
        ones16, ones16B = alloc(glob, [128, 128], BF16, "ones16")
        cvec, cvecB = alloc(glob, [128, 32], F32, "cvec")
        bgT, bgTB = alloc(glob, [128, NE * 8], F32, "bgT")
        bu1T, bu1TB = alloc(glob, [128, NE * 8], F32, "bu1T")
        gates_all, _ = alloc(glob, [128, NT, 4], F32, "gates")
        slots_all, _ = alloc(glob, [128, NT, 4], I32, "slots")
        gateB = [Buf("gate%d" % t) for t in range(NT)]
        slotB = [Buf("slot%d" % t) for t in range(NT)]
        stat, _ = alloc(glob, [128, 128], F32, "stat")
        statR = Ring([(stat[:, i:i + 1], Buf("stat%d" % i)) for i in range(128)])

        S.dma("sp", lambda e: e.dma_start(out=cvec[:], in_=cvec_d), (), [cvecB])
        S.dma("sp", lambda e: e.dma_start(out=bgT[:], in_=bgT_d), (), [bgTB])
        S.dma("sp", lambda e: e.dma_start(out=bu1T[:], in_=buT_d), (), [bu1TB])
        dve(lambda e: e.tensor_scalar_add(out=bu1T[:], in0=bu1T[:], scalar1=1.0), [bu1TB], [bu1TB])
        pool(lambda e: e.memset(ones16[:], 1.0), (), [ones16B])

        def rms_sd(src_ap, srcB, junk_ap, junkB):
            ss, ssB = statR.next()
            act(lambda e: e.activation(out=junk_ap, in_=src_ap, func=AF.Square, accum_out=ss), [srcB], [junkB, ssB])
            sd, sdB = statR.next()
            act(lambda e: e.activation(out=sd, in_=ss, func=AF.Sqrt, bias=RMS_EPS, scale=1.0 / D), [ssB], [sdB])
            return sd, sdB

        def rms_recip(sd, sdB):
            rs, rsB = statR.next()
            dve(lambda e: e.reciprocal(out=rs, in_=sd), [sdB], [rsB])
            return rs, rsB

        def rms_rstd(src_ap, srcB, junk_ap, junkB):
            sd, sdB = rms_sd(src_ap, srcB, junk_ap, junkB)
            return rms_recip(sd, sdB)

        with ExitStack() as pa:
            win, _ = alloc(pa, [128, 8, INW], BF16, "win")
            winB = [Buf("win%d" % kc) for kc in range(8)]
            wa, _ = alloc(pa, [128, 4, D], BF16, "wa")
            waB = [Buf("wa0"), Buf("wa1")]
            wb, wbB = alloc(pa, [128, 4, D], BF16, "wb")
            wout, woutB = alloc(pa, [128, 8, D], BF16, "wout")
            wr, wrB = alloc(pa, [128, 8, NE], F32, "wr")
            rowp, rowpB = alloc(pa, [128, 32 + 4 + 512], F32, "rowp")
            kaug, kaugB = alloc(pa, [2, 2, 128], BF16, "kaug")
            qaug, qaugB = alloc(pa, [2, 2, 512], BF16, "qaug")
            mask01, mask01B = alloc(pa, [128, 2, 128], BF16, "mask01")
            triU, triUB = alloc(pa, [128, 128], BF16, "triU")
            iota, iotaB = alloc(pa, [128, 32], F32, "iota")
            wsT, wsTB = alloc(pa, [128, 4, 128], BF16, "wsT")
            comb, combB = alloc(pa, [128, 4, 128], F32, "comb")
            cnt, cntB = alloc(pa, [128, 32], F32, "cnt")

            tpR = Ring([palloc(pa, [128, 1024], BF16, "tp") for _ in range(2)])
            fR = Ring([palloc(pa, [128, 512], F32, "f") for _ in range(6)])

            S.dma("sp", lambda e: e.dma_start(out=wr[:], in_=wr_d.rearrange("(c p) o -> p c o", p=128)), (), [wrB])
            S.dma("sp", lambda e: e.dma_start(out=rowp[:], in_=rowp_d[:, 0:32 + 4 + 512]), (), [rowpB])
            brt = rowp[:, 0:32]
            expsink = rowp[:, 32:36]
            bspat = rowp[:, 36:36 + 512].rearrange("p (g t) -> p g t", g=4)

            with ExitStack() as st:
                cst, cstB = alloc(st, [128, CSTW], F32, "cst")
                wsn, wsnB = alloc(st, [128, 4, 128], F32, "wsn")
                S.dma("sp", lambda e: e.dma_start(out=cst[:], in_=cst_d), (), [cstB])
                S.dma("sp", lambda e: e.dma_start(out=wsn[:], in_=ws_d.rearrange("g t s -> t g s")), (), [wsnB])
                pool(lambda e: e.memset(cnt[:], 0.0), (), [cntB])
                zf, zfB = alloc(st, [128, D], F32, "zf")
                pool(lambda e: e.memset(zf[:], 0.0), (), [zfB])
                S.dma("sp", lambda e: e.dma_start(out=ys_d[NSLOT:NSLOT + 128, :], in_=zf[:]), [zfB], ())
                dve(lambda e: e.tensor_copy(out=ident32[:], in_=cst[:, 0:128]), [cstB], [ident32B])
                dve(lambda e: e.tensor_copy(out=ident16[:], in_=cst[:, 0:128]), [cstB], [ident16B])
                dve(lambda e: e.tensor_copy(out=triU[:], in_=cst[:, 128:256]), [cstB], [triUB])
                dve(lambda e: e.tensor_copy(out=iota[:], in_=cst[:, 384:416]), [cstB], [iotaB])
                dve(lambda e: e.tensor_copy(out=kaug[:], in_=cst[0:2, 416:672].rearrange("p (j n) -> p j n", j=2)), [cstB], [kaugB])
                dve(lambda e: e.tensor_copy(out=qaug[:], in_=cst[0:2, 672:1696].rearrange("p (k n) -> p k n", k=2)), [cstB], [qaugB])
                dve(lambda e: e.tensor_copy(out=mask01[:], in_=cst[:, 1696:1952].rearrange("p (j n) -> p j n", j=2)), [cstB], [mask01B])
                act(lambda e: e.activation(out=expsink, in_=expsink, func=AF.Exp), [rowpB], [rowpB])
                for g in range(4):
                    f, fB = fR.next()
                    pe(lambda e: e.transpose(out=f[:, 0:128], in_=wsn[:, g, :], identity=ident32[:]), [wsnB, ident32B], [fB])
                    dve(lambda e: e.tensor_tensor(out=wsT[:, g, :], in0=f[:, 0:128], in1=cst[:, 256:384], op=ALU.mult), [fB, cstB], [wsTB])
                f, fB = fR.next()
                mmgroup(f[:, :], fB, [(ones16[:, :], wsT[:, :, :].rearrange("p g t -> p (g t)"))], [ones16B, wsTB])
                for g in range(4):
                    dve(lambda e: e.scalar_tensor_tensor(out=comb[:, g, :], in0=f[:, g * 128:(g + 1) * 128], scalar=cvec[:, 28 + g:29 + g],
                                                         in1=bspat[:, g, :], op0=ALU.mult, op1=ALU.add), [fB, cvecB, rowpB], [combB])
                S.barrier()
            check_stop("setup")
            for kc in range(8):
                S.dma("pool", lambda e: e.dma_start(out=win[:, kc, :], in_=win_d[kc * 128:(kc + 1) * 128, :]), (), [winB[kc]])
            for k in range(2):
                S.dma("pool", lambda e: e.dma_start(
                    out=wa[k * 64:(k + 1) * 64, :, :],
                    in_=wa_d[k * 256:(k + 1) * 256, :].rearrange("(g hd) o -> hd g o", g=4)), (), [waB[k]])
            S.dma("pool", lambda e: e.dma_start(out=wb[:], in_=wb_d.rearrange("(c p) o -> p c o", p=128)), (), [wbB])
            S.dma("pool", lambda e: e.dma_start(out=wout[:], in_=wout_d.rearrange("(c p) o -> p c o", p=128)), (), [woutB])
            zt, ztB = alloc(pa, [128, 2 * D], BF16, "zt")
            pool(lambda e: e.memset(zt[:], 0.0), (), [ztB])
            xszB = Buf("xsz")
            xs_z = xs_d[0:NSLOT, :].rearrange("(n p r) c -> n p (r c)", p=128, r=2)
            xsz_tokens = []
            for n in range(NSLOT // 256):
                S.dma("pool", lambda e: e.dma_start(out=xs_z[n], in_=zt[:]), [ztB], (), track=xsz_tokens)

            xR = Ring([alloc(pa, [128, D], F32, "x") for _ in range(4)])
            xnR = Ring([alloc(pa, [128, D], BF16, "xn") for _ in range(2)])
            hT, hTB = alloc(pa, [128, 8, 512], BF16, "hT")
            qT, qTB = alloc(pa, [128, 4, 512], BF16, "qT")
            kT, kTB = alloc(pa, [128, 5 * 128], BF16, "kT")
            vS, vSB = alloc(pa, [128, 5, 128], BF16, "vS")
            guT, guTB = alloc(pa, [128, 4, 512], BF16, "guT")
            gvgR = Ring([alloc(pa, [128, 512], F32, "gvg") for _ in range(2)])
            vln, vlnB = alloc(pa, [128, 4, 512], BF16, "vln")
            bnR = Ring([alloc(pa, [128, 8], F32, "bnst") for _ in range(2)])
            ptR = Ring([alloc(pa, [128, 2, 2, 512], BF16, "pt") for _ in range(2)])
            attnT, attnTB = alloc(pa, [128, 4, 512], BF16, "attnT")
            rden, rdenB = alloc(pa, [128, 512], F32, "rden")
            sguT, sguTB = alloc(pa, [128, 4, 512], BF16, "sguT")
            sgtmp, sgtmpB = alloc(pa, [128, 512], F32, "sgtmp")
            sigAR = Ring([alloc(pa, [128, 512], F32, "sigA") for _ in range(1)])
            sigBR = Ring([alloc(pa, [128, 512], F32, "sigB") for _ in range(1)])
            mrgT, mrgTB = alloc(pa, [128, 8, 512], BF16, "mrgT")
            hn4 = [alloc(pa, [128, D], BF16, "hn") for _ in range(4)]
            xgT, xgTB = alloc(pa, [128, 8, 128], F32, "xgT")
            lg4, lg4B = alloc(pa, [128, 4, 32], F32, "lg4")
            mx8, mx8B = alloc(pa, [128, 4, 8], F32, "mx8")
            ix8, ix8B = alloc(pa, [128, 4, 8], U32, "ix8")
            ixf, ixfB = alloc(pa, [128, 4, 4], F32, "ixf")
            ex4, ex4B = alloc(pa, [128, 4, 4], F32, "ex4")
            sm4, sm4B = alloc(pa, [128, 4], F32, "sm4")
            oh, ohB = alloc(pa, [128, 4, 4, 32], F32, "oh")
            msk16, msk16B = alloc(pa, [128, 4, 32], BF16, "msk16")
            posf, posfB = alloc(pa, [128, 4, 32], F32, "posf")
            ohp, ohpB = alloc(pa, [128, 4, 4, 32], F32, "ohp")
            p4, p4B = alloc(pa, [128, 4, 4], F32, "p4")
            slf, slfB = alloc(pa, [128, 4, 4], F32, "slf")
            ovf, ovfB = alloc(pa, [128, 4, 4], F32, "ovf")

            gmixT = cvec[:, 0:8]
            gffnT = cvec[:, 8:16]

            pending_route = []
            s1st = {}

            def route_a(t0):
                tl = list(range(t0, t0 + 4))
                for i in range(4):
                    dve(lambda e: e.max(out=mx8[:, i, :], in_=lg4[:, i, :]), [lg4B], [mx8B])
                    dve(lambda e: e.max_index(out=ix8[:, i, :], in_max=mx8[:, i, :], in_values=lg4[:, i, :]), [lg4B, mx8B], [ix8B])
                dve(lambda e: e.tensor_copy(out=ixf[:], in_=ix8[:, :, 0:4]), [ix8B], [ixfB])
                dve(lambda e: e.tensor_tensor(out=ex4[:], in0=mx8[:, :, 0:4], in1=mx8[:, :, 0:1].to_broadcast([128, 4, 4]), op=ALU.subtract), [mx8B], [ex4B])
                act(lambda e: e.activation(out=ex4[:], in_=ex4[:], func=AF.Exp), [ex4B], [ex4B])
                dve(lambda e: e.reduce_sum(out=sm4[:], in_=ex4[:], axis=mybir.AxisListType.X), [ex4B], [sm4B])
                dve(lambda e: e.reciprocal(out=sm4[:], in_=sm4[:]), [sm4B], [sm4B])
                dve(lambda e: e.tensor_tensor(out=gates_all[:, t0:t0 + 4, :], in0=ex4[:], in1=sm4[:, :].unsqueeze(2).to_broadcast([128, 4, 4]), op=ALU.mult),
                    [ex4B, sm4B], [gateB[t] for t in tl])
                dve(lambda e: e.tensor_tensor(out=oh[:], in0=iota[:, :].unsqueeze(1).unsqueeze(1).to_broadcast([128, 4, 4, 32]),
                                              in1=ixf[:, :, :].unsqueeze(3).to_broadcast([128, 4, 4, 32]), op=ALU.is_equal), [iotaB, ixfB], [ohB])
                with nc.allow_low_precision(reason="0/1 mask sums are exact in bf16"):
                    dve(lambda e: e.tensor_reduce(out=msk16[:], in_=oh[:, :, :, :].rearrange("p i k e -> p i e k"), axis=mybir.AxisListType.X, op=ALU.add),
                        [ohB], [msk16B])

            def route_b(t0):
                tl = list(range(t0, t0 + 4))
                f, fB = fR.next()
                for i in range(4):
                    pairs = [(triU[:, :], msk16[:, i, :])] + [(ones16[:, :], msk16[:, i2, :]) for i2 in range(i)]
                    mmgroup(f[:, i * 32:(i + 1) * 32], fB, pairs, [triUB, ones16B, msk16B])
                mmgroup(f[:, 128:160], fB, [(ones16[:, :], msk16[:, i, :]) for i in range(4)], [ones16B, msk16B])
                dve(lambda e: e.tensor_tensor(out=posf[:], in0=f[:, 0:128].rearrange("p (i e) -> p i e", i=4),
                                              in1=cnt[:, :].unsqueeze(1).to_broadcast([128, 4, 32]), op=ALU.add), [fB, cntB], [posfB])
                dve(lambda e: e.tensor_tensor(out=cnt[:], in0=f[:, 128:160], in1=cnt[:], op=ALU.add), [fB, cntB, posfB], [cntB])
                dve(lambda e: e.tensor_tensor(out=ohp[:], in0=oh[:], in1=posf[:, :, :].unsqueeze(2).to_broadcast([128, 4, 4, 32]), op=ALU.mult), [ohB, posfB], [ohpB])
                dve(lambda e: e.reduce_sum(out=p4[:], in_=ohp[:], axis=mybir.AxisListType.X), [ohpB], [p4B])
                dve(lambda e: e.scalar_tensor_tensor(out=slf[:], in0=ixf[:], scalar=float(CAP), in1=p4[:], op0=ALU.mult, op1=ALU.add), [ixfB, p4B], [slfB])
                dve(lambda e: e.tensor_scalar(out=ovf[:], in0=p4[:], scalar1=float(CAP), scalar2=BIGF, op0=ALU.is_ge, op1=ALU.mult), [p4B], [ovfB])
                dve(lambda e: e.tensor_tensor(out=slf[:], in0=slf[:], in1=ovf[:], op=ALU.max), [slfB, ovfB], [slfB])
                dve(lambda e: e.tensor_scalar(out=slf[:], in0=slf[:], scalar1=float(NSLOT), scalar2=0.0, op0=ALU.min, op1=ALU.max), [slfB], [slfB])
                dve(lambda e: e.tensor_copy(out=slots_all[:, t0:t0 + 4, :], in_=slf[:]), [slfB], [slotB[t] for t in tl])
                if t0 == 0:
                    dump("lg", lg4[:, 0, :], lg4B, 32)
                    dump("gates", gates_all[:, 0, :], gateB[0], 4)
                    dump("slf", slf[:, 0, :], slfB, 4)
                    dump("posf", posf[:, 0, :], posfB, 32)
                S._wait("pool", xsz_tokens)
                for i in range(4):
                    hn, hnB = hn4[i]
                    for k in range(4):
                        S.dma("pool", lambda e: e.indirect_dma_start(
                            out=xs_d, out_offset=bass.IndirectOffsetOnAxis(ap=slots_all[:, t0 + i, k:k + 1], axis=0),
                            in_=hn[:, :], in_offset=None), [hnB, slotB[t0 + i]], ())

            for gi in range(NG):
                t0 = gi * 4
                def s1_load(tg, i):
                    t = tg * 4 + i
                    xt, xtB = xR.next()
                    S.dma("sp", lambda e: e.dma_start(out=xt[:], in_=x_d[t * 128:(t + 1) * 128, :]), (), [xtB])
                    s1st[(tg, i)] = dict(xt=xt, xtB=xtB)

                def s1a(tg, i):
                    c = s1st[(tg, i)]
                    xt, xtB = c["xt"], c["xtB"]
                    xn, xnB = xnR.next()
                    rs, rsB = rms_rstd(xt[:], xtB, xn[:], xnB)
                    dve(lambda e: e.tensor_scalar_mul(out=xn[:], in0=xt[:], scalar1=rs), [xtB, rsB], [xnB])
                    c.update(xn=xn, xnB=xnB)

                def s1b(tg, i):
                    c = s1st.pop((tg, i))
                    xn, xnB = c["xn"], c["xnB"]
                    tp, tpB = tpR.next()
                    for kc in range(8):
                        pe(lambda e: e.transpose(out=tp[:, kc * 128:(kc + 1) * 128], in_=xn[:, kc * 128:(kc + 1) * 128], identity=ident16[:]),
                           [xnB, ident16B], [tpB], inc=(kc == 7))
                    dve(lambda e: e.tensor_tensor(out=hT[:, :, i * 128:(i + 1) * 128], in0=tp[:, :].rearrange("p (c t) -> p c t", c=8),
                                                  in1=gmixT.unsqueeze(2).to_broadcast([128, 8, 128]), op=ALU.mult), [tpB, cvecB], [hTB])

                if gi == 0:
                    for i in range(4):
                        s1_load(0, i)
                        s1a(0, i)
                        s1b(0, i)
                if gi == 0:
                    for kc in range(8):
                        dump("hT%d" % kc, hT[:, kc, :], hTB, 512)
                    check_stop("g0s1")

                def inproj_fm(col0, evac):
                    f, fB = fR.next()
                    mmgroup(f[:, :], fB, [(win[:, kc, col0:col0 + 128], hT[:, kc, :]) for kc in range(8)], [hTB] + winB)
                    evac(f, fB)

                for t0r in pending_route:
                    route_a(t0r)
                gst = {}

                def gv_mm(i):
                    f, fB = fR.next()
                    mmgroup(f[:, :], fB, [(hT[:, kc, i * 128:(i + 1) * 128], win[:, kc, GVO:GVO + 512]) for kc in range(8)], [hTB] + winB)
                    gvg, gvgB = gvgR.next()
                    act(lambda e: e.activation(out=gvg[:], in_=f[:, :], func=AF.Gelu_apprx_tanh), [fB], [gvgB])
                    gst[i] = (gvg, gvgB)

                def gv_ln(i):
                    gvg, gvgB = gst[i]
                    bn, bnB = bnR.next()
                    dve(lambda e: e.bn_stats(out=bn[:, 0:6], in_=gvg[:]), [gvgB], [bnB])
                    dve(lambda e: e.bn_aggr(out=bn[:, 6:8], in_=bn[:, 0:6]), [bnB], [bnB])
                    sd, sdB = statR.next()
                    act(lambda e: e.activation(out=sd, in_=bn[:, 7:8], func=AF.Sqrt, bias=LN_EPS, scale=1.0), [bnB], [sdB])
                    rs, rsB = statR.next()
                    dve(lambda e: e.reciprocal(out=rs, in_=sd), [sdB], [rsB])
                    dve(lambda e: e.tensor_scalar(out=vln[:, i, :], in0=gvg[:], scalar1=bn[:, 6:7], scalar2=rs, op0=ALU.subtract, op1=ALU.mult),
                        [gvgB, bnB, rsB], [vlnB])

                for i in range(5):
                    if i < 4:
                        gv_mm(i)
                    if i >= 1:
                        gv_ln(i - 1)

                for g in range(4):
                    inproj_fm(QO + g * 128, lambda f, fB: act(lambda e: e.copy(out=qT[:, g, :], in_=f[:, :]), [fB], [qTB]))
                inproj_fm(KO, lambda f, fB: act(lambda e: e.copy(out=kT[:, 128:640], in_=f[:, :]), [fB], [kTB]))
                while pending_route:
                    route_b(pending_route.pop(0))
                for c in range(4):
                    inproj_fm(GUO + c * 128, lambda f, fB: act(lambda e: e.activation(out=guT[:, c, :], in_=f[:, :], func=AF.Gelu_apprx_tanh), [fB], [guTB]))
                f, fB = fR.next()
                for i in range(4):
                    mmgroup(f[:, i * 128:(i + 1) * 128], fB, [(hT[:, kc, i * 128:(i + 1) * 128], win[:, kc, VO:VO + 128]) for kc in range(8)], [hTB] + winB)
                act(lambda e: e.copy(out=vS[:, 1:5, :], in_=f[:, :].rearrange("p (i c) -> p i c", i=4)), [fB], [vSB])

                if gi == 0:
                    for g in range(4):
                        dump("qT%d" % g, qT[:, g, :], qTB, 512)
                        dump("guT%d" % g, guT[:, g, :], guTB, 512)
                        dump("vln%d" % g, vln[:, g, :], vlnB, 512)
                    dump("kT", kT[:, :], kTB, 640)
                    dump("vS", vS[:, :, :].rearrange("p a b -> p (a b)"), vSB, 640)
                    check_stop("g0s2")
                for cg in range(4):
                    f, fB = fR.next()
                    for i in range(4):
                        mmgroup(f[:, i * 128:(i + 1) * 128], fB, [(vln[:, i, cg * 128:(cg + 1) * 128], wsT[:, cg, :])], [vlnB, wsTB])
                    dve(lambda e: e.scalar_tensor_tensor(out=sgtmp[:, :].rearrange("p (i t) -> p i t", i=4), in0=f[:, :].rearrange("p (i t) -> p i t", i=4),
                                                         scalar=cvec[:, 24 + cg:25 + cg], in1=comb[:, cg, :].unsqueeze(1).to_broadcast([128, 4, 128]),
                                                         op0=ALU.mult, op1=ALU.add), [fB, cvecB, combB], [sgtmpB])
                    dve(lambda e: e.tensor_tensor(out=sguT[:, cg, :], in0=sgtmp[:], in1=guT[:, cg, :], op=ALU.mult), [sgtmpB, guTB], [sguTB])

                ast = {}

                def att_scores(i):
                    t = t0 + i
                    n = t % TPS
                    js = [(0, i), (1, i + 1)] if n > 0 else [(1, i + 1)]
                    pt, ptB = ptR.next()
                    for k in range(2):
                        for (j, slot) in js:
                            f, fB = fR.next()
                            mmgroup(f[:, :], fB, [(kT[k * 64:(k + 1) * 64, slot * 128:(slot + 1) * 128], qT[k * 64:(k + 1) * 64, :, i * 128:(i + 1) * 128]),
                                                  (kaug[0:2, j, :], qaug[0:2, k, :])], [kTB, qTB, kaugB, qaugB])
                            act(lambda e: e.activation(out=pt[:, k, j, :], in_=f[:, :], func=AF.Exp, scale=0.125), [fB], [ptB])
                            dve(lambda e: e.tensor_tensor(out=pt[:, k, j, :].rearrange("p (g q) -> p g q", g=4),
                                                           in0=pt[:, k, j, :].rearrange("p (g q) -> p g q", g=4),
                                                           in1=mask01[:, j, :].unsqueeze(1).to_broadcast([128, 4, 128]), op=ALU.mult), [ptB, mask01B], [ptB])
                    ast[i] = (js, pt, ptB)

                def att_pv(i):
                    js, pt, ptB = ast[i]
                    pv, pvB = fR.next()
                    dn, dnB = fR.next()
                    for k in range(2):
                        mmgroup(pv[k * 64:(k + 1) * 64, :], pvB, [(vS[:, slot, k * 64:(k + 1) * 64], pt[:, k, j, :]) for (j, slot) in js], [vSB, ptB])
                    for k in range(2):
                        mmgroup(dn[k * 64:(k + 1) * 64, :], dnB, [(ones16[:, 0:64], pt[:, k, j, :]) for (j, slot) in js], [ones16B, ptB])
                    dve(lambda e: e.tensor_tensor(out=rden[:, :].rearrange("p (g q) -> p g q", g=4), in0=dn[:, :].rearrange("p (g q) -> p g q", g=4),
                                                  in1=expsink.unsqueeze(2).to_broadcast([128, 4, 128]), op=ALU.add), [dnB, rowpB], [rdenB])
                    act(lambda e: e.activation(out=rden[:], in_=rden[:], func=AF.Ln), [rdenB], [rdenB])
                    act(lambda e: e.activation(out=rden[:], in_=rden[:], func=AF.Exp, scale=-1.0), [rdenB], [rdenB])
                    dve(lambda e: e.tensor_tensor(out=attnT[:, :, i * 128:(i + 1) * 128], in0=pv[:, :].rearrange("p (g q) -> p g q", g=4),
                                                  in1=rden[:, :].rearrange("p (g q) -> p g q", g=4), op=ALU.mult), [pvB, rdenB], [attnTB])

                for i in range(5):
                    if i < 4:
                        att_scores(i)
                    if i >= 1:
                        att_pv(i - 1)
                pool(lambda e: e.tensor_copy(out=kT[:, 0:128], in_=kT[:, 512:640]), [kTB], [kTB])
                pool(lambda e: e.tensor_copy(out=vS[:, 0, :], in_=vS[:, 4, :]), [vSB], [vSB])

                if gi == 0:
                    for g in range(4):
                        dump("attnT%d" % g, attnT[:, g, :], attnTB, 512)
                        dump("sguT%d" % g, sguT[:, g, :], sguTB, 512)
                    check_stop("g0s3")
                for oc in range(8):
                    sigA, sigAB = sigAR.next()
                    sigB_, sigBB = sigBR.next()
                    inproj_fm(GAO + oc * 128, lambda f, fB: act(lambda e: e.activation(out=sigA[:], in_=f[:, :], func=AF.Sigmoid), [fB], [sigAB]))
                    inproj_fm(GBO + oc * 128, lambda f, fB: act(lambda e: e.activation(out=sigB_[:], in_=f[:, :], func=AF.Sigmoid), [fB], [sigBB]))
                    f, fB = fR.next()
                    mmgroup(f[:, :], fB, [(wa[:, g, oc * 128:(oc + 1) * 128], attnT[:, g, :]) for g in range(4)], waB + [attnTB])
                    dve(lambda e: e.tensor_tensor(out=sigA[:], in0=f[:, :], in1=sigA[:], op=ALU.mult), [fB, sigAB], [sigAB])
                    f, fB = fR.next()
                    mmgroup(f[:, :], fB, [(wb[:, c, oc * 128:(oc + 1) * 128], sguT[:, c, :]) for c in range(4)], [wbB, sguTB])
                    dve(lambda e: e.tensor_tensor(out=sigB_[:], in0=f[:, :], in1=sigB_[:], op=ALU.mult), [fB, sigBB], [sigBB])
                    dve(lambda e: e.tensor_tensor(out=mrgT[:, oc, :], in0=sigA[:], in1=sigB_[:], op=ALU.add), [sigAB, sigBB], [mrgTB])

                if gi == 0:
                    for kc in range(8):
                        dump("mrgT%d" % kc, mrgT[:, kc, :], mrgTB, 512)
                    check_stop("g0s4")
                s5 = {}

                def s5_op(i):
                    t = t0 + i
                    xr, xrB = xR.next()
                    S.dma("sp", lambda e: e.dma_start(out=xr[:], in_=x_d[t * 128:(t + 1) * 128, :]), (), [xrB])
                    for half in range(2):
                        f, fB = fR.next()
                        mmgroup(f[:, :], fB, [(mrgT[:, kc, i * 128:(i + 1) * 128], wout[:, kc, half * 512:(half + 1) * 512]) for kc in range(8)], [mrgTB, woutB])
                        dve(lambda e: e.tensor_tensor(out=xr[:, half * 512:(half + 1) * 512], in0=f[:, :], in1=xr[:, half * 512:(half + 1) * 512], op=ALU.add),
                            [fB, xrB], [xrB])
                    S.dma("sp", lambda e: e.dma_start(out=x1_d[t * 128:(t + 1) * 128, :], in_=xr[:]), [xrB], ())
                    hn, hnB = hn4[i]
                    sd2, sd2B = rms_sd(xr[:], xrB, hn[:], hnB)
                    s5[i] = dict(xr=xr, xrB=xrB, hn=hn, hnB=hnB, sd2=sd2, sd2B=sd2B)

                def s5_tr(i):
                    c = s5[i]
                    xr, xrB, hn, hnB = c["xr"], c["xrB"], c["hn"], c["hnB"]
                    rs2, rs2B = rms_recip(c["sd2"], c["sd2B"])
                    c["rs2"], c["rs2B"] = rs2, rs2B
                    act(lambda e: e.activation(out=hn[:], in_=xr[:], func=AF.Copy, scale=rs2), [xrB, rs2B], [hnB])
                    for hh in range(2):
                        f, fB = fR.next()
                        for c4 in range(4):
                            kc = hh * 4 + c4
                            pe(lambda e: e.transpose(out=f[:, c4 * 128:(c4 + 1) * 128], in_=xr[:, kc * 128:(kc + 1) * 128], identity=ident32[:]),
                               [xrB, ident32B], [fB], inc=(c4 == 3))
                        dve(lambda e: e.tensor_tensor(out=xgT[:, hh * 4:(hh + 1) * 4, :], in0=f[:, :].rearrange("p (c t) -> p c t", c=4),
                                                      in1=gffnT[:, hh * 4:(hh + 1) * 4].unsqueeze(2).to_broadcast([128, 4, 128]), op=ALU.mult),
                            [fB, cvecB], [xgTB])

                def s5_lg(i):
                    c = s5[i]
                    f, fB = fR.next()
                    mmgroup(f[:, 0:32], fB, [(xgT[:, kc, :], wr[:, kc, :]) for kc in range(8)], [xgTB, wrB])
                    dve(lambda e: e.scalar_tensor_tensor(out=lg4[:, i, :], in0=f[:, 0:32], scalar=c["rs2"], in1=brt, op0=ALU.mult, op1=ALU.add),
                        [fB, c["rs2B"], rowpB], [lg4B])

                if gi + 1 < NG:
                    s1_load(gi + 1, 0)
                for j in range(6):
                    if j < 4:
                        s5_op(j)
                    if 2 <= j:
                        s5_lg(j - 2)
                    if 1 <= j <= 4:
                        s5_tr(j - 1)
                    if gi + 1 < NG:
                        if 1 <= j <= 4:
                            s1b(gi + 1, j - 1)
                        if j < 4:
                            s1a(gi + 1, j)
                        if j + 1 < 4:
                            s1_load(gi + 1, j + 1)
                pending_route.append(t0)

                if gi == 0:
                    check_stop("g0")
            while pending_route:
                t0r = pending_route.pop(0)
                route_a(t0r)
                route_b(t0r)
            S.barrier()
            dump("cnt", cnt[:], cntB, 32)
            check_stop("A")

        with ExitStack() as pb:
            def walloc(name):
                t_, _b = alloc(pb, [128, 8, D], BF16, name)
                return t_, [Buf(name + "h0"), Buf(name + "h1")]
            wgS = [walloc("wg") for _ in range(2)]
            wuS = [walloc("wu") for _ in range(2)]
            wdS = [walloc("wd") for _ in range(2)]
            bd16, _ = alloc(pb, [1, 2, D], BF16, "bd16")
            bdB = [Buf("bd0"), Buf("bd1")]
            xsS = [alloc(pb, [128, NB, D], BF16, "xsb") for _ in range(2)]
            xTS = [alloc(pb, [128, 8, CAP], BF16, "xT") for _ in range(2)]
            actT, _ = alloc(pb, [128, 8, CAP], BF16, "actT")
            actTB = [Buf("actT0"), Buf("actT1")]
            acR = Ring([alloc(pb, [128, 384], F32, "ac") for _ in range(3)])
            sgR = Ring([alloc(pb, [128, 384], F32, "sg") for _ in range(3)])
            ttR = Ring([alloc(pb, [128, 384], F32, "tt") for _ in range(3)])
            b1R = Ring([alloc(pb, [128, 384], F32, "b1") for _ in range(3)])
            yR = Ring([alloc(pb, [128, D], F32, "yb") for _ in range(2)])
            tpR = Ring([palloc(pb, [128, 1024], BF16, "tpb") for _ in range(2)])
            gR = Ring([palloc(pb, [128, 512], F32, "gb") for _ in range(4)])
            dR = Ring([palloc(pb, [128, 512], F32, "db") for _ in range(2)])
            gffnT = cvec[:, 8:16]

            def load(e):
                b = e % 2
                for (ws, src) in ((wgS, wg_d), (wuS, wu_d), (wdS, wd_d)):
                    w, wB = ws[b]
                    for hh in range(2):
                        S.dma("pool", lambda eng: eng.dma_start(out=w[:, hh * 4:(hh + 1) * 4, :],
                                                                in_=src[e, hh * 512:(hh + 1) * 512, :].rearrange("(c p) o -> p c o", p=128)), (), [wB[hh]])
                S.dma("pool", lambda eng: eng.dma_start(out=bd16[0:1, b, :], in_=bd_d[e:e + 1, :]), (), [bdB[b]])
                xb, xbB = xsS[b]
                S.dma("sp", lambda eng: eng.dma_start(out=xb[:], in_=xs_d[e * CAP:(e + 1) * CAP, :].rearrange("(n p) c -> p n c", p=128)), (), [xbB])

            def transposes(e):
                b = e % 2
                xb, xbB = xsS[b]
                xT, xTB = xTS[b]
                for blk in range(NB):
                    tp, tpB = tpR.next()
                    for kc in range(8):
                        pe(lambda eng: eng.transpose(out=tp[:, kc * 128:(kc + 1) * 128], in_=xb[:, blk, kc * 128:(kc + 1) * 128], identity=ident16[:]),
                           [xbB, ident16B], [tpB], inc=(kc == 7))
                    dve(lambda eng: eng.tensor_tensor(out=xT[:, :, blk * 128:(blk + 1) * 128], in0=tp[:, :].rearrange("p (c t) -> p c t", c=8),
                                                      in1=gffnT.unsqueeze(2).to_broadcast([128, 8, 128]), op=ALU.mult), [tpB, cvecB], [xTB])

            HALVES = ((0, 384), (384, 256))
            pend = []

            def flush_fin():
                while pend:
                    pend.pop(0)()

            def gate_up(e, hf):
                b = e % 2
                wg, wgB = wgS[b]
                wu, wuB = wuS[b]
                xT, xTB = xTS[b]
                s0, sn = HALVES[hf]
                for fc in range(8):
                    gA, gAB = gR.next()
                    gB_, gBB = gR.next()
                    mmgroup(gA[:, 0:sn], gAB, [(wg[:, kc, fc * 128:(fc + 1) * 128], xT[:, kc, s0:s0 + sn]) for kc in range(8)], wgB + [xTB])
                    mmgroup(gB_[:, 0:sn], gBB, [(wu[:, kc, fc * 128:(fc + 1) * 128], xT[:, kc, s0:s0 + sn]) for kc in range(8)], wuB + [xTB])
                    ac, acB = acR.next()
                    sg, sgB = sgR.next()
                    tt, ttB = ttR.next()
                    b1, b1B = b1R.next()
                    col = e * 8 + fc
                    dve(lambda eng: eng.tensor_scalar(out=ac[:, 0:sn], in0=gA[:, 0:sn], scalar1=bgT[:, col:col + 1], scalar2=7.0, op0=ALU.add, op1=ALU.min),
                        [gAB, bgTB], [acB])
                    act(lambda eng: eng.activation(out=sg[:, 0:sn], in_=ac[:, 0:sn], func=AF.Sigmoid, scale=1.702), [acB], [sgB])
                    pool(lambda eng: eng.tensor_tensor(out=tt[:, 0:sn], in0=ac[:, 0:sn], in1=sg[:, 0:sn], op=ALU.mult), [acB, sgB], [ttB])
                    dve(lambda eng: eng.tensor_scalar(out=b1[:, 0:sn], in0=gB_[:, 0:sn], scalar1=bu1T[:, col:col + 1], scalar2=8.0, op0=ALU.add, op1=ALU.min),
                        [gBB, bu1TB], [b1B])
                    flush_fin()

                    def fin(fc=fc, s0=s0, sn=sn, b1=b1, b1B=b1B, tt=tt, ttB=ttB, hf=hf):
                        dve(lambda eng: eng.scalar_tensor_tensor(out=actT[:, fc, s0:s0 + sn], in0=b1[:, 0:sn], scalar=-6.0, in1=tt[:, 0:sn], op0=ALU.max, op1=ALU.mult),
                            [b1B, ttB], [actTB[hf]])
                    pend.append(fin)

            def down(e, blks):
                b = e % 2
                wd, wdB = wdS[b]
                for blk in blks:
                    yb, ybB = yR.next()
                    for ohf in range(2):
                        dp, dpB = dR.next()
                        pairs = [(actT[:, fc, blk * 128:(blk + 1) * 128], wd[:, fc, ohf * 512:(ohf + 1) * 512]) for fc in range(8)]
                        pairs.append((ones16[0:1, 0:128], bd16[0:1, b, ohf * 512:(ohf + 1) * 512]))
                        mmgroup(dp[:, :], dpB, pairs, [actTB[0 if blk < 3 else 1], bdB[b], ones16B] + wdB)
                        act(lambda eng: eng.copy(out=yb[:, ohf * 512:(ohf + 1) * 512], in_=dp[:, :]), [dpB], [ybB])
                    r0 = e * CAP + blk * 128
                    S.dma("sp", lambda eng: eng.dma_start(out=ys_d[r0:r0 + 128, :], in_=yb[:]), [ybB], ())

            load(0)
            transposes(0)
            for e in range(NE):
                if e + 1 < NE:
                    load(e + 1)
                gate_up(e, 0)
                gate_up(e, 1)
                flush_fin()
                down(e, (0, 1, 2))
                if e + 1 < NE:
                    transposes(e + 1)
                down(e, (3, 4))
            S.barrier()
            check_stop("B")

        with ExitStack() as pc:
            wpg, wpgB = alloc(pc, [128, 8, D], BF16, "wpg")
            wpp, wppB = alloc(pc, [128, 2, D], BF16, "wpp")
            gfin, gfinB = alloc(pc, [128, D], F32, "gfin")
            gpleT = cvec[:, 16:24]
            with ExitStack() as stc:
                wstg, wstgB = alloc(stc, [128, 8, D], F32, "wstg")
                S.dma("sp", lambda e: e.dma_start(out=wstg[:], in_=wpg_d.rearrange("(c p) o -> p c o", p=128)), (), [wstgB])
                for kc in range(8):
                    dve(lambda e: e.tensor_scalar_mul(out=wpg[:, kc, :], in0=wstg[:, kc, :], scalar1=gpleT[:, kc:kc + 1]), [wstgB, cvecB], [wpgB])
                S.barrier()
            S.dma("pool", lambda e: e.dma_start(out=wpp[:], in_=wpp_d.rearrange("(c p) o -> p c o", p=128)), (), [wppB])
            S.dma("sp", lambda e: e.dma_start(out=gfin[:], in_=rowp_d[:, 548:548 + D]), (), [gfinB])
            xcR = Ring([alloc(pc, [128, D], F32, "xc") for _ in range(6)])
            ygR = Ring([alloc(pc, [128, D], F32, "yg") for _ in range(16)])
            ptR_ = Ring([alloc(pc, [128, 256], F32, "pt32") for _ in range(4)])
            p16R = Ring([alloc(pc, [128, 256], BF16, "p16") for _ in range(2)])
            hpR = Ring([alloc(pc, [128, D], BF16, "hp") for _ in range(3)])
            hpTR = Ring([alloc(pc, [128, 8, 128], BF16, "hpT") for _ in range(3)])
            pTR = Ring([alloc(pc, [128, 2, 128], BF16, "pT") for _ in range(3)])
            sgR = Ring([alloc(pc, [128, D], F32, "sgc") for _ in range(3)])
            junkR = Ring([alloc(pc, [128, D], BF16, "junk") for _ in range(2)])
            tpR = Ring([palloc(pc, [128, 1024], BF16, "tpc") for _ in range(2)])
            fR = Ring([palloc(pc, [128, 512], F32, "fc") for _ in range(6)])
            cs = {}

            def stL(t):
                xc, xcB = xcR.next()
                S.dma("sp", lambda e: e.dma_start(out=xc[:], in_=x1_d[t * 128:(t + 1) * 128, :]), (), [xcB])
                p32, p32B = ptR_.next()
                S.dma("sp", lambda e: e.dma_start(out=p32[:], in_=p_d[t * 128:(t + 1) * 128, :]), (), [p32B])
                ygs = []
                for k in range(4):
                    yg, ygB = ygR.next()
                    S.dma("pool", lambda e: e.indirect_dma_start(
                        out=yg[:, :], out_offset=None, in_=ys_d,
                        in_offset=bass.IndirectOffsetOnAxis(ap=slots_all[:, t, k:k + 1], axis=0)), [slotB[t]], [ygB])
                    ygs.append((yg, ygB))
                cs[t] = dict(xc=xc, xcB=xcB, p32=p32, p32B=p32B, ygs=ygs)

            def stB(t):
                c = cs[t]
                xc, xcB = c["xc"], c["xcB"]
                for k in range(4):
                    yg, ygB = c["ygs"][k]
                    dve(lambda e: e.scalar_tensor_tensor(out=xc[:], in0=yg[:], scalar=gates_all[:, t, k:k + 1], in1=xc[:], op0=ALU.mult, op1=ALU.add),
                        [ygB, gateB[t], xcB], [xcB])
                hp, hpB = hpR.next()
                act(lambda e: e.copy(out=hp[:], in_=xc[:]), [xcB], [hpB])
                p16, p16B = p16R.next()
                act(lambda e: e.copy(out=p16[:], in_=c["p32"][:]), [c["p32B"]], [p16B])
                junk, junkB = junkR.next()
                sd3, sd3B = rms_sd(xc[:], xcB, junk[:], junkB)
                c.update(hp=hp, hpB=hpB, p16=p16, p16B=p16B, sd3=sd3, sd3B=sd3B)

            def stC(t):
                c = cs[t]
                hp, hpB, p16, p16B = c["hp"], c["hpB"], c["p16"], c["p16B"]
                c["rs3"], c["rs3B"] = rms_recip(c["sd3"], c["sd3B"])
                tp, tpB = tpR.next()
                for kc in range(8):
                    pe(lambda e: e.transpose(out=tp[:, kc * 128:(kc + 1) * 128], in_=hp[:, kc * 128:(kc + 1) * 128], identity=ident16[:]),
                       [hpB, ident16B], [tpB], inc=(kc == 7))
                hpT, hpTB = hpTR.next()
                act(lambda e: e.copy(out=hpT[:], in_=tp[:, :].rearrange("p (c t) -> p c t", c=8)), [tpB], [hpTB])
                tp2, tp2B = tpR.next()
                for cc in range(2):
                    pe(lambda e: e.transpose(out=tp2[:, cc * 128:(cc + 1) * 128], in_=p16[:, cc * 128:(cc + 1) * 128], identity=ident16[:]),
                       [p16B, ident16B], [tp2B], inc=(cc == 1))
                pT, pTB = pTR.next()
                act(lambda e: e.copy(out=pT[:], in_=tp2[:, 0:256].rearrange("p (c t) -> p c t", c=2)), [tp2B], [pTB])
                c.update(hpT=hpT, hpTB=hpTB, pT=pT, pTB=pTB)

            def stD(t):
                c = cs[t]
                xc, xcB = c["xc"], c["xcB"]
                hpT, hpTB, pT, pTB = c["hpT"], c["hpTB"], c["pT"], c["pTB"]
                sg, sgB = sgR.next()
                for half in range(2):
                    fg, fgB = fR.next()
                    mmgroup(fg[:, :], fgB, [(hpT[:, kc, :], wpg[:, kc, half * 512:(half + 1) * 512]) for kc in range(8)], [hpTB, wpgB])
                    fp, fpB = fR.next()
                    mmgroup(fp[:, :], fpB, [(pT[:, cc, :], wpp[:, cc, half * 512:(half + 1) * 512]) for cc in range(2)], [pTB, wppB])
                    act(lambda e: e.activation(out=sg[:, half * 512:(half + 1) * 512], in_=fg[:, :], func=AF.Sigmoid, scale=c["rs3"]), [fgB, c["rs3B"]], [sgB])
                    c["fp%d" % half] = (fp, fpB)
                c.update(sg=sg, sgB=sgB)

            def stD2(t):
                c = cs[t]
                xc, xcB, sg, sgB = c["xc"], c["xcB"], c["sg"], c["sgB"]
                for half in range(2):
                    fp, fpB = c["fp%d" % half]
                    dve(lambda e: e.tensor_tensor(out=sg[:, half * 512:(half + 1) * 512], in0=fp[:, :], in1=sg[:, half * 512:(half + 1) * 512], op=ALU.mult),
                        [fpB, sgB], [sgB])
                dve(lambda e: e.tensor_tensor(out=xc[:], in0=xc[:], in1=sg[:], op=ALU.add), [xcB, sgB], [xcB])
                junk, junkB = junkR.next()
                sd4, sd4B = rms_sd(xc[:], xcB, junk[:], junkB)
                c.update(sg=sg, sgB=sgB, sd4=sd4, sd4B=sd4B)

            def stE(t):
                c = cs[t]
                xc, xcB, sg, sgB = c["xc"], c["xcB"], c["sg"], c["sgB"]
                c["rs4"], c["rs4B"] = rms_recip(c["sd4"], c["sd4B"])
                dve(lambda e: e.scalar_tensor_tensor(out=sg[:], in0=xc[:], scalar=c["rs4"], in1=gfin[:], op0=ALU.mult, op1=ALU.mult), [xcB, c["rs4B"], gfinB], [sgB])
                S.dma("sp", lambda e: e.dma_start(out=out_d[t * 128:(t + 1) * 128, :], in_=sg[:]), [sgB], (), track=out_tokens)
                del cs[t]

            for i in range(-3, NT + 1):
                for fn, tt_ in ((stL, i + 3), (stC, i + 1), (stD, i), (stB, i + 2), (stD2, i), (stE, i - 1)):
                    if 0 <= tt_ < NT:
                        fn(tt_)
            S._wait("sp", out_tokens)
            S.barrier()


def _consts():
    ident = np.eye(128, dtype=np.float32)
    ar = np.arange(128)
    triU = (ar[:, None] < ar[None, :]).astype(np.float32)
    maskI = (ar[:, None] <= ar[None, :]).astype(np.float32)
    iota = np.broadcast_to(np.arange(32, dtype=np.float32), (128, 32))
    kaug = np.zeros((128, 2, 128), np.float32)
    kaug[0, 0, :] = ar - 128.0
    kaug[0, 1, :] = ar
    kaug[1, :, :] = 1.0
    qaug = np.zeros((128, 2, 4, 128), np.float32)
    for k in range(2):
        for g in range(4):
            slope = 2.0 ** (-8.0 * (k * 4 + g + 1) / 8.0)
            qaug[0, k, g, :] = 8.0 * slope
            qaug[1, k, g, :] = -8.0 * slope * ar
    mask01 = np.zeros((128, 2, 128), np.float32)
    mask01[:, 0, :] = (ar[:, None] > ar[None, :])
    mask01[:, 1, :] = (ar[:, None] <= ar[None, :])
    return np.ascontiguousarray(np.concatenate([ident, triU, maskI, iota, kaug.reshape(128, 256), qaug.reshape(128, 1024),
                                                mask01.reshape(128, 256)], axis=1))


_NC_CACHE = {}


def _prep(x, p, g_mix, w_in, attn_sinks, g_sgu, b_sgu, w_spatial, b_spatial, w_attn_proj, w_sgu_proj,
          w_out, g_ffn, w_router, b_router, w_gate, b_gate, w_up, b_up, w_down, b_down,
          g_ple, w_ple_gate, w_ple_proj, g_final):
    f = lambda a: np.ascontiguousarray(np.asarray(a, dtype=np.float32))
    x = f(x).reshape(NCORES, T, D)
    p = f(p)[0].reshape(NCORES, T, 256)
    w_in0 = f(w_in)[0]
    qcols = np.concatenate([np.arange((k * 4 + g) * 64, (k * 4 + g + 1) * 64) for g in range(4) for k in range(2)])
    w_in_p = np.ascontiguousarray(np.concatenate([w_in0[:, qcols], w_in0[:, 512:]], axis=1))
    colT = lambda v: f(v).reshape(-1, 128).T
    cvec = np.ascontiguousarray(np.concatenate([colT(g_mix[0]), colT(g_ffn[0]), colT(g_ple[0]), colT(g_sgu[0]), colT(b_sgu[0])], axis=1))
    bgT = np.ascontiguousarray(f(b_gate)[0].reshape(NE, 8, 128).transpose(2, 0, 1).reshape(128, NE * 8))
    buT = np.ascontiguousarray(f(b_up)[0].reshape(NE, 8, 128).transpose(2, 0, 1).reshape(128, NE * 8))
    sinks = f(attn_sinks)[0].reshape(2, 4)
    sink_rows = np.repeat(sinks, 64, axis=0)
    bc = lambda v: np.broadcast_to(f(v).reshape(1, -1), (128, f(v).size))
    rowp = np.ascontiguousarray(np.concatenate([bc(b_router[0]), sink_rows, bc(b_spatial[0]), bc(g_final)], axis=1))
    shared = {
        "w_in": w_in_p, "w_attn_proj": f(w_attn_proj)[0], "w_sgu_proj": f(w_sgu_proj)[0], "w_out": f(w_out)[0],
        "w_router": f(w_router)[0], "w_gate": f(w_gate)[0], "w_up": f(w_up)[0], "w_down": f(w_down)[0],
        "b_down": f(b_down)[0], "w_ple_gate": f(w_ple_gate)[0], "w_ple_proj": f(w_ple_proj)[0],
        "w_spatial": f(w_spatial)[0], "cvec": cvec, "bgT": bgT, "buT": buT, "rowp": rowp, "cst": _consts(),
    }
    return shared, x, p


def kernel(**inputs):
    shared, x, p = _prep(**inputs)
    if "nc" not in _NC_CACHE:
        _NC_CACHE["nc"] = build_nc()
    nc = _NC_CACHE["nc"]
    in_maps = []
    for c in range(NCORES):
        m = dict(shared)
        m["x"] = x[c]
        m["p"] = p[c]
        in_maps.append(m)
    res = run_bass_kernel_spmd(nc, in_maps, core_ids=list(range(NCORES)))
    out = np.stack([np.asarray(r["out"], dtype=np.float32) for r in res.results], axis=0)
    return out.reshape(16, 2048, D)
```

```python
from contextlib import ExitStack
import numpy as np
import concourse.bass as bass
import concourse.mybir as mybir
from concourse.bass_utils import run_bass_kernel_spmd

F32 = mybir.dt.float32
BF16 = mybir.dt.bfloat16
I32 = mybir.dt.int32
U32 = mybir.dt.uint32
AF = mybir.ActivationFunctionType
ALU = mybir.AluOpType

NCORES = 8
D = 1024
T = 4096
NT = 32
TPS = 16
NG = 8
NE = 32
CAP = 640
NB = CAP // 128
NSLOT = NE * CAP
BIGF = 1.0e6
QO, KO, VO, GUO, GVO, GAO, GBO = 0, 512, 640, 768, 1280, 1792, 2816
INW = 3840
RMS_EPS = 1e-6
LN_EPS = 1e-5
CSTW = 128 * 3 + 32 + 256 + 1024 + 256


class Buf:
    __slots__ = ("name", "w", "r")

    def __init__(self, name=""):
        self.name = name
        self.w = None
        self.r = []


class Sched:
    def __init__(self, nc, n_dma_sems=32):
        self.nc = nc
        self.engs = {"pe": nc.tensor, "act": nc.scalar, "dve": nc.vector, "pool": nc.gpsimd, "sp": nc.sync}
        self.sems = {}
        self.cnt = {}
        self._ctx = []
        for k in list(self.engs) + ["d%d" % i for i in range(n_dma_sems)]:
            cm = nc.semaphore("s_" + k)
            self.sems[k] = cm.__enter__()
            self._ctx.append(cm)
            self.cnt[k] = 0
        half = n_dma_sems // 2
        self.dma_keys = {"sp": ["d%d" % i for i in range(half)], "pool": ["d%d" % i for i in range(half, n_dma_sems)]}
        self.dma_rr = {"sp": 0, "pool": 0}
        self.seen = {e: {} for e in self.engs}
        self.pe_pending = False

    def close(self):
        for cm in reversed(self._ctx):
            cm.__exit__(None, None, None)

    def _wait(self, e, deps):
        need = {}
        for d in deps:
            if d is None:
                continue
            k, v = d
            if k == "pe" and e == "pe":
                continue
            if v > need.get(k, 0):
                need[k] = v
        for k, v in need.items():
            if self.seen[e].get(k, 0) >= v:
                continue
            assert v <= self.cnt[k], (e, k, v, self.cnt[k])
            self.engs[e].wait_ge(self.sems[k], v)
            self.seen[e][k] = v

    @staticmethod
    def _deps(reads, writes):
        deps = []
        for b in reads:
            deps.append(b.w)
        for b in writes:
            deps.append(b.w)
            deps.extend(b.r)
        return deps

    def _record(self, tok, reads, writes):
        for b in reads:
            b.r.append(tok)
            if len(b.r) > 64:
                best = {}
                for k, v in b.r:
                    if v > best.get(k, 0):
                        best[k] = v
                b.r = list(best.items())
        for b in writes:
            b.w = tok
            b.r = []

    def op(self, e, fn, reads=(), writes=(), inc=True):
        self._wait(e, self._deps(reads, writes))
        ins = fn(self.engs[e])
        if inc:
            self.cnt[e] += 1
            ins.then_inc(self.sems[e], 1)
            tok = (e, self.cnt[e])
            if e == "pe":
                self.pe_pending = False
        else:
            assert e == "pe"
            tok = (e, self.cnt[e] + 1)
            self.pe_pending = True
        self._record(tok, reads, writes)
        return tok

    def dma(self, q, fn, reads=(), writes=(), track=None):
        keys = self.dma_keys[q]
        k = keys[self.dma_rr[q] % len(keys)]
        self.dma_rr[q] += 1
        deps = self._deps(reads, writes)
        if self.cnt[k] > 0:
            deps.append((k, self.cnt[k]))
        self._wait(q, deps)
        ins = fn(self.engs[q])
        self.cnt[k] += 16
        ins.then_inc(self.sems[k], 16)
        tok = (k, self.cnt[k])
        self._record(tok, reads, writes)
        if track is not None:
            track.append(tok)
        return tok

    def barrier(self):
        assert not self.pe_pending
        for e in self.engs:
            self._wait(e, [(k, v) for k, v in self.cnt.items() if v > 0])


class _Stop(Exception):
    pass


def build_nc(stop=None, dumps=()):
    nc = bass.Bass("TRN2", target_bir_lowering=False)

    def din(name, shape, dt=F32):
        return nc.dram_tensor(name, list(shape), dt, kind="ExternalInput").ap()

    x_d = din("x", [T, D])
    p_d = din("p", [T, 256])
    win_d = din("w_in", [D, INW])
    wa_d = din("w_attn_proj", [512, D])
    wb_d = din("w_sgu_proj", [512, D])
    wout_d = din("w_out", [D, D])
    wr_d = din("w_router", [D, NE])
    wg_d = din("w_gate", [NE, D, D])
    wu_d = din("w_up", [NE, D, D])
    wd_d = din("w_down", [NE, D, D])
    bd_d = din("b_down", [NE, D])
    wpg_d = din("w_ple_gate", [D, D])
    wpp_d = din("w_ple_proj", [256, D])
    ws_d = din("w_spatial", [4, 128, 128])
    cvec_d = din("cvec", [128, 32])
    bgT_d = din("bgT", [128, NE * 8])
    buT_d = din("buT", [128, NE * 8])
    rowp_d = din("rowp", [128, 32 + 4 + 512 + 1024])
    cst_d = din("cst", [128, CSTW])
    out_d = nc.dram_tensor("out", [T, D], F32, kind="ExternalOutput").ap()
    x1_d = nc.dram_tensor("x1_scr", [T, D], F32, kind="Internal").ap()
    xs_d = nc.dram_tensor("xs_scr", [NSLOT + 128, D], BF16, kind="Internal").ap()
    ys_d = nc.dram_tensor("ys_scr", [NSLOT + 128, D], F32, kind="Internal").ap()

    S = Sched(nc)
    uid = [0]

    def alloc(es, shape, dt, name="t"):
        uid[0] += 1
        t = es.enter_context(nc.sbuf_tensor("%s_%d" % (name, uid[0]), list(shape), dt))
        return t, Buf(name)

    def palloc(es, shape, dt, name="p"):
        uid[0] += 1
        t = es.enter_context(nc.psum_tensor("%s_%d" % (name, uid[0]), list(shape), dt))
        return t, Buf(name)

    class Ring:
        def __init__(self, items):
            self.items = items
            self.i = 0

        def next(self):
            it = self.items[self.i % len(self.items)]
            self.i += 1
            return it

    def pe(fn, r=(), w=(), inc=True):
        return S.op("pe", fn, r, w, inc=inc)

    def act(fn, r=(), w=()):
        return S.op("act", fn, r, w)

    def dve(fn, r=(), w=()):
        return S.op("dve", fn, r, w)

    def pool(fn, r=(), w=()):
        return S.op("pool", fn, r, w)

    def mmgroup(out_ap, outB, pairs, rB, first=True, last=True):
        n = len(pairs)
        for i, (l, r) in enumerate(pairs):
            st = first and i == 0
            sp_ = last and i == n - 1
            pe(lambda e: e.matmul(out_ap, lhsT=l, rhs=r, start=st, stop=sp_), rB, [outB], inc=(i == n - 1))

    out_tokens = []
    dump_names = []

    def dump(name, ap, B, cols):
        if name not in dumps:
            return
        dd = nc.dram_tensor("dbg_" + name, [128, cols], F32, kind="ExternalOutput").ap()
        dump_names.append(name)
        with nc.sbuf_tensor("dbgst_" + name, [128, cols], F32) as stg:
            sB = Buf("stg")
            dve(lambda e: e.tensor_copy(out=stg[:], in_=ap), [B], [sB])
            S.dma("sp", lambda e: e.dma_start(out=dd, in_=stg[:]), [sB], ())
            S.barrier()

    def check_stop(tag):
        if stop == tag:
            raise _Stop()

    try:
        _body(locals())
    except _Stop:
        pass
    S.barrier()
    S.close()
    return nc


def _body(L):
    globals_ = L
    nc = L["nc"]; S = L["S"]; alloc = L["alloc"]; palloc = L["palloc"]; Ring = L["Ring"]
    pe = L["pe"]; act = L["act"]; dve = L["dve"]; pool = L["pool"]; mmgroup = L["mmgroup"]
    out_tokens = L["out_tokens"]; dump = L["dump"]; check_stop = L["check_stop"]
    x_d = L["x_d"]; p_d = L["p_d"]; win_d = L["win_d"]; wa_d = L["wa_d"]; wb_d = L["wb_d"]; wout_d = L["wout_d"]
    wr_d = L["wr_d"]; wg_d = L["wg_d"]; wu_d = L["wu_d"]; wd_d = L["wd_d"]; bd_d = L["bd_d"]; wpg_d = L["wpg_d"]
    wpp_d = L["wpp_d"]; ws_d = L["ws_d"]; cvec_d = L["cvec_d"]; bgT_d = L["bgT_d"]; buT_d = L["buT_d"]
    rowp_d = L["rowp_d"]; cst_d = L["cst_d"]; out_d = L["out_d"]; x1_d = L["x1_d"]; xs_d = L["xs_d"]; ys_d = L["ys_d"]

    with ExitStack() as glob:
        ident16, ident16B = alloc(glob, [128, 128], BF16, "ident16")
        ident32, ident32B = alloc(glob, [128, 128], F32, "ident32")
        ones16, ones16B = alloc(glob, [128, 128], BF16, "ones16")
        cvec, cvecB = alloc(glob, [128, 32], F32, "cvec")
        bgT, bgTB = alloc(glob, [128, NE * 8], F32, "bgT")
        bu1T, bu1TB = alloc(glob, [128, NE * 8], F32, "bu1T")
        gates_all, _ = alloc(glob, [128, NT, 4], F32, "gates")
        slots_all, _ = alloc(glob, [128, NT, 4], I32, "slots")
        gateB = [Buf("gate%d" % t) for t in range(NT)]
        slotB = [Buf("slot%d" % t) for t in range(NT)]
        stat, _ = alloc(glob, [128, 128], F32, "stat")
        statR = Ring([(stat[:, i:i + 1], Buf("stat%d" % i)) for i in range(128)])

        S.dma("sp", lambda e: e.dma_start(out=cvec[:], in_=cvec_d), (), [cvecB])
        S.dma("sp", lambda e: e.dma_start(out=bgT[:], in_=bgT_d), (), [bgTB])
        S.dma("sp", lambda e: e.dma_start(out=bu1T[:], in_=buT_d), (), [bu1TB])
        dve(lambda e: e.tensor_scalar_add(out=bu1T[:], in0=bu1T[:], scalar1=1.0), [bu1TB], [bu1TB])
        pool(lambda e: e.memset(ones16[:], 1.0), (), [ones16B])
        pool(lambda e: e.memset(slots_all[:], 0), (), slotB)
        pool(lambda e: e.memset(gates_all[:], 0.0), (), gateB)

        def rms_sd(src_ap, srcB, junk_ap, junkB):
            ss, ssB = statR.next()
            act(lambda e: e.activation(out=junk_ap, in_=src_ap, func=AF.Square, accum_out=ss), [srcB], [junkB, ssB])
            sd, sdB = statR.next()
            act(lambda e: e.activation(out=sd, in_=ss, func=AF.Sqrt, bias=RMS_EPS, scale=1.0 / D), [ssB], [sdB])
            return sd, sdB

        def rms_recip(sd, sdB):
            rs, rsB = statR.next()
            dve(lambda e: e.reciprocal(out=rs, in_=sd), [sdB], [rsB])
            return rs, rsB

        def rms_rstd(src_ap, srcB, junk_ap, junkB):
            sd, sdB = rms_sd(src_ap, srcB, junk_ap, junkB)
            return rms_recip(sd, sdB)

        with ExitStack() as pa:
            win, _ = alloc(pa, [128, 8, INW], BF16, "win")
            winB = [Buf("win%d" % kc) for kc in range(8)]
            wa, _ = alloc(pa, [128, 4, D], BF16, "wa")
            waB = [Buf("wa0"), Buf("wa1")]
            wb, wbB = alloc(pa, [128, 4, D], BF16, "wb")
            wout, woutB = alloc(pa, [128, 8, D], BF16, "wout")
            wr, wrB = alloc(pa, [128, 8, NE], F32, "wr")
            rowp, rowpB = alloc(pa, [128, 32 + 4 + 512], F32, "rowp")
            kaug, kaugB = alloc(pa, [2, 2, 128], BF16, "kaug")
            qaug, qaugB = alloc(pa, [2, 2, 512], BF16, "qaug")
            mask01, mask01B = alloc(pa, [128, 2, 128], BF16, "mask01")
            triU, triUB = alloc(pa, [128, 128], BF16, "triU")
            iota, iotaB = alloc(pa, [128, 32], F32, "iota")
            wsT, wsTB = alloc(pa, [128, 4, 128], BF16, "wsT")
            comb, combB = alloc(pa, [128, 4, 128], F32, "comb")
            cnt, cntB = alloc(pa, [128, 32], F32, "cnt")

            tpR = Ring([palloc(pa, [128, 1024], BF16, "tp") for _ in range(2)])
            fR = Ring([palloc(pa, [128, 512], F32, "f") for _ in range(6)])

            S.dma("sp", lambda e: e.dma_start(out=wr[:], in_=wr_d.rearrange("(c p) o -> p c o", p=128)), (), [wrB])
            S.dma("sp", lambda e: e.dma_start(out=rowp[:], in_=rowp_d[:, 0:32 + 4 + 512]), (), [rowpB])
            brt = rowp[:, 0:32]
            expsink = rowp[:, 32:36]
            bspat = rowp[:, 36:36 + 512].rearrange("p (g t) -> p g t", g=4)

            with ExitStack() as st:
                cst, cstB = alloc(st, [128, CSTW], F32, "cst")
                wsn, wsnB = alloc(st, [128, 4, 128], F32, "wsn")
                S.dma("sp", lambda e: e.dma_start(out=cst[:], in_=cst_d), (), [cstB])
                S.dma("sp", lambda e: e.dma_start(out=wsn[:], in_=ws_d.rearrange("g t s -> t g s")), (), [wsnB])
                pool(lambda e: e.memset(cnt[:], 0.0), (), [cntB])
                zf, zfB = alloc(st, [128, D], F32, "zf")
                pool(lambda e: e.memset(zf[:], 0.0), (), [zfB])
                S.dma("sp", lambda e: e.dma_start(out=ys_d[NSLOT:NSLOT + 128, :], in_=zf[:]), [zfB], ())
                dve(lambda e: e.tensor_copy(out=ident32[:], in_=cst[:, 0:128]), [cstB], [ident32B])
                dve(lambda e: e.tensor_copy(out=ident16[:], in_=cst[:, 0:128]), [cstB], [ident16B])
                dve(lambda e: e.tensor_copy(out=triU[:], in_=cst[:, 128:256]), [cstB], [triUB])
                dve(lambda e: e.tensor_copy(out=iota[:], in_=cst[:, 384:416]), [cstB], [iotaB])
                dve(lambda e: e.tensor_copy(out=kaug[:], in_=cst[0:2, 416:672].rearrange("p (j n) -> p j n", j=2)), [cstB], [kaugB])
                dve(lambda e: e.tensor_copy(out=qaug[:], in_=cst[0:2, 672:1696].rearrange("p (k n) -> p k n", k=2)), [cstB], [qaugB])
                dve(lambda e: e.tensor_copy(out=mask01[:], in_=cst[:, 1696:1952].rearrange("p (j n) -> p j n", j=2)), [cstB], [mask01B])
                act(lambda e: e.activation(out=expsink, in_=expsink, func=AF.Exp), [rowpB], [rowpB])
                for g in range(4):
                    f, fB = fR.next()
                    pe(lambda e: e.transpose(out=f[:, 0:128], in_=wsn[:, g, :], identity=ident32[:]), [wsnB, ident32B], [fB])
                    dve(lambda e: e.tensor_tensor(out=wsT[:, g, :], in0=f[:, 0:128], in1=cst[:, 256:384], op=ALU.mult), [fB, cstB], [wsTB])
                f, fB = fR.next()
                mmgroup(f[:, :], fB, [(ones16[:, :], wsT[:, :, :].rearrange("p g t -> p (g t)"))], [ones16B, wsTB])
                for g in range(4):
                    dve(lambda e: e.scalar_tensor_tensor(out=comb[:, g, :], in0=f[:, g * 128:(g + 1) * 128], scalar=cvec[:, 28 + g:29 + g],
                                                         in1=bspat[:, g, :], op0=ALU.mult, op1=ALU.add), [fB, cvecB, rowpB], [combB])
                S.barrier()
            check_stop("setup")
            for kc in range(8):
                S.dma("pool", lambda e: e.dma_start(out=win[:, kc, :], in_=win_d[kc * 128:(kc + 1) * 128, :]), (), [winB[kc]])
            for k in range(2):
                S.dma("pool", lambda e: e.dma_start(
                    out=wa[k * 64:(k + 1) * 64, :, :],
                    in_=wa_d[k * 256:(k + 1) * 256, :].rearrange("(g hd) o -> hd g o", g=4)), (), [waB[k]])
            S.dma("pool", lambda e: e.dma_start(out=wb[:], in_=wb_d.rearrange("(c p) o -> p c o", p=128)), (), [wbB])
            S.dma("pool", lambda e: e.dma_start(out=wout[:], in_=wout_d.rearrange("(c p) o -> p c o", p=128)), (), [woutB])
            xR = Ring([alloc(pa, [128, D], F32, "x") for _ in range(4)])
            xnR = Ring([alloc(pa, [128, D], BF16, "xn") for _ in range(2)])
            hT, hTB = alloc(pa, [128, 8, 512], BF16, "hT")
            qT, qTB = alloc(pa, [128, 4, 512], BF16, "qT")
            kT, kTB = alloc(pa, [128, 5 * 128], BF16, "kT")
            vS, vSB = alloc(pa, [128, 5, 128], BF16, "vS")
            guT, guTB = alloc(pa, [128, 4, 512], BF16, "guT")
            gvgR = Ring([alloc(pa, [128, 512], F32, "gvg") for _ in range(2)])
            vln, vlnB = alloc(pa, [128, 4, 512], BF16, "vln")
            bnR = Ring([alloc(pa, [128, 8], F32, "bnst") for _ in range(2)])
            ptR = Ring([alloc(pa, [128, 2, 2, 512], BF16, "pt") for _ in range(2)])
            attnT, attnTB = alloc(pa, [128, 4, 512], BF16, "attnT")
            rden, rdenB = alloc(pa, [128, 512], F32, "rden")
            sguT, sguTB = alloc(pa, [128, 4, 512], BF16, "sguT")
            sgtmp, sgtmpB = alloc(pa, [128, 512], F32, "sgtmp")
            sigAR = Ring([alloc(pa, [128, 512], F32, "sigA") for _ in range(1)])
            sigBR = Ring([alloc(pa, [128, 512], F32, "sigB") for _ in range(1)])
            mrgT, mrgTB = alloc(pa, [128, 8, 512], BF16, "mrgT")
            hn4 = [alloc(pa, [128, D], BF16, "hn") for _ in range(4)]
            xgT, xgTB = alloc(pa, [128, 8, 128], F32, "xgT")
            lg4, lg4B = alloc(pa, [128, 4, 32], F32, "lg4")
            mx8, mx8B = alloc(pa, [128, 4, 8], F32, "mx8")
            ix8, ix8B = alloc(pa, [128, 4, 8], U32, "ix8")
            ixf, ixfB = alloc(pa, [128, 4, 4], F32, "ixf")
            ex4, ex4B = alloc(pa, [128, 4, 4], F32, "ex4")
            sm4, sm4B = alloc(pa, [128, 4], F32, "sm4")
            oh, ohB = alloc(pa, [128, 4, 4, 32], F32, "oh")
            msk16, msk16B = alloc(pa, [128, 4, 32], BF16, "msk16")
            posf, posfB = alloc(pa, [128, 4, 32], F32, "posf")
            ohp, ohpB = alloc(pa, [128, 4, 4, 32], F32, "ohp")
            p4, p4B = alloc(pa, [128, 4, 4], F32, "p4")
            slf, slfB = alloc(pa, [128, 4, 4], F32, "slf")
            ovf, ovfB = alloc(pa, [128, 4, 4], F32, "ovf")

            gmixT = cvec[:, 0:8]
            gffnT = cvec[:, 8:16]

            pending_route = []
            s1st = {}

            def route_a(t0):
                tl = list(range(t0, t0 + 4))
                for i in range(4):
                    dve(lambda e: e.max(out=mx8[:, i, :], in_=lg4[:, i, :]), [lg4B], [mx8B])
                    dve(lambda e: e.max_index(out=ix8[:, i, :], in_max=mx8[:, i, :], in_values=lg4[:, i, :]), [lg4B, mx8B], [ix8B])
                dve(lambda e: e.tensor_copy(out=ixf[:], in_=ix8[:, :, 0:4]), [ix8B], [ixfB])
                dve(lambda e: e.tensor_tensor(out=ex4[:], in0=mx8[:, :, 0:4], in1=mx8[:, :, 0:1].to_broadcast([128, 4, 4]), op=ALU.subtract), [mx8B], [ex4B])
                act(lambda e: e.activation(out=ex4[:], in_=ex4[:], func=AF.Exp), [ex4B], [ex4B])
                dve(lambda e: e.reduce_sum(out=sm4[:], in_=ex4[:], axis=mybir.AxisListType.X), [ex4B], [sm4B])
                dve(lambda e: e.reciprocal(out=sm4[:], in_=sm4[:]), [sm4B], [sm4B])
                dve(lambda e: e.tensor_tensor(out=gates_all[:, t0:t0 + 4, :], in0=ex4[:], in1=sm4[:, :].unsqueeze(2).to_broadcast([128, 4, 4]), op=ALU.mult),
                    [ex4B, sm4B], [gateB[t] for t in tl])
                dve(lambda e: e.tensor_tensor(out=oh[:], in0=iota[:, :].unsqueeze(1).unsqueeze(1).to_broadcast([128, 4, 4, 32]),
                                              in1=ixf[:, :, :].unsqueeze(3).to_broadcast([128, 4, 4, 32]), op=ALU.is_equal), [iotaB, ixfB], [ohB])
                with nc.allow_low_precision(reason="0/1 mask sums are exact in bf16"):
                    dve(lambda e: e.tensor_reduce(out=msk16[:], in_=oh[:, :, :, :].rearrange("p i k e -> p i e k"), axis=mybir.AxisListType.X, op=ALU.add),
                        [ohB], [msk16B])

            def route_b(t0):
                tl = list(range(t0, t0 + 4))
                f, fB = fR.next()
                for i in range(4):
                    pairs = [(triU[:, :], msk16[:, i, :])] + [(ones16[:, :], msk16[:, i2, :]) for i2 in range(i)]
                    mmgroup(f[:, i * 32:(i + 1) * 32], fB, pairs, [triUB, ones16B, msk16B])
                mmgroup(f[:, 128:160], fB, [(ones16[:, :], msk16[:, i, :]) for i in range(4)], [ones16B, msk16B])
                dve(lambda e: e.tensor_tensor(out=posf[:], in0=f[:, 0:128].rearrange("p (i e) -> p i e", i=4),
                                              in1=cnt[:, :].unsqueeze(1).to_broadcast([128, 4, 32]), op=ALU.add), [fB, cntB], [posfB])
                dve(lambda e: e.tensor_tensor(out=cnt[:], in0=f[:, 128:160], in1=cnt[:], op=ALU.add), [fB, cntB, posfB], [cntB])
                dve(lambda e: e.tensor_tensor(out=ohp[:], in0=oh[:], in1=posf[:, :, :].unsqueeze(2).to_broadcast([128, 4, 4, 32]), op=ALU.mult), [ohB, posfB], [ohpB])
                dve(lambda e: e.reduce_sum(out=p4[:], in_=ohp[:], axis=mybir.AxisListType.X), [ohpB], [p4B])
                dve(lambda e: e.scalar_tensor_tensor(out=slf[:], in0=ixf[:], scalar=float(CAP), in1=p4[:], op0=ALU.mult, op1=ALU.add), [ixfB, p4B], [slfB])
                dve(lambda e: e.tensor_scalar(out=ovf[:], in0=p4[:], scalar1=float(CAP), scalar2=BIGF, op0=ALU.is_ge, op1=ALU.mult), [p4B], [ovfB])
                dve(lambda e: e.tensor_tensor(out=slf[:], in0=slf[:], in1=ovf[:], op=ALU.max), [slfB, ovfB], [slfB])
                dve(lambda e: e.tensor_scalar(out=slf[:], in0=slf[:], scalar1=float(NSLOT), scalar2=0.0, op0=ALU.min, op1=ALU.max), [slfB], [slfB])
                dve(lambda e: e.tensor_copy(out=slots_all[:, t0:t0 + 4, :], in_=slf[:]), [slfB], [slotB[t] for t in tl])
                if t0 == 0:
                    dump("lg", lg4[:, 0, :], lg4B, 32)
                    dump("gates", gates_all[:, 0, :], gateB[0], 4)
                    dump("slf", slf[:, 0, :], slfB, 4)
                    dump("posf", posf[:, 0, :], posfB, 32)
                for i in range(4):
                    hn, hnB = hn4[i]
                    for k in range(4):
                        S.dma("pool", lambda e: e.indirect_dma_start(
                            out=xs_d, out_offset=bass.IndirectOffsetOnAxis(ap=slots_all[:, t0 + i, k:k + 1], axis=0),
                            in_=hn[:, :], in_offset=None), [hnB, slotB[t0 + i]], ())

            for gi in range(NG):
                t0 = gi * 4
                def s1_load(tg, i):
                    t = tg * 4 + i
                    xt, xtB = xR.next()
                    S.dma("sp", lambda e: e.dma_start(out=xt[:], in_=x_d[t * 128:(t + 1) * 128, :]), (), [xtB])
                    s1st[(tg, i)] = dict(xt=xt, xtB=xtB)

                def s1a(tg, i):
                    c = s1st[(tg, i)]
                    xt, xtB = c["xt"], c["xtB"]
                    xn, xnB = xnR.next()
                    rs, rsB = rms_rstd(xt[:], xtB, xn[:], xnB)
                    dve(lambda e: e.tensor_scalar_mul(out=xn[:], in0=xt[:], scalar1=rs), [xtB, rsB], [xnB])
                    c.update(xn=xn, xnB=xnB)

                def s1b(tg, i):
                    c = s1st.pop((tg, i))
                    xn, xnB = c["xn"], c["xnB"]
                    tp, tpB = tpR.next()
                    for kc in range(8):
                        pe(lambda e: e.transpose(out=tp[:, kc * 128:(kc + 1) * 128], in_=xn[:, kc * 128:(kc + 1) * 128], identity=ident16[:]),
                           [xnB, ident16B], [tpB], inc=(kc == 7))
                    dve(lambda e: e.tensor_tensor(out=hT[:, :, i * 128:(i + 1) * 128], in0=tp[:, :].rearrange("p (c t) -> p c t", c=8),
                                                  in1=gmixT.unsqueeze(2).to_broadcast([128, 8, 128]), op=ALU.mult), [tpB, cvecB], [hTB])

                if gi == 0:
                    for i in range(4):
                        s1_load(0, i)
                        s1a(0, i)
                        s1b(0, i)
                if gi == 0:
                    for kc in range(8):
                        dump("hT%d" % kc, hT[:, kc, :], hTB, 512)
                    check_stop("g0s1")

                def inproj_fm(col0, evac):
                    f, fB = fR.next()
                    mmgroup(f[:, :], fB, [(win[:, kc, col0:col0 + 128], hT[:, kc, :]) for kc in range(8)], [hTB] + winB)
                    evac(f, fB)

                for t0r in pending_route:
                    route_a(t0r)
                gst = {}

                def gv_mm(i):
                    f, fB = fR.next()
                    mmgroup(f[:, :], fB, [(hT[:, kc, i * 128:(i + 1) * 128], win[:, kc, GVO:GVO + 512]) for kc in range(8)], [hTB] + winB)
                    gvg, gvgB = gvgR.next()
                    act(lambda e: e.activation(out=gvg[:], in_=f[:, :], func=AF.Gelu_apprx_tanh), [fB], [gvgB])
                    gst[i] = (gvg, gvgB)

                def gv_ln(i):
                    gvg, gvgB = gst[i]
                    bn, bnB = bnR.next()
                    dve(lambda e: e.bn_stats(out=bn[:, 0:6], in_=gvg[:]), [gvgB], [bnB])
                    dve(lambda e: e.bn_aggr(out=bn[:, 6:8], in_=bn[:, 0:6]), [bnB], [bnB])
                    sd, sdB = statR.next()
                    act(lambda e: e.activation(out=sd, in_=bn[:, 7:8], func=AF.Sqrt, bias=LN_EPS, scale=1.0), [bnB], [sdB])
                    rs, rsB = statR.next()
                    dve(lambda e: e.reciprocal(out=rs, in_=sd), [sdB], [rsB])
                    dve(lambda e: e.tensor_scalar(out=vln[:, i, :], in0=gvg[:], scalar1=bn[:, 6:7], scalar2=rs, op0=ALU.subtract, op1=ALU.mult),
                        [gvgB, bnB, rsB], [vlnB])

                for i in range(5):
                    if i < 4:
                        gv_mm(i)
                    if i >= 1:
                        gv_ln(i - 1)

                for g in range(4):
                    inproj_fm(QO + g * 128, lambda f, fB: act(lambda e: e.copy(out=qT[:, g, :], in_=f[:, :]), [fB], [qTB]))
                inproj_fm(KO, lambda f, fB: act(lambda e: e.copy(out=kT[:, 128:640], in_=f[:, :]), [fB], [kTB]))
                while pending_route:
                    route_b(pending_route.pop(0))
                for c in range(4):
                    inproj_fm(GUO + c * 128, lambda f, fB: act(lambda e: e.activation(out=guT[:, c, :], in_=f[:, :], func=AF.Gelu_apprx_tanh), [fB], [guTB]))
                f, fB = fR.next()
                for i in range(4):
                    mmgroup(f[:, i * 128:(i + 1) * 128], fB, [(hT[:, kc, i * 128:(i + 1) * 128], win[:, kc, VO:VO + 128]) for kc in range(8)], [hTB] + winB)
                act(lambda e: e.copy(out=vS[:, 1:5, :], in_=f[:, :].rearrange("p (i c) -> p i c", i=4)), [fB], [vSB])

                if gi == 0:
                    for g in range(4):
                        dump("qT%d" % g, qT[:, g, :], qTB, 512)
                        dump("guT%d" % g, guT[:, g, :], guTB, 512)
                        dump("vln%d" % g, vln[:, g, :], vlnB, 512)
                    dump("kT", kT[:, :], kTB, 640)
                    dump("vS", vS[:, :, :].rearrange("p a b -> p (a b)"), vSB, 640)
                    check_stop("g0s2")
                for cg in range(4):
                    f, fB = fR.next()
                    for i in range(4):
                        mmgroup(f[:, i * 128:(i + 1) * 128], fB, [(vln[:, i, cg * 128:(cg + 1) * 128], wsT[:, cg, :])], [vlnB, wsTB])
                    dve(lambda e: e.scalar_tensor_tensor(out=sgtmp[:, :].rearrange("p (i t) -> p i t", i=4), in0=f[:, :].rearrange("p (i t) -> p i t", i=4),
                                                         scalar=cvec[:, 24 + cg:25 + cg], in1=comb[:, cg, :].unsqueeze(1).to_broadcast([128, 4, 128]),
                                                         op0=ALU.mult, op1=ALU.add), [fB, cvecB, combB], [sgtmpB])
                    dve(lambda e: e.tensor_tensor(out=sguT[:, cg, :], in0=sgtmp[:], in1=guT[:, cg, :], op=ALU.mult), [sgtmpB, guTB], [sguTB])

                ast = {}

                def att_scores(i):
                    t = t0 + i
                    n = t % TPS
                    js = [(0, i), (1, i + 1)] if n > 0 else [(1, i + 1)]
                    pt, ptB = ptR.next()
                    for k in range(2):
                        for (j, slot) in js:
                            f, fB = fR.next()
                            mmgroup(f[:, :], fB, [(kT[k * 64:(k + 1) * 64, slot * 128:(slot + 1) * 128], qT[k * 64:(k + 1) * 64, :, i * 128:(i + 1) * 128]),
                                                  (kaug[0:2, j, :], qaug[0:2, k, :])], [kTB, qTB, kaugB, qaugB])
                            act(lambda e: e.activation(out=pt[:, k, j, :], in_=f[:, :], func=AF.Exp, scale=0.125), [fB], [ptB])
                            dve(lambda e: e.tensor_tensor(out=pt[:, k, j, :].rearrange("p (g q) -> p g q", g=4),
                                                           in0=pt[:, k, j, :].rearrange("p (g q) -> p g q", g=4),
                                                           in1=mask01[:, j, :].unsqueeze(1).to_broadcast([128, 4, 128]), op=ALU.mult), [ptB, mask01B], [ptB])
                    ast[i] = (js, pt, ptB)

                def att_pv(i):
                    js, pt, ptB = ast[i]
                    pv, pvB = fR.next()
                    dn, dnB = fR.next()
                    for k in range(2):
                        mmgroup(pv[k * 64:(k + 1) * 64, :], pvB, [(vS[:, slot, k * 64:(k + 1) * 64], pt[:, k, j, :]) for (j, slot) in js], [vSB, ptB])
                    for k in range(2):
                        mmgroup(dn[k * 64:(k + 1) * 64, :], dnB, [(ones16[:, 0:64], pt[:, k, j, :]) for (j, slot) in js], [ones16B, ptB])
                    dve(lambda e: e.tensor_tensor(out=rden[:, :].rearrange("p (g q) -> p g q", g=4), in0=dn[:, :].rearrange("p (g q) -> p g q", g=4),
                                                  in1=expsink.unsqueeze(2).to_broadcast([128, 4, 128]), op=ALU.add), [dnB, rowpB], [rdenB])
                    act(lambda e: e.activation(out=rden[:], in_=rden[:], func=AF.Ln), [rdenB], [rdenB])
                    act(lambda e: e.activation(out=rden[:], in_=rden[:], func=AF.Exp, scale=-1.0), [rdenB], [rdenB])
                    dve(lambda e: e.tensor_tensor(out=attnT[:, :, i * 128:(i + 1) * 128], in0=pv[:, :].rearrange("p (g q) -> p g q", g=4),
                                                  in1=rden[:, :].rearrange("p (g q) -> p g q", g=4), op=ALU.mult), [pvB, rdenB], [attnTB])

                for i in range(5):
                    if i < 4:
                        att_scores(i)
                    if i >= 1:
                        att_pv(i - 1)
                pool(lambda e: e.tensor_copy(out=kT[:, 0:128], in_=kT[:, 512:640]), [kTB], [kTB])
                pool(lambda e: e.tensor_copy(out=vS[:, 0, :], in_=vS[:, 4, :]), [vSB], [vSB])

                if gi == 0:
                    for g in range(4):
                        dump("attnT%d" % g, attnT[:, g, :], attnTB, 512)
                        dump("sguT%d" % g, sguT[:, g, :], sguTB, 512)
                    check_stop("g0s3")
                for oc in range(8):
                    sigA, sigAB = sigAR.next()
                    sigB_, sigBB = sigBR.next()
                    inproj_fm(GAO + oc * 128, lambda f, fB: act(lambda e: e.activation(out=sigA[:], in_=f[:, :], func=AF.Sigmoid), [fB], [sigAB]))
                    inproj_fm(GBO + oc * 128, lambda f, fB: act(lambda e: e.activation(out=sigB_[:], in_=f[:, :], func=AF.Sigmoid), [fB], [sigBB]))
                    f, fB = fR.next()
                    mmgroup(f[:, :], fB, [(wa[:, g, oc * 128:(oc + 1) * 128], attnT[:, g, :]) for g in range(4)], waB + [attnTB])
                    dve(lambda e: e.tensor_tensor(out=sigA[:], in0=f[:, :], in1=sigA[:], op=ALU.mult), [fB, sigAB], [sigAB])
                    f, fB = fR.next()
                    mmgroup(f[:, :], fB, [(wb[:, c, oc * 128:(oc + 1) * 128], sguT[:, c, :]) for c in range(4)], [wbB, sguTB])
                    dve(lambda e: e.tensor_tensor(out=sigB_[:], in0=f[:, :], in1=sigB_[:], op=ALU.mult), [fB, sigBB], [sigBB])
                    dve(lambda e: e.tensor_tensor(out=mrgT[:, oc, :], in0=sigA[:], in1=sigB_[:], op=ALU.add), [sigAB, sigBB], [mrgTB])

                if gi == 0:
                    for kc in range(8):
                        dump("mrgT%d" % kc, mrgT[:, kc, :], mrgTB, 512)
                    check_stop("g0s4")
                s5 = {}

                def s5_op(i):
                    t = t0 + i
                    xr, xrB = xR.next()
                    S.dma("sp", lambda e: e.dma_start(out=xr[:], in_=x_d[t * 128:(t + 1) * 128, :]), (), [xrB])
                    for half in range(2):
                        f, fB = fR.next()
                        mmgroup(f[:, :], fB, [(mrgT[:, kc, i * 128:(i + 1) * 128], wout[:, kc, half * 512:(half + 1) * 512]) for kc in range(8)], [mrgTB, woutB])
                        dve(lambda e: e.tensor_tensor(out=xr[:, half * 512:(half + 1) * 512], in0=f[:, :], in1=xr[:, half * 512:(half + 1) * 512], op=ALU.add),
                            [fB, xrB], [xrB])
                    S.dma("sp", lambda e: e.dma_start(out=x1_d[t * 128:(t + 1) * 128, :], in_=xr[:]), [xrB], ())
                    hn, hnB = hn4[i]
                    sd2, sd2B = rms_sd(xr[:], xrB, hn[:], hnB)
                    s5[i] = dict(xr=xr, xrB=xrB, hn=hn, hnB=hnB, sd2=sd2, sd2B=sd2B)

                def s5_tr(i):
                    c = s5[i]
                    xr, xrB, hn, hnB = c["xr"], c["xrB"], c["hn"], c["hnB"]
                    rs2, rs2B = rms_recip(c["sd2"], c["sd2B"])
                    c["rs2"], c["rs2B"] = rs2, rs2B
                    act(lambda e: e.activation(out=hn[:], in_=xr[:], func=AF.Copy, scale=rs2), [xrB, rs2B], [hnB])
                    for hh in range(2):
                        f, fB = fR.next()
                        for c4 in range(4):
                            kc = hh * 4 + c4
                            pe(lambda e: e.transpose(out=f[:, c4 * 128:(c4 + 1) * 128], in_=xr[:, kc * 128:(kc + 1) * 128], identity=ident32[:]),
                               [xrB, ident32B], [fB], inc=(c4 == 3))
                        dve(lambda e: e.tensor_tensor(out=xgT[:, hh * 4:(hh + 1) * 4, :], in0=f[:, :].rearrange("p (c t) -> p c t", c=4),
                                                      in1=gffnT[:, hh * 4:(hh + 1) * 4].unsqueeze(2).to_broadcast([128, 4, 128]), op=ALU.mult),
                            [fB, cvecB], [xgTB])

                def s5_lg(i):
                    c = s5[i]
                    f, fB = fR.next()
                    mmgroup(f[:, 0:32], fB, [(xgT[:, kc, :], wr[:, kc, :]) for kc in range(8)], [xgTB, wrB])
                    dve(lambda e: e.scalar_tensor_tensor(out=lg4[:, i, :], in0=f[:, 0:32], scalar=c["rs2"], in1=brt, op0=ALU.mult, op1=ALU.add),
                        [fB, c["rs2B"], rowpB], [lg4B])

                if gi + 1 < NG:
                    s1_load(gi + 1, 0)
                for j in range(6):
                    if j < 4:
                        s5_op(j)
                    if 2 <= j:
                        s5_lg(j - 2)
                    if 1 <= j <= 4:
                        s5_tr(j - 1)
                    if gi + 1 < NG:
                        if 1 <= j <= 4:
                            s1b(gi + 1, j - 1)
                        if j < 4:
                            s1a(gi + 1, j)
                        if j + 1 < 4:
                            s1_load(gi + 1, j + 1)
                pending_route.append(t0)

                if gi == 0:
                    check_stop("g0")
            while pending_route:
                t0r = pending_route.pop(0)
                route_a(t0r)
                route_b(t0r)
            S.barrier()
            dump("cnt", cnt[:], cntB, 32)
            check_stop("A")

        with ExitStack() as pb:
            def walloc(name):
                t_, _b = alloc(pb, [128, 8, D], BF16, name)
                return t_, [Buf(name + "h0"), Buf(name + "h1")]
            wgS = [walloc("wg") for _ in range(2)]
            wuS = [walloc("wu") for _ in range(2)]
            wdS = [walloc("wd") for _ in range(2)]
            bd16, _ = alloc(pb, [1, 2, D], BF16, "bd16")
            bdB = [Buf("bd0"), Buf("bd1")]
            def xalloc():
                t_, _b = alloc(pb, [128, 8, CAP], BF16, "xT")
                return t_, [Buf("xT%d" % kc) for kc in range(8)]
            xTS = [xalloc() for _ in range(2)]
            actT, _ = alloc(pb, [128, 8, CAP], BF16, "actT")
            actTB = [Buf("actT0"), Buf("actT1")]
            acR = Ring([alloc(pb, [128, 384], F32, "ac") for _ in range(3)])
            sgR = Ring([alloc(pb, [128, 384], F32, "sg") for _ in range(3)])
            ttR = Ring([alloc(pb, [128, 384], F32, "tt") for _ in range(3)])
            b1R = Ring([alloc(pb, [128, 384], F32, "b1") for _ in range(3)])
            yR = Ring([alloc(pb, [128, D], F32, "yb") for _ in range(2)])
            gR = Ring([palloc(pb, [128, 512], F32, "gb") for _ in range(6)])
            dR = Ring([palloc(pb, [128, 512], F32, "db") for _ in range(2)])
            gffnT = cvec[:, 8:16]

            def load(e):
                b = e % 2
                for (ws, src) in ((wgS, wg_d), (wuS, wu_d), (wdS, wd_d)):
                    w, wB = ws[b]
                    for hh in range(2):
                        S.dma("pool", lambda eng: eng.dma_start(out=w[:, hh * 4:(hh + 1) * 4, :],
                                                                in_=src[e, hh * 512:(hh + 1) * 512, :].rearrange("(c p) o -> p c o", p=128)), (), [wB[hh]])
                S.dma("pool", lambda eng: eng.dma_start(out=bd16[0:1, b, :], in_=bd_d[e:e + 1, :]), (), [bdB[b]])
                xT, xTB = xTS[b]
                for kc in range(8):
                    S.dma("sp", lambda eng: eng.dma_start_transpose(out=xT[:, kc, :], in_=xs_d[e * CAP:(e + 1) * CAP, kc * 128:(kc + 1) * 128]), (), [xTB[kc]])

            def transposes(e):
                b = e % 2
                xT, xTB = xTS[b]
                dve(lambda eng: eng.tensor_tensor(out=xT[:, :, :], in0=xT[:, :, :], in1=gffnT.unsqueeze(2).to_broadcast([128, 8, CAP]), op=ALU.mult),
                    xTB + [cvecB], xTB)

            HALVES = ((0, 384), (384, 256))
            pend = []

            def flush_fin():
                while pend:
                    pend.pop(0)()

            def gate_up(e, hf):
                b = e % 2
                wg, wgB = wgS[b]
                wu, wuB = wuS[b]
                xT, xTB = xTS[b]
                s0, sn = HALVES[hf]
                for fc in range(8):
                    gA, gAB = gR.next()
                    gB_, gBB = gR.next()
                    mmgroup(gA[:, 0:sn], gAB, [(wg[:, kc, fc * 128:(fc + 1) * 128], xT[:, kc, s0:s0 + sn]) for kc in range(8)], wgB + xTB)
                    mmgroup(gB_[:, 0:sn], gBB, [(wu[:, kc, fc * 128:(fc + 1) * 128], xT[:, kc, s0:s0 + sn]) for kc in range(8)], wuB + xTB)
                    ac, acB = acR.next()
                    sg, sgB = sgR.next()
                    tt, ttB = ttR.next()
                    b1, b1B = b1R.next()
                    col = e * 8 + fc
                    dve(lambda eng: eng.tensor_scalar(out=ac[:, 0:sn], in0=gA[:, 0:sn], scalar1=bgT[:, col:col + 1], scalar2=7.0, op0=ALU.add, op1=ALU.min),
                        [gAB, bgTB], [acB])
                    act(lambda eng: eng.activation(out=sg[:, 0:sn], in_=ac[:, 0:sn], func=AF.Sigmoid, scale=1.702), [acB], [sgB])
                    pool(lambda eng: eng.tensor_tensor(out=tt[:, 0:sn], in0=ac[:, 0:sn], in1=sg[:, 0:sn], op=ALU.mult), [acB, sgB], [ttB])
                    dve(lambda eng: eng.tensor_scalar(out=b1[:, 0:sn], in0=gB_[:, 0:sn], scalar1=bu1T[:, col:col + 1], scalar2=8.0, op0=ALU.add, op1=ALU.min),
                        [gBB, bu1TB], [b1B])
                    flush_fin()

                    def fin(fc=fc, s0=s0, sn=sn, b1=b1, b1B=b1B, tt=tt, ttB=ttB, hf=hf):
                        dve(lambda eng: eng.scalar_tensor_tensor(out=actT[:, fc, s0:s0 + sn], in0=b1[:, 0:sn], scalar=-6.0, in1=tt[:, 0:sn], op0=ALU.max, op1=ALU.mult),
                            [b1B, ttB], [actTB[hf]])
                    pend.append(fin)

            def down(e, blks):
                b = e % 2
                wd, wdB = wdS[b]
                for blk in blks:
                    yb, ybB = yR.next()
                    for ohf in range(2):
                        dp, dpB = dR.next()
                        pairs = [(actT[:, fc, blk * 128:(blk + 1) * 128], wd[:, fc, ohf * 512:(ohf + 1) * 512]) for fc in range(8)]
                        pairs.append((ones16[0:1, 0:128], bd16[0:1, b, ohf * 512:(ohf + 1) * 512]))
                        mmgroup(dp[:, :], dpB, pairs, [actTB[0 if blk < 3 else 1], bdB[b], ones16B] + wdB)
                        act(lambda eng: eng.copy(out=yb[:, ohf * 512:(ohf + 1) * 512], in_=dp[:, :]), [dpB], [ybB])
                    r0 = e * CAP + blk * 128
                    S.dma("sp", lambda eng: eng.dma_start(out=ys_d[r0:r0 + 128, :], in_=yb[:]), [ybB], ())

            load(0)
            transposes(0)
            for e in range(NE):
                if e + 1 < NE:
                    load(e + 1)
                gate_up(e, 0)
                gate_up(e, 1)
                flush_fin()
                down(e, (0, 1, 2))
                if e + 1 < NE:
                    transposes(e + 1)
                down(e, (3, 4))
            S.barrier()
            check_stop("B")

        with ExitStack() as pc:
            wpg, wpgB = alloc(pc, [128, 8, D], BF16, "wpg")
            wpp, wppB = alloc(pc, [128, 2, D], BF16, "wpp")
            gfin, gfinB = alloc(pc, [128, D], F32, "gfin")
            gpleT = cvec[:, 16:24]
            with ExitStack() as stc:
                wstg, wstgB = alloc(stc, [128, 8, D], F32, "wstg")
                S.dma("sp", lambda e: e.dma_start(out=wstg[:], in_=wpg_d.rearrange("(c p) o -> p c o", p=128)), (), [wstgB])
                for kc in range(8):
                    dve(lambda e: e.tensor_scalar_mul(out=wpg[:, kc, :], in0=wstg[:, kc, :], scalar1=gpleT[:, kc:kc + 1]), [wstgB, cvecB], [wpgB])
                S.barrier()
            S.dma("pool", lambda e: e.dma_start(out=wpp[:], in_=wpp_d.rearrange("(c p) o -> p c o", p=128)), (), [wppB])
            S.dma("sp", lambda e: e.dma_start(out=gfin[:], in_=rowp_d[:, 548:548 + D]), (), [gfinB])
            xcR = Ring([alloc(pc, [128, D], F32, "xc") for _ in range(6)])
            ygR = Ring([alloc(pc, [128, D], F32, "yg") for _ in range(16)])
            ptR_ = Ring([alloc(pc, [128, 256], F32, "pt32") for _ in range(4)])
            p16R = Ring([alloc(pc, [128, 256], BF16, "p16") for _ in range(2)])
            hpR = Ring([alloc(pc, [128, D], BF16, "hp") for _ in range(3)])
            hpTR = Ring([alloc(pc, [128, 8, 128], BF16, "hpT") for _ in range(3)])
            pTR = Ring([alloc(pc, [128, 2, 128], BF16, "pT") for _ in range(3)])
            sgR = Ring([alloc(pc, [128, D], F32, "sgc") for _ in range(3)])
            junkR = Ring([alloc(pc, [128, D], BF16, "junk") for _ in range(2)])
            tpR = Ring([palloc(pc, [128, 1024], BF16, "tpc") for _ in range(2)])
            fR = Ring([palloc(pc, [128, 512], F32, "fc") for _ in range(6)])
            cs = {}

            def stL(t):
                xc, xcB = xcR.next()
                S.dma("sp", lambda e: e.dma_start(out=xc[:], in_=x1_d[t * 128:(t + 1) * 128, :]), (), [xcB])
                p32, p32B = ptR_.next()
                S.dma("sp", lambda e: e.dma_start(out=p32[:], in_=p_d[t * 128:(t + 1) * 128, :]), (), [p32B])
                ygs = []
                for k in range(4):
                    yg, ygB = ygR.next()
                    S.dma("pool", lambda e: e.indirect_dma_start(
                        out=yg[:, :], out_offset=None, in_=ys_d,
                        in_offset=bass.IndirectOffsetOnAxis(ap=slots_all[:, t, k:k + 1], axis=0)), [slotB[t]], [ygB])
                    ygs.append((yg, ygB))
                cs[t] = dict(xc=xc, xcB=xcB, p32=p32, p32B=p32B, ygs=ygs)

            def stB(t):
                c = cs[t]
                xc, xcB = c["xc"], c["xcB"]
                for k in range(4):
                    yg, ygB = c["ygs"][k]
                    dve(lambda e: e.scalar_tensor_tensor(out=xc[:], in0=yg[:], scalar=gates_all[:, t, k:k + 1], in1=xc[:], op0=ALU.mult, op1=ALU.add),
                        [ygB, gateB[t], xcB], [xcB])
                hp, hpB = hpR.next()
                act(lambda e: e.copy(out=hp[:], in_=xc[:]), [xcB], [hpB])
                p16, p16B = p16R.next()
                act(lambda e: e.copy(out=p16[:], in_=c["p32"][:]), [c["p32B"]], [p16B])
                junk, junkB = junkR.next()
                sd3, sd3B = rms_sd(xc[:], xcB, junk[:], junkB)
                c.update(hp=hp, hpB=hpB, p16=p16, p16B=p16B, sd3=sd3, sd3B=sd3B)

            def stC(t):
                c = cs[t]
                hp, hpB, p16, p16B = c["hp"], c["hpB"], c["p16"], c["p16B"]
                c["rs3"], c["rs3B"] = rms_recip(c["sd3"], c["sd3B"])
                tp, tpB = tpR.next()
                for kc in range(8):
                    pe(lambda e: e.transpose(out=tp[:, kc * 128:(kc + 1) * 128], in_=hp[:, kc * 128:(kc + 1) * 128], identity=ident16[:]),
                       [hpB, ident16B], [tpB], inc=(kc == 7))
                hpT, hpTB = hpTR.next()
                act(lambda e: e.copy(out=hpT[:], in_=tp[:, :].rearrange("p (c t) -> p c t", c=8)), [tpB], [hpTB])
                tp2, tp2B = tpR.next()
                for cc in range(2):
                    pe(lambda e: e.transpose(out=tp2[:, cc * 128:(cc + 1) * 128], in_=p16[:, cc * 128:(cc + 1) * 128], identity=ident16[:]),
                       [p16B, ident16B], [tp2B], inc=(cc == 1))
                pT, pTB = pTR.next()
                act(lambda e: e.copy(out=pT[:], in_=tp2[:, 0:256].rearrange("p (c t) -> p c t", c=2)), [tp2B], [pTB])
                c.update(hpT=hpT, hpTB=hpTB, pT=pT, pTB=pTB)

            def stD(t):
                c = cs[t]
                xc, xcB = c["xc"], c["xcB"]
                hpT, hpTB, pT, pTB = c["hpT"], c["hpTB"], c["pT"], c["pTB"]
                sg, sgB = sgR.next()
                for half in range(2):
                    fg, fgB = fR.next()
                    mmgroup(fg[:, :], fgB, [(hpT[:, kc, :], wpg[:, kc, half * 512:(half + 1) * 512]) for kc in range(8)], [hpTB, wpgB])
                    fp, fpB = fR.next()
                    mmgroup(fp[:, :], fpB, [(pT[:, cc, :], wpp[:, cc, half * 512:(half + 1) * 512]) for cc in range(2)], [pTB, wppB])
                    act(lambda e: e.activation(out=sg[:, half * 512:(half + 1) * 512], in_=fg[:, :], func=AF.Sigmoid, scale=c["rs3"]), [fgB, c["rs3B"]], [sgB])
                    c["fp%d" % half] = (fp, fpB)
                c.update(sg=sg, sgB=sgB)

            def stD2(t):
                c = cs[t]
                xc, xcB, sg, sgB = c["xc"], c["xcB"], c["sg"], c["sgB"]
                for half in range(2):
                    fp, fpB = c["fp%d" % half]
                    dve(lambda e: e.tensor_tensor(out=sg[:, half * 512:(half + 1) * 512], in0=fp[:, :], in1=sg[:, half * 512:(half + 1) * 512], op=ALU.mult),
                        [fpB, sgB], [sgB])
                dve(lambda e: e.tensor_tensor(out=xc[:], in0=xc[:], in1=sg[:], op=ALU.add), [xcB, sgB], [xcB])
                junk, junkB = junkR.next()
                sd4, sd4B = rms_sd(xc[:], xcB, junk[:], junkB)
                c.update(sg=sg, sgB=sgB, sd4=sd4, sd4B=sd4B)

            def stE(t):
                c = cs[t]
                xc, xcB, sg, sgB = c["xc"], c["xcB"], c["sg"], c["sgB"]
                c["rs4"], c["rs4B"] = rms_recip(c["sd4"], c["sd4B"])
                dve(lambda e: e.scalar_tensor_tensor(out=sg[:], in0=xc[:], scalar=c["rs4"], in1=gfin[:], op0=ALU.mult, op1=ALU.mult), [xcB, c["rs4B"], gfinB], [sgB])
                S.dma("sp", lambda e: e.dma_start(out=out_d[t * 128:(t + 1) * 128, :], in_=sg[:]), [sgB], (), track=out_tokens)
                del cs[t]

            for i in range(-3, NT + 1):
                for fn, tt_ in ((stL, i + 3), (stC, i + 1), (stD, i), (stB, i + 2), (stD2, i), (stE, i - 1)):
                    if 0 <= tt_ < NT:
                        fn(tt_)
            S._wait("sp", out_tokens)
            S.barrier()


def _consts():
    ident = np.eye(128, dtype=np.float32)
    ar = np.arange(128)
    triU = (ar[:, None] < ar[None, :]).astype(np.float32)
    maskI = (ar[:, None] <= ar[None, :]).astype(np.float32)
    iota = np.broadcast_to(np.arange(32, dtype=np.float32), (128, 32))
    kaug = np.zeros((128, 2, 128), np.float32)
    kaug[0, 0, :] = ar - 128.0
    kaug[0, 1, :] = ar
    kaug[1, :, :] = 1.0
    qaug = np.zeros((128, 2, 4, 128), np.float32)
    for k in range(2):
        for g in range(4):
            slope = 2.0 ** (-8.0 * (k * 4 + g + 1) / 8.0)
            qaug[0, k, g, :] = 8.0 * slope
            qaug[1, k, g, :] = -8.0 * slope * ar
    mask01 = np.zeros((128, 2, 128), np.float32)
    mask01[:, 0, :] = (ar[:, None] > ar[None, :])
    mask01[:, 1, :] = (ar[:, None] <= ar[None, :])
    return np.ascontiguousarray(np.concatenate([ident, triU, maskI, iota, kaug.reshape(128, 256), qaug.reshape(128, 1024),
                                                mask01.reshape(128, 256)], axis=1))


_NC_CACHE = {}


def _prep(x, p, g_mix, w_in, attn_sinks, g_sgu, b_sgu, w_spatial, b_spatial, w_attn_proj, w_sgu_proj,
          w_out, g_ffn, w_router, b_router, w_gate, b_gate, w_up, b_up, w_down, b_down,
          g_ple, w_ple_gate, w_ple_proj, g_final):
    f = lambda a: np.ascontiguousarray(np.asarray(a, dtype=np.float32))
    x = f(x).reshape(NCORES, T, D)
    p = f(p)[0].reshape(NCORES, T, 256)
    w_in0 = f(w_in)[0]
    qcols = np.concatenate([np.arange((k * 4 + g) * 64, (k * 4 + g + 1) * 64) for g in range(4) for k in range(2)])
    w_in_p = np.ascontiguousarray(np.concatenate([w_in0[:, qcols], w_in0[:, 512:]], axis=1))
    colT = lambda v: f(v).reshape(-1, 128).T
    cvec = np.ascontiguousarray(np.concatenate([colT(g_mix[0]), colT(g_ffn[0]), colT(g_ple[0]), colT(g_sgu[0]), colT(b_sgu[0])], axis=1))
    bgT = np.ascontiguousarray(f(b_gate)[0].reshape(NE, 8, 128).transpose(2, 0, 1).reshape(128, NE * 8))
    buT = np.ascontiguousarray(f(b_up)[0].reshape(NE, 8, 128).transpose(2, 0, 1).reshape(128, NE * 8))
    sinks = f(attn_sinks)[0].reshape(2, 4)
    sink_rows = np.repeat(sinks, 64, axis=0)
    bc = lambda v: np.broadcast_to(f(v).reshape(1, -1), (128, f(v).size))
    rowp = np.ascontiguousarray(np.concatenate([bc(b_router[0]), sink_rows, bc(b_spatial[0]), bc(g_final)], axis=1))
    shared = {
        "w_in": w_in_p, "w_attn_proj": f(w_attn_proj)[0], "w_sgu_proj": f(w_sgu_proj)[0], "w_out": f(w_out)[0],
        "w_router": f(w_router)[0], "w_gate": f(w_gate)[0], "w_up": f(w_up)[0], "w_down": f(w_down)[0],
        "b_down": f(b_down)[0], "w_ple_gate": f(w_ple_gate)[0], "w_ple_proj": f(w_ple_proj)[0],
        "w_spatial": f(w_spatial)[0], "cvec": cvec, "bgT": bgT, "buT": buT, "rowp": rowp, "cst": _consts(),
    }
    return shared, x, p


def kernel(**inputs):
    shared, x, p = _prep(**inputs)
    if "nc" not in _NC_CACHE:
        _NC_CACHE["nc"] = build_nc()
    nc = _NC_CACHE["nc"]
    in_maps = []
    for c in range(NCORES):
        m = dict(shared)
        m["x"] = x[c]
        m["p"] = p[c]
        in_maps.append(m)
    res = run_bass_kernel_spmd(nc, in_maps, core_ids=list(range(NCORES)))
    out = np.stack([np.asarray(r["out"], dtype=np.float32) for r in res.results], axis=0)
    return out.reshape(16, 2048, D)
```

```python
from contextlib import ExitStack
import numpy as np
import concourse.bass as bass
import concourse.mybir as mybir
from concourse.bass_utils import run_bass_kernel_spmd

F32 = mybir.dt.float32
BF16 = mybir.dt.bfloat16
I32 = mybir.dt.int32
U32 = mybir.dt.uint32
AF = mybir.ActivationFunctionType
ALU = mybir.AluOpType

NCORES = 8
D = 1024
T = 4096
NT = 32
TPS = 16
NG = 8
NE = 32
CAP = 640
NB = CAP // 128
NSLOT = NE * CAP
BIGF = 1.0e6
QO, KO, VO, GUO, GVO, GAO, GBO = 0, 512, 640, 768, 1280, 1792, 2816
INW = 3840
RMS_EPS = 1e-6
LN_EPS = 1e-5
CSTW = 128 * 3 + 32 + 256 + 1024 + 256


class Buf:
    __slots__ = ("name", "w", "r")

    def __init__(self, name=""):
        self.name = name
        self.w = None
        self.r = []


class Sched:
    def __init__(self, nc, n_dma_sems=32):
        self.nc = nc
        self.engs = {"pe": nc.tensor, "act": nc.scalar, "dve": nc.vector, "pool": nc.gpsimd, "sp": nc.sync}
        self.sems = {}
        self.cnt = {}
        self._ctx = []
        for k in list(self.engs) + ["d%d" % i for i in range(n_dma_sems)]:
            cm = nc.semaphore("s_" + k)
            self.sems[k] = cm.__enter__()
            self._ctx.append(cm)
            self.cnt[k] = 0
        half = n_dma_sems // 2
        self.dma_keys = {"sp": ["d%d" % i for i in range(half)], "pool": ["d%d" % i for i in range(half, n_dma_sems)]}
        self.dma_rr = {"sp": 0, "pool": 0}
        self.seen = {e: {} for e in self.engs}
        self.pe_pending = False

    def close(self):
        for cm in reversed(self._ctx):
            cm.__exit__(None, None, None)

    def _wait(self, e, deps):
        need = {}
        for d in deps:
            if d is None:
                continue
            k, v = d
            if k == "pe" and e == "pe":
                continue
            if v > need.get(k, 0):
                need[k] = v
        for k, v in need.items():
            if self.seen[e].get(k, 0) >= v:
                continue
            assert v <= self.cnt[k], (e, k, v, self.cnt[k])
            self.engs[e].wait_ge(self.sems[k], v)
            self.seen[e][k] = v

    @staticmethod
    def _deps(reads, writes):
        deps = []
        for b in reads:
            deps.append(b.w)
        for b in writes:
            deps.append(b.w)
            deps.extend(b.r)
        return deps

    def _record(self, tok, reads, writes):
        for b in reads:
            b.r.append(tok)
            if len(b.r) > 64:
                best = {}
                for k, v in b.r:
                    if v > best.get(k, 0):
                        best[k] = v
                b.r = list(best.items())
        for b in writes:
            b.w = tok
            b.r = []

    def op(self, e, fn, reads=(), writes=(), inc=True):
        self._wait(e, self._deps(reads, writes))
        ins = fn(self.engs[e])
        if inc:
            self.cnt[e] += 1
            ins.then_inc(self.sems[e], 1)
            tok = (e, self.cnt[e])
            if e == "pe":
                self.pe_pending = False
        else:
            assert e == "pe"
            tok = (e, self.cnt[e] + 1)
            self.pe_pending = True
        self._record(tok, reads, writes)
        return tok

    def dma(self, q, fn, reads=(), writes=(), track=None):
        keys = self.dma_keys[q]
        k = keys[self.dma_rr[q] % len(keys)]
        self.dma_rr[q] += 1
        deps = self._deps(reads, writes)
        if self.cnt[k] > 0:
            deps.append((k, self.cnt[k]))
        self._wait(q, deps)
        ins = fn(self.engs[q])
        self.cnt[k] += 16
        ins.then_inc(self.sems[k], 16)
        tok = (k, self.cnt[k])
        self._record(tok, reads, writes)
        if track is not None:
            track.append(tok)
        return tok

    def barrier(self):
        assert not self.pe_pending
        for e in self.engs:
            self._wait(e, [(k, v) for k, v in self.cnt.items() if v > 0])


class _Stop(Exception):
    pass


def build_nc(stop=None, dumps=()):
    nc = bass.Bass("TRN2", target_bir_lowering=False)

    def din(name, shape, dt=F32):
        return nc.dram_tensor(name, list(shape), dt, kind="ExternalInput").ap()

    x_d = din("x", [T, D])
    p_d = din("p", [T, 256])
    win_d = din("w_in", [D, INW])
    wa_d = din("w_attn_proj", [512, D])
    wb_d = din("w_sgu_proj", [512, D])
    wout_d = din("w_out", [D, D])
    wr_d = din("w_router", [D, NE])
    wg_d = din("w_gate", [NE, D, D])
    wu_d = din("w_up", [NE, D, D])
    wd_d = din("w_down", [NE, D, D])
    bd_d = din("b_down", [NE, D])
    wpg_d = din("w_ple_gate", [D, D])
    wpp_d = din("w_ple_proj", [256, D])
    ws_d = din("w_spatial", [4, 128, 128])
    cvec_d = din("cvec", [128, 32])
    bgT_d = din("bgT", [128, NE * 8])
    buT_d = din("buT", [128, NE * 8])
    rowp_d = din("rowp", [128, 32 + 4 + 512 + 1024])
    cst_d = din("cst", [128, CSTW])
    out_d = nc.dram_tensor("out", [T, D], F32, kind="ExternalOutput").ap()
    x1_d = nc.dram_tensor("x1_scr", [T, D], F32, kind="Internal").ap()
    xs_d = nc.dram_tensor("xs_scr", [NSLOT + 128, D], BF16, kind="Internal").ap()
    ys_d = nc.dram_tensor("ys_scr", [NSLOT + 128, D], F32, kind="Internal").ap()

    S = Sched(nc)
    uid = [0]

    def alloc(es, shape, dt, name="t", side=None):
        uid[0] += 1
        if side is None:
            t = es.enter_context(nc.sbuf_tensor("%s_%d" % (name, uid[0]), list(shape), dt))
        else:
            t = es.enter_context(nc.sbuf_tensor("%s_%d" % (name, uid[0]), list(shape), dt, side=side))
        return t, Buf(name)

    def palloc(es, shape, dt, name="p"):
        uid[0] += 1
        t = es.enter_context(nc.psum_tensor("%s_%d" % (name, uid[0]), list(shape), dt))
        return t, Buf(name)

    class Ring:
        def __init__(self, items):
            self.items = items
            self.i = 0

        def next(self):
            it = self.items[self.i % len(self.items)]
            self.i += 1
            return it

    def pe(fn, r=(), w=(), inc=True):
        return S.op("pe", fn, r, w, inc=inc)

    def act(fn, r=(), w=()):
        return S.op("act", fn, r, w)

    def dve(fn, r=(), w=()):
        return S.op("dve", fn, r, w)

    def pool(fn, r=(), w=()):
        return S.op("pool", fn, r, w)

    def mmgroup(out_ap, outB, pairs, rB, first=True, last=True):
        n = len(pairs)
        for i, (l, r) in enumerate(pairs):
            st = first and i == 0
            sp_ = last and i == n - 1
            pe(lambda e: e.matmul(out_ap, lhsT=l, rhs=r, start=st, stop=sp_), rB, [outB], inc=(i == n - 1))

    out_tokens = []
    dump_names = []

    def dump(name, ap, B, cols):
        if name not in dumps:
            return
        dd = nc.dram_tensor("dbg_" + name, [128, cols], F32, kind="ExternalOutput").ap()
        dump_names.append(name)
        with nc.sbuf_tensor("dbgst_" + name, [128, cols], F32) as stg:
            sB = Buf("stg")
            dve(lambda e: e.tensor_copy(out=stg[:], in_=ap), [B], [sB])
            S.dma("sp", lambda e: e.dma_start(out=dd, in_=stg[:]), [sB], ())
            S.barrier()

    def check_stop(tag):
        if stop == tag:
            raise _Stop()

    try:
        _body(locals())
    except _Stop:
        pass
    S.barrier()
    S.close()
    return nc


def _body(L):
    globals_ = L
    nc = L["nc"]; S = L["S"]; alloc = L["alloc"]; palloc = L["palloc"]; Ring = L["Ring"]
    pe = L["pe"]; act = L["act"]; dve = L["dve"]; pool = L["pool"]; mmgroup = L["mmgroup"]
    out_tokens = L["out_tokens"]; dump = L["dump"]; check_stop = L["check_stop"]
    x_d = L["x_d"]; p_d = L["p_d"]; win_d = L["win_d"]; wa_d = L["wa_d"]; wb_d = L["wb_d"]; wout_d = L["wout_d"]
    wr_d = L["wr_d"]; wg_d = L["wg_d"]; wu_d = L["wu_d"]; wd_d = L["wd_d"]; bd_d = L["bd_d"]; wpg_d = L["wpg_d"]
    wpp_d = L["wpp_d"]; ws_d = L["ws_d"]; cvec_d = L["cvec_d"]; bgT_d = L["bgT_d"]; buT_d = L["buT_d"]
    rowp_d = L["rowp_d"]; cst_d = L["cst_d"]; out_d = L["out_d"]; x1_d = L["x1_d"]; xs_d = L["xs_d"]; ys_d = L["ys_d"]

    with ExitStack() as glob:
        ident16, ident16B = alloc(glob, [128, 128], BF16, "ident16")
        ident32, ident32B = alloc(glob, [128, 128], F32, "ident32")
        ones16, ones16B = alloc(glob, [128, 128], BF16, "ones16")
        cvec, cvecB = alloc(glob, [128, 32], F32, "cvec")
        bgT, bgTB = alloc(glob, [128, NE * 8], F32, "bgT")
        bu1T, bu1TB = alloc(glob, [128, NE * 8], F32, "bu1T")
        gates_all, _ = alloc(glob, [128, NT, 4], F32, "gates")
        slots_all, _ = alloc(glob, [128, NT, 4], I32, "slots")
        gateB = [Buf("gate%d" % t) for t in range(NT)]
        slotB = [Buf("slot%d" % t) for t in range(NT)]
        stat, _ = alloc(glob, [128, 128], F32, "stat")
        statR = Ring([(stat[:, i:i + 1], Buf("stat%d" % i)) for i in range(128)])

        S.dma("sp", lambda e: e.dma_start(out=cvec[:], in_=cvec_d), (), [cvecB])
        S.dma("sp", lambda e: e.dma_start(out=bgT[:], in_=bgT_d), (), [bgTB])
        S.dma("sp", lambda e: e.dma_start(out=bu1T[:], in_=buT_d), (), [bu1TB])
        dve(lambda e: e.tensor_scalar_add(out=bu1T[:], in0=bu1T[:], scalar1=1.0), [bu1TB], [bu1TB])
        pool(lambda e: e.memset(ones16[:], 1.0), (), [ones16B])
        pool(lambda e: e.memset(slots_all[:], 0), (), slotB)
        pool(lambda e: e.memset(gates_all[:], 0.0), (), gateB)

        def rms_sd(src_ap, srcB, junk_ap, junkB):
            ss, ssB = statR.next()
            act(lambda e: e.activation(out=junk_ap, in_=src_ap, func=AF.Square, accum_out=ss), [srcB], [junkB, ssB])
            sd, sdB = statR.next()
            act(lambda e: e.activation(out=sd, in_=ss, func=AF.Sqrt, bias=RMS_EPS, scale=1.0 / D), [ssB], [sdB])
            return sd, sdB

        def rms_recip(sd, sdB):
            rs, rsB = statR.next()
            dve(lambda e: e.reciprocal(out=rs, in_=sd), [sdB], [rsB])
            return rs, rsB

        def rms_rstd(src_ap, srcB, junk_ap, junkB):
            sd, sdB = rms_sd(src_ap, srcB, junk_ap, junkB)
            return rms_recip(sd, sdB)

        pbr = ExitStack()
        pre = {}
        with ExitStack() as pa:
            pw = ExitStack()
            win, _ = alloc(pw, [128, 8, INW], BF16, "win", side="right")
            WIN_GROUPS = [(GVO, 512), (QO, 512), (KO, 128), (GUO, 512), (VO, 128), (GAO, 512), (GBO, 512), (GAO + 512, 512), (GBO + 512, 512)]
            winG = [Buf("win_c%d" % c0) for (c0, _) in WIN_GROUPS]

            def winB_for(col0):
                for (c0, w), b in zip(WIN_GROUPS, winG):
                    if c0 <= col0 < c0 + w:
                        return [b]
                raise AssertionError(col0)
            wa, _ = alloc(pa, [128, 4, D], BF16, "wa")
            waB = [Buf("wa0"), Buf("wa1")]
            wb, wbB = alloc(pa, [128, 4, D], BF16, "wb")
            wout, woutB = alloc(pa, [128, 8, D], BF16, "wout")
            wr, wrB = alloc(pa, [128, 8, NE], F32, "wr")
            rowp, rowpB = alloc(pa, [128, 32 + 4 + 512], F32, "rowp")
            kaug, kaugB = alloc(pa, [2, 2, 128], BF16, "kaug")
            qaug, qaugB = alloc(pa, [2, 2, 512], BF16, "qaug")
            mask01, mask01B = alloc(pa, [128, 2, 128], BF16, "mask01")
            triU, triUB = alloc(pa, [128, 128], BF16, "triU")
            iota, iotaB = alloc(pa, [128, 32], F32, "iota")
            wsT, wsTB = alloc(pa, [128, 4, 128], BF16, "wsT")
            comb, combB = alloc(pa, [128, 4, 128], F32, "comb")
            cnt, cntB = alloc(pa, [128, 32], F32, "cnt")

            tpR = Ring([palloc(pa, [128, 1024], BF16, "tp") for _ in range(2)])
            fR = Ring([palloc(pa, [128, 512], F32, "f") for _ in range(6)])

            S.dma("sp", lambda e: e.dma_start(out=wr[:], in_=wr_d.rearrange("(c p) o -> p c o", p=128)), (), [wrB])
            S.dma("sp", lambda e: e.dma_start(out=rowp[:], in_=rowp_d[:, 0:32 + 4 + 512]), (), [rowpB])
            brt = rowp[:, 0:32]
            expsink = rowp[:, 32:36]
            bspat = rowp[:, 36:36 + 512].rearrange("p (g t) -> p g t", g=4)

            with ExitStack() as st:
                cst, cstB = alloc(st, [128, CSTW], F32, "cst")
                wsn, wsnB = alloc(st, [128, 4, 128], F32, "wsn")
                S.dma("sp", lambda e: e.dma_start(out=cst[:], in_=cst_d), (), [cstB])
                S.dma("sp", lambda e: e.dma_start(out=wsn[:], in_=ws_d.rearrange("g t s -> t g s")), (), [wsnB])
                pool(lambda e: e.memset(cnt[:], 0.0), (), [cntB])
                zf, zfB = alloc(st, [128, D], F32, "zf")
                pool(lambda e: e.memset(zf[:], 0.0), (), [zfB])
                S.dma("sp", lambda e: e.dma_start(out=ys_d[NSLOT:NSLOT + 128, :], in_=zf[:]), [zfB], ())
                dve(lambda e: e.tensor_copy(out=ident32[:], in_=cst[:, 0:128]), [cstB], [ident32B])
                dve(lambda e: e.tensor_copy(out=ident16[:], in_=cst[:, 0:128]), [cstB], [ident16B])
                dve(lambda e: e.tensor_copy(out=triU[:], in_=cst[:, 128:256]), [cstB], [triUB])
                dve(lambda e: e.tensor_copy(out=iota[:], in_=cst[:, 384:416]), [cstB], [iotaB])
                dve(lambda e: e.tensor_copy(out=kaug[:], in_=cst[0:2, 416:672].rearrange("p (j n) -> p j n", j=2)), [cstB], [kaugB])
                dve(lambda e: e.tensor_copy(out=qaug[:], in_=cst[0:2, 672:1696].rearrange("p (k n) -> p k n", k=2)), [cstB], [qaugB])
                dve(lambda e: e.tensor_copy(out=mask01[:], in_=cst[:, 1696:1952].rearrange("p (j n) -> p j n", j=2)), [cstB], [mask01B])
                act(lambda e: e.activation(out=expsink, in_=expsink, func=AF.Exp), [rowpB], [rowpB])
                for g in range(4):
                    f, fB = fR.next()
                    pe(lambda e: e.transpose(out=f[:, 0:128], in_=wsn[:, g, :], identity=ident32[:]), [wsnB, ident32B], [fB])
                    dve(lambda e: e.tensor_tensor(out=wsT[:, g, :], in0=f[:, 0:128], in1=cst[:, 256:384], op=ALU.mult), [fB, cstB], [wsTB])
                f, fB = fR.next()
                mmgroup(f[:, :], fB, [(ones16[:, :], wsT[:, :, :].rearrange("p g t -> p (g t)"))], [ones16B, wsTB])
                for g in range(4):
                    dve(lambda e: e.scalar_tensor_tensor(out=comb[:, g, :], in0=f[:, g * 128:(g + 1) * 128], scalar=cvec[:, 28 + g:29 + g],
                                                         in1=bspat[:, g, :], op0=ALU.mult, op1=ALU.add), [fB, cvecB, rowpB], [combB])
                S.barrier()
            check_stop("setup")
            for (c0, w), b in zip(WIN_GROUPS, winG):
                S.dma("pool", lambda e: e.dma_start(out=win[:, :, c0:c0 + w], in_=win_d[:, c0:c0 + w].rearrange("(kc p) f -> p kc f", p=128)), (), [b])
            for k in range(2):
                S.dma("pool", lambda e: e.dma_start(
                    out=wa[k * 64:(k + 1) * 64, :, :],
                    in_=wa_d[k * 256:(k + 1) * 256, :].rearrange("(g hd) o -> hd g o", g=4)), (), [waB[k]])
            S.dma("pool", lambda e: e.dma_start(out=wb[:], in_=wb_d.rearrange("(c p) o -> p c o", p=128)), (), [wbB])
            S.dma("pool", lambda e: e.dma_start(out=wout[:], in_=wout_d.rearrange("(c p) o -> p c o", p=128)), (), [woutB])
            xR = Ring([alloc(pa, [128, D], F32, "x") for _ in range(4)])
            xnR = Ring([alloc(pa, [128, D], BF16, "xn") for _ in range(2)])
            hT, hTB = alloc(pa, [128, 8, 512], BF16, "hT")
            qT, qTB = alloc(pa, [128, 4, 512], BF16, "qT")
            kT, kTB = alloc(pa, [128, 5 * 128], BF16, "kT")
            vS, vSB = alloc(pa, [128, 5, 128], BF16, "vS")
            guT, guTB = alloc(pa, [128, 4, 512], BF16, "guT")
            gvgR = Ring([alloc(pa, [128, 512], F32, "gvg") for _ in range(2)])
            vln, vlnB = alloc(pa, [128, 4, 512], BF16, "vln")
            bnR = Ring([alloc(pa, [128, 8], F32, "bnst") for _ in range(2)])
            ptR = Ring([alloc(pa, [128, 2, 2, 512], BF16, "pt") for _ in range(2)])
            attnT, attnTB = alloc(pa, [128, 4, 512], BF16, "attnT")
            rden, rdenB = alloc(pa, [128, 512], F32, "rden")
            sguT, sguTB = alloc(pa, [128, 4, 512], BF16, "sguT")
            sgtmp, sgtmpB = alloc(pa, [128, 512], F32, "sgtmp")
            sigAR = Ring([alloc(pa, [128, 512], F32, "sigA") for _ in range(1)])
            sigBR = Ring([alloc(pa, [128, 512], F32, "sigB") for _ in range(1)])
            mrgT, mrgTB = alloc(pa, [128, 8, 512], BF16, "mrgT")
            hn4 = [alloc(pa, [128, D], BF16, "hn") for _ in range(4)]
            xgT, xgTB = alloc(pa, [128, 8, 128], F32, "xgT")
            lg4, lg4B = alloc(pa, [128, 4, 32], F32, "lg4")
            mx8, mx8B = alloc(pa, [128, 4, 8], F32, "mx8")
            ix8, ix8B = alloc(pa, [128, 4, 8], U32, "ix8")
            ixf, ixfB = alloc(pa, [128, 4, 4], F32, "ixf")
            ex4, ex4B = alloc(pa, [128, 4, 4], F32, "ex4")
            sm4, sm4B = alloc(pa, [128, 4], F32, "sm4")
            oh, ohB = alloc(pa, [128, 4, 4, 32], F32, "oh")
            msk16, msk16B = alloc(pa, [128, 4, 32], BF16, "msk16")
            posf, posfB = alloc(pa, [128, 4, 32], F32, "posf")
            ohp, ohpB = alloc(pa, [128, 4, 4, 32], F32, "ohp")
            p4, p4B = alloc(pa, [128, 4, 4], F32, "p4")
            slf, slfB = alloc(pa, [128, 4, 4], F32, "slf")
            ovf, ovfB = alloc(pa, [128, 4, 4], F32, "ovf")

            gmixT = cvec[:, 0:8]
            gffnT = cvec[:, 8:16]

            pending_route = []
            s1st = {}

            def route_a(t0):
                tl = list(range(t0, t0 + 4))
                for i in range(4):
                    dve(lambda e: e.max(out=mx8[:, i, :], in_=lg4[:, i, :]), [lg4B], [mx8B])
                    dve(lambda e: e.max_index(out=ix8[:, i, :], in_max=mx8[:, i, :], in_values=lg4[:, i, :]), [lg4B, mx8B], [ix8B])
                dve(lambda e: e.tensor_copy(out=ixf[:], in_=ix8[:, :, 0:4]), [ix8B], [ixfB])
                dve(lambda e: e.tensor_tensor(out=ex4[:], in0=mx8[:, :, 0:4], in1=mx8[:, :, 0:1].to_broadcast([128, 4, 4]), op=ALU.subtract), [mx8B], [ex4B])
                act(lambda e: e.activation(out=ex4[:], in_=ex4[:], func=AF.Exp), [ex4B], [ex4B])
                dve(lambda e: e.reduce_sum(out=sm4[:], in_=ex4[:], axis=mybir.AxisListType.X), [ex4B], [sm4B])
                dve(lambda e: e.reciprocal(out=sm4[:], in_=sm4[:]), [sm4B], [sm4B])
                dve(lambda e: e.tensor_tensor(out=gates_all[:, t0:t0 + 4, :], in0=ex4[:], in1=sm4[:, :].unsqueeze(2).to_broadcast([128, 4, 4]), op=ALU.mult),
                    [ex4B, sm4B], [gateB[t] for t in tl])
                dve(lambda e: e.tensor_tensor(out=oh[:], in0=iota[:, :].unsqueeze(1).unsqueeze(1).to_broadcast([128, 4, 4, 32]),
                                              in1=ixf[:, :, :].unsqueeze(3).to_broadcast([128, 4, 4, 32]), op=ALU.is_equal), [iotaB, ixfB], [ohB])
                with nc.allow_low_precision(reason="0/1 mask sums are exact in bf16"):
                    dve(lambda e: e.tensor_reduce(out=msk16[:], in_=oh[:, :, :, :].rearrange("p i k e -> p i e k"), axis=mybir.AxisListType.X, op=ALU.add),
                        [ohB], [msk16B])

            def route_b(t0):
                tl = list(range(t0, t0 + 4))
                f, fB = fR.next()
                for i in range(4):
                    pairs = [(triU[:, :], msk16[:, i, :])] + [(ones16[:, :], msk16[:, i2, :]) for i2 in range(i)]
                    mmgroup(f[:, i * 32:(i + 1) * 32], fB, pairs, [triUB, ones16B, msk16B])
                mmgroup(f[:, 128:160], fB, [(ones16[:, :], msk16[:, i, :]) for i in range(4)], [ones16B, msk16B])
                dve(lambda e: e.tensor_tensor(out=posf[:], in0=f[:, 0:128].rearrange("p (i e) -> p i e", i=4),
                                              in1=cnt[:, :].unsqueeze(1).to_broadcast([128, 4, 32]), op=ALU.add), [fB, cntB], [posfB])
                dve(lambda e: e.tensor_tensor(out=cnt[:], in0=f[:, 128:160], in1=cnt[:], op=ALU.add), [fB, cntB, posfB], [cntB])
                dve(lambda e: e.tensor_tensor(out=ohp[:], in0=oh[:], in1=posf[:, :, :].unsqueeze(2).to_broadcast([128, 4, 4, 32]), op=ALU.mult), [ohB, posfB], [ohpB])
                dve(lambda e: e.reduce_sum(out=p4[:], in_=ohp[:], axis=mybir.AxisListType.X), [ohpB], [p4B])
                dve(lambda e: e.scalar_tensor_tensor(out=slf[:], in0=ixf[:], scalar=float(CAP), in1=p4[:], op0=ALU.mult, op1=ALU.add), [ixfB, p4B], [slfB])
                dve(lambda e: e.tensor_scalar(out=ovf[:], in0=p4[:], scalar1=float(CAP), scalar2=BIGF, op0=ALU.is_ge, op1=ALU.mult), [p4B], [ovfB])
                dve(lambda e: e.tensor_tensor(out=slf[:], in0=slf[:], in1=ovf[:], op=ALU.max), [slfB, ovfB], [slfB])
                dve(lambda e: e.tensor_scalar(out=slf[:], in0=slf[:], scalar1=float(NSLOT), scalar2=0.0, op0=ALU.min, op1=ALU.max), [slfB], [slfB])
                dve(lambda e: e.tensor_copy(out=slots_all[:, t0:t0 + 4, :], in_=slf[:]), [slfB], [slotB[t] for t in tl])
                if t0 == 0:
                    dump("lg", lg4[:, 0, :], lg4B, 32)
                    dump("gates", gates_all[:, 0, :], gateB[0], 4)
                    dump("slf", slf[:, 0, :], slfB, 4)
                    dump("posf", posf[:, 0, :], posfB, 32)
                for i in range(4):
                    hn, hnB = hn4[i]
                    for k in range(4):
                        S.dma("pool", lambda e: e.indirect_dma_start(
                            out=xs_d, out_offset=bass.IndirectOffsetOnAxis(ap=slots_all[:, t0 + i, k:k + 1], axis=0),
                            in_=hn[:, :], in_offset=None), [hnB, slotB[t0 + i]], ())

            for gi in range(NG):
                t0 = gi * 4
                def s1_load(tg, i):
                    t = tg * 4 + i
                    xt, xtB = xR.next()
                    S.dma("sp", lambda e: e.dma_start(out=xt[:], in_=x_d[t * 128:(t + 1) * 128, :]), (), [xtB])
                    s1st[(tg, i)] = dict(xt=xt, xtB=xtB)

                def s1a(tg, i):
                    c = s1st[(tg, i)]
                    xt, xtB = c["xt"], c["xtB"]
                    xn, xnB = xnR.next()
                    rs, rsB = rms_rstd(xt[:], xtB, xn[:], xnB)
                    dve(lambda e: e.tensor_scalar_mul(out=xn[:], in0=xt[:], scalar1=rs), [xtB, rsB], [xnB])
                    c.update(xn=xn, xnB=xnB)

                def s1b(tg, i):
                    c = s1st.pop((tg, i))
                    xn, xnB = c["xn"], c["xnB"]
                    tp, tpB = tpR.next()
                    for kc in range(8):
                        pe(lambda e: e.transpose(out=tp[:, kc * 128:(kc + 1) * 128], in_=xn[:, kc * 128:(kc + 1) * 128], identity=ident16[:]),
                           [xnB, ident16B], [tpB], inc=(kc == 7))
                    dve(lambda e: e.tensor_tensor(out=hT[:, :, i * 128:(i + 1) * 128], in0=tp[:, :].rearrange("p (c t) -> p c t", c=8),
                                                  in1=gmixT.unsqueeze(2).to_broadcast([128, 8, 128]), op=ALU.mult), [tpB, cvecB], [hTB])

                if gi == 0:
                    for i in range(4):
                        s1_load(0, i)
                        s1a(0, i)
                        s1b(0, i)
                if gi == 0:
                    for kc in range(8):
                        dump("hT%d" % kc, hT[:, kc, :], hTB, 512)
                    check_stop("g0s1")

                def inproj_fm(col0, evac):
                    f, fB = fR.next()
                    mmgroup(f[:, :], fB, [(win[:, kc, col0:col0 + 128], hT[:, kc, :]) for kc in range(8)], [hTB] + winB_for(col0))
                    evac(f, fB)

                for t0r in pending_route:
                    route_a(t0r)
                gst = {}

                def gv_mm(i):
                    f, fB = fR.next()
                    mmgroup(f[:, :], fB, [(hT[:, kc, i * 128:(i + 1) * 128], win[:, kc, GVO:GVO + 512]) for kc in range(8)], [hTB] + winB_for(GVO))
                    gvg, gvgB = gvgR.next()
                    act(lambda e: e.activation(out=gvg[:], in_=f[:, :], func=AF.Gelu_apprx_tanh), [fB], [gvgB])
                    gst[i] = (gvg, gvgB)

                def gv_ln(i):
                    gvg, gvgB = gst[i]
                    bn, bnB = bnR.next()
                    dve(lambda e: e.bn_stats(out=bn[:, 0:6], in_=gvg[:]), [gvgB], [bnB])
                    dve(lambda e: e.bn_aggr(out=bn[:, 6:8], in_=bn[:, 0:6]), [bnB], [bnB])
                    sd, sdB = statR.next()
                    act(lambda e: e.activation(out=sd, in_=bn[:, 7:8], func=AF.Sqrt, bias=LN_EPS, scale=1.0), [bnB], [sdB])
                    rs, rsB = statR.next()
                    dve(lambda e: e.reciprocal(out=rs, in_=sd), [sdB], [rsB])
                    dve(lambda e: e.tensor_scalar(out=vln[:, i, :], in0=gvg[:], scalar1=bn[:, 6:7], scalar2=rs, op0=ALU.subtract, op1=ALU.mult),
                        [gvgB, bnB, rsB], [vlnB])

                for i in range(5):
                    if i < 4:
                        gv_mm(i)
                    if i >= 1:
                        gv_ln(i - 1)

                for g in range(4):
                    inproj_fm(QO + g * 128, lambda f, fB: act(lambda e: e.copy(out=qT[:, g, :], in_=f[:, :]), [fB], [qTB]))
                inproj_fm(KO, lambda f, fB: act(lambda e: e.copy(out=kT[:, 128:640], in_=f[:, :]), [fB], [kTB]))
                while pending_route:
                    route_b(pending_route.pop(0))
                for c in range(4):
                    inproj_fm(GUO + c * 128, lambda f, fB: act(lambda e: e.activation(out=guT[:, c, :], in_=f[:, :], func=AF.Gelu_apprx_tanh), [fB], [guTB]))
                f, fB = fR.next()
                for i in range(4):
                    mmgroup(f[:, i * 128:(i + 1) * 128], fB, [(hT[:, kc, i * 128:(i + 1) * 128], win[:, kc, VO:VO + 128]) for kc in range(8)], [hTB] + winB_for(VO))
                act(lambda e: e.copy(out=vS[:, 1:5, :], in_=f[:, :].rearrange("p (i c) -> p i c", i=4)), [fB], [vSB])

                if gi == 0:
                    for g in range(4):
                        dump("qT%d" % g, qT[:, g, :], qTB, 512)
                        dump("guT%d" % g, guT[:, g, :], guTB, 512)
                        dump("vln%d" % g, vln[:, g, :], vlnB, 512)
                    dump("kT", kT[:, :], kTB, 640)
                    dump("vS", vS[:, :, :].rearrange("p a b -> p (a b)"), vSB, 640)
                    check_stop("g0s2")
                for cg in range(4):
                    f, fB = fR.next()
                    for i in range(4):
                        mmgroup(f[:, i * 128:(i + 1) * 128], fB, [(vln[:, i, cg * 128:(cg + 1) * 128], wsT[:, cg, :])], [vlnB, wsTB])
                    dve(lambda e: e.scalar_tensor_tensor(out=sgtmp[:, :].rearrange("p (i t) -> p i t", i=4), in0=f[:, :].rearrange("p (i t) -> p i t", i=4),
                                                         scalar=cvec[:, 24 + cg:25 + cg], in1=comb[:, cg, :].unsqueeze(1).to_broadcast([128, 4, 128]),
                                                         op0=ALU.mult, op1=ALU.add), [fB, cvecB, combB], [sgtmpB])
                    dve(lambda e: e.tensor_tensor(out=sguT[:, cg, :], in0=sgtmp[:], in1=guT[:, cg, :], op=ALU.mult), [sgtmpB, guTB], [sguTB])

                ast = {}

                def att_scores(i):
                    t = t0 + i
                    n = t % TPS
                    js = [(0, i), (1, i + 1)] if n > 0 else [(1, i + 1)]
                    pt, ptB = ptR.next()
                    for k in range(2):
                        for (j, slot) in js:
                            f, fB = fR.next()
                            mmgroup(f[:, :], fB, [(kT[k * 64:(k + 1) * 64, slot * 128:(slot + 1) * 128], qT[k * 64:(k + 1) * 64, :, i * 128:(i + 1) * 128]),
                                                  (kaug[0:2, j, :], qaug[0:2, k, :])], [kTB, qTB, kaugB, qaugB])
                            act(lambda e: e.activation(out=pt[:, k, j, :], in_=f[:, :], func=AF.Exp, scale=0.125), [fB], [ptB])
                            dve(lambda e: e.tensor_tensor(out=pt[:, k, j, :].rearrange("p (g q) -> p g q", g=4),
                                                           in0=pt[:, k, j, :].rearrange("p (g q) -> p g q", g=4),
                                                           in1=mask01[:, j, :].unsqueeze(1).to_broadcast([128, 4, 128]), op=ALU.mult), [ptB, mask01B], [ptB])
                    ast[i] = (js, pt, ptB)

                def att_pv(i):
                    js, pt, ptB = ast[i]
                    pv, pvB = fR.next()
                    dn, dnB = fR.next()
                    for k in range(2):
                        mmgroup(pv[k * 64:(k + 1) * 64, :], pvB, [(vS[:, slot, k * 64:(k + 1) * 64], pt[:, k, j, :]) for (j, slot) in js], [vSB, ptB])
                    for k in range(2):
                        mmgroup(dn[k * 64:(k + 1) * 64, :], dnB, [(ones16[:, 0:64], pt[:, k, j, :]) for (j, slot) in js], [ones16B, ptB])
                    dve(lambda e: e.tensor_tensor(out=rden[:, :].rearrange("p (g q) -> p g q", g=4), in0=dn[:, :].rearrange("p (g q) -> p g q", g=4),
                                                  in1=expsink.unsqueeze(2).to_broadcast([128, 4, 128]), op=ALU.add), [dnB, rowpB], [rdenB])
                    act(lambda e: e.activation(out=rden[:], in_=rden[:], func=AF.Ln), [rdenB], [rdenB])
                    act(lambda e: e.activation(out=rden[:], in_=rden[:], func=AF.Exp, scale=-1.0), [rdenB], [rdenB])
                    dve(lambda e: e.tensor_tensor(out=attnT[:, :, i * 128:(i + 1) * 128], in0=pv[:, :].rearrange("p (g q) -> p g q", g=4),
                                                  in1=rden[:, :].rearrange("p (g q) -> p g q", g=4), op=ALU.mult), [pvB, rdenB], [attnTB])

                for i in range(5):
                    if i < 4:
                        att_scores(i)
                    if i >= 1:
                        att_pv(i - 1)
                pool(lambda e: e.tensor_copy(out=kT[:, 0:128], in_=kT[:, 512:640]), [kTB], [kTB])
                pool(lambda e: e.tensor_copy(out=vS[:, 0, :], in_=vS[:, 4, :]), [vSB], [vSB])

                if gi == 0:
                    for g in range(4):
                        dump("attnT%d" % g, attnT[:, g, :], attnTB, 512)
                        dump("sguT%d" % g, sguT[:, g, :], sguTB, 512)
                    check_stop("g0s3")
                for oc in range(8):
                    sigA, sigAB = sigAR.next()
                    sigB_, sigBB = sigBR.next()
                    inproj_fm(GAO + oc * 128, lambda f, fB: act(lambda e: e.activation(out=sigA[:], in_=f[:, :], func=AF.Sigmoid), [fB], [sigAB]))
                    inproj_fm(GBO + oc * 128, lambda f, fB: act(lambda e: e.activation(out=sigB_[:], in_=f[:, :], func=AF.Sigmoid), [fB], [sigBB]))
                    f, fB = fR.next()
                    mmgroup(f[:, :], fB, [(wa[:, g, oc * 128:(oc + 1) * 128], attnT[:, g, :]) for g in range(4)], waB + [attnTB])
                    dve(lambda e: e.tensor_tensor(out=sigA[:], in0=f[:, :], in1=sigA[:], op=ALU.mult), [fB, sigAB], [sigAB])
                    f, fB = fR.next()
                    mmgroup(f[:, :], fB, [(wb[:, c, oc * 128:(oc + 1) * 128], sguT[:, c, :]) for c in range(4)], [wbB, sguTB])
                    dve(lambda e: e.tensor_tensor(out=sigB_[:], in0=f[:, :], in1=sigB_[:], op=ALU.mult), [fB, sigBB], [sigBB])
                    dve(lambda e: e.tensor_tensor(out=mrgT[:, oc, :], in0=sigA[:], in1=sigB_[:], op=ALU.add), [sigAB, sigBB], [mrgTB])

                if gi == 0:
                    for kc in range(8):
                        dump("mrgT%d" % kc, mrgT[:, kc, :], mrgTB, 512)
                    check_stop("g0s4")
                if gi == NG - 1:
                    pw.close()
                    for nm, src in (("wg", wg_d), ("wu", wu_d), ("wd", wd_d)):
                        t_, _b = alloc(pbr, [128, 8, D], BF16, nm + "0", side="right")
                        wB2 = [Buf(nm + "0h0"), Buf(nm + "0h1")]
                        for hh in range(2):
                            S.dma("pool", lambda eng: eng.dma_start(out=t_[:, hh * 4:(hh + 1) * 4, :],
                                                                    in_=src[0, hh * 512:(hh + 1) * 512, :].rearrange("(c p) o -> p c o", p=128)),
                                  (), [wB2[hh]] + winG)
                        pre[nm] = (t_, wB2)
                s5 = {}

                def s5_op(i):
                    t = t0 + i
                    xr, xrB = xR.next()
                    S.dma("sp", lambda e: e.dma_start(out=xr[:], in_=x_d[t * 128:(t + 1) * 128, :]), (), [xrB])
                    for half in range(2):
                        f, fB = fR.next()
                        mmgroup(f[:, :], fB, [(mrgT[:, kc, i * 128:(i + 1) * 128], wout[:, kc, half * 512:(half + 1) * 512]) for kc in range(8)], [mrgTB, woutB])
                        dve(lambda e: e.tensor_tensor(out=xr[:, half * 512:(half + 1) * 512], in0=f[:, :], in1=xr[:, half * 512:(half + 1) * 512], op=ALU.add),
                            [fB, xrB], [xrB])
                    S.dma("sp", lambda e: e.dma_start(out=x1_d[t * 128:(t + 1) * 128, :], in_=xr[:]), [xrB], ())
                    hn, hnB = hn4[i]
                    sd2, sd2B = rms_sd(xr[:], xrB, hn[:], hnB)
                    s5[i] = dict(xr=xr, xrB=xrB, hn=hn, hnB=hnB, sd2=sd2, sd2B=sd2B)

                def s5_tr(i):
                    c = s5[i]
                    xr, xrB, hn, hnB = c["xr"], c["xrB"], c["hn"], c["hnB"]
                    rs2, rs2B = rms_recip(c["sd2"], c["sd2B"])
                    c["rs2"], c["rs2B"] = rs2, rs2B
                    act(lambda e: e.activation(out=hn[:], in_=xr[:], func=AF.Copy, scale=rs2), [xrB, rs2B], [hnB])
                    for hh in range(2):
                        f, fB = fR.next()
                        for c4 in range(4):
                            kc = hh * 4 + c4
                            pe(lambda e: e.transpose(out=f[:, c4 * 128:(c4 + 1) * 128], in_=xr[:, kc * 128:(kc + 1) * 128], identity=ident32[:]),
                               [xrB, ident32B], [fB], inc=(c4 == 3))
                        dve(lambda e: e.tensor_tensor(out=xgT[:, hh * 4:(hh + 1) * 4, :], in0=f[:, :].rearrange("p (c t) -> p c t", c=4),
                                                      in1=gffnT[:, hh * 4:(hh + 1) * 4].unsqueeze(2).to_broadcast([128, 4, 128]), op=ALU.mult),
                            [fB, cvecB], [xgTB])

                def s5_lg(i):
                    c = s5[i]
                    f, fB = fR.next()
                    mmgroup(f[:, 0:32], fB, [(xgT[:, kc, :], wr[:, kc, :]) for kc in range(8)], [xgTB, wrB])
                    dve(lambda e: e.scalar_tensor_tensor(out=lg4[:, i, :], in0=f[:, 0:32], scalar=c["rs2"], in1=brt, op0=ALU.mult, op1=ALU.add),
                        [fB, c["rs2B"], rowpB], [lg4B])

                if gi + 1 < NG:
                    s1_load(gi + 1, 0)
                for j in range(6):
                    if j < 4:
                        s5_op(j)
                    if 2 <= j:
                        s5_lg(j - 2)
                    if 1 <= j <= 4:
                        s5_tr(j - 1)
                    if gi + 1 < NG:
                        if 1 <= j <= 4:
                            s1b(gi + 1, j - 1)
                        if j < 4:
                            s1a(gi + 1, j)
                        if j + 1 < 4:
                            s1_load(gi + 1, j + 1)
                pending_route.append(t0)

                if gi == 0:
                    check_stop("g0")
            while pending_route:
                t0r = pending_route.pop(0)
                route_a(t0r)
                route_b(t0r)
            S.barrier()
            dump("cnt", cnt[:], cntB, 32)
            check_stop("A")

        with ExitStack() as pb:
            def walloc(name):
                t_, _b = alloc(pb, [128, 8, D], BF16, name)
                return t_, [Buf(name + "h0"), Buf(name + "h1")]
            wgS = [pre["wg"], walloc("wg")]
            wuS = [pre["wu"], walloc("wu")]
            wdS = [pre["wd"], walloc("wd")]
            bd16, _ = alloc(pb, [1, 2, D], BF16, "bd16")
            bdB = [Buf("bd0"), Buf("bd1")]
            def xalloc():
                t_, _b = alloc(pb, [128, 8, CAP], BF16, "xT")
                return t_, [Buf("xT%d" % kc) for kc in range(8)]
            xTS = [xalloc() for _ in range(2)]
            actT, _ = alloc(pb, [128, 8, CAP], BF16, "actT")
            actTB = [Buf("actT0"), Buf("actT1")]
            acR = Ring([alloc(pb, [128, 384], F32, "ac") for _ in range(3)])
            sgR = Ring([alloc(pb, [128, 384], F32, "sg") for _ in range(3)])
            ttR = Ring([alloc(pb, [128, 384], F32, "tt") for _ in range(3)])
            b1R = Ring([alloc(pb, [128, 384], F32, "b1") for _ in range(3)])
            yR = Ring([alloc(pb, [128, D], F32, "yb") for _ in range(2)])
            gR = Ring([palloc(pb, [128, 512], F32, "gb") for _ in range(6)])
            dR = Ring([palloc(pb, [128, 512], F32, "db") for _ in range(2)])
            gffnT = cvec[:, 8:16]

            def load(e):
                b = e % 2
                for (ws, src) in ((wgS, wg_d), (wuS, wu_d), (wdS, wd_d)):
                    if e == 0:
                        break
                    w, wB = ws[b]
                    for hh in range(2):
                        S.dma("pool", lambda eng: eng.dma_start(out=w[:, hh * 4:(hh + 1) * 4, :],
                                                                in_=src[e, hh * 512:(hh + 1) * 512, :].rearrange("(c p) o -> p c o", p=128)), (), [wB[hh]])
                S.dma("pool", lambda eng: eng.dma_start(out=bd16[0:1, b, :], in_=bd_d[e:e + 1, :]), (), [bdB[b]])
                xT, xTB = xTS[b]
                for kc in range(8):
                    S.dma("sp", lambda eng: eng.dma_start_transpose(out=xT[:, kc, :], in_=xs_d[e * CAP:(e + 1) * CAP, kc * 128:(kc + 1) * 128]), (), [xTB[kc]])

            def transposes(e):
                b = e % 2
                xT, xTB = xTS[b]
                dve(lambda eng: eng.tensor_tensor(out=xT[:, :, :], in0=xT[:, :, :], in1=gffnT.unsqueeze(2).to_broadcast([128, 8, CAP]), op=ALU.mult),
                    xTB + [cvecB], xTB)

            HALVES = ((0, 384), (384, 256))
            pend = []

            def flush_fin():
                while pend:
                    pend.pop(0)()

            def gate_up(e, hf):
                b = e % 2
                wg, wgB = wgS[b]
                wu, wuB = wuS[b]
                xT, xTB = xTS[b]
                s0, sn = HALVES[hf]
                for fc in range(8):
                    gA, gAB = gR.next()
                    gB_, gBB = gR.next()
                    mmgroup(gA[:, 0:sn], gAB, [(wg[:, kc, fc * 128:(fc + 1) * 128], xT[:, kc, s0:s0 + sn]) for kc in range(8)], wgB + xTB)
                    mmgroup(gB_[:, 0:sn], gBB, [(wu[:, kc, fc * 128:(fc + 1) * 128], xT[:, kc, s0:s0 + sn]) for kc in range(8)], wuB + xTB)
                    ac, acB = acR.next()
                    sg, sgB = sgR.next()
                    tt, ttB = ttR.next()
                    b1, b1B = b1R.next()
                    col = e * 8 + fc
                    dve(lambda eng: eng.tensor_scalar(out=ac[:, 0:sn], in0=gA[:, 0:sn], scalar1=bgT[:, col:col + 1], scalar2=7.0, op0=ALU.add, op1=ALU.min),
                        [gAB, bgTB], [acB])
                    act(lambda eng: eng.activation(out=sg[:, 0:sn], in_=ac[:, 0:sn], func=AF.Sigmoid, scale=1.702), [acB], [sgB])
                    pool(lambda eng: eng.tensor_tensor(out=tt[:, 0:sn], in0=ac[:, 0:sn], in1=sg[:, 0:sn], op=ALU.mult), [acB, sgB], [ttB])
                    dve(lambda eng: eng.tensor_scalar(out=b1[:, 0:sn], in0=gB_[:, 0:sn], scalar1=bu1T[:, col:col + 1], scalar2=8.0, op0=ALU.add, op1=ALU.min),
                        [gBB, bu1TB], [b1B])
                    flush_fin()

                    def fin(fc=fc, s0=s0, sn=sn, b1=b1, b1B=b1B, tt=tt, ttB=ttB, hf=hf):
                        dve(lambda eng: eng.scalar_tensor_tensor(out=actT[:, fc, s0:s0 + sn], in0=b1[:, 0:sn], scalar=-6.0, in1=tt[:, 0:sn], op0=ALU.max, op1=ALU.mult),
                            [b1B, ttB], [actTB[hf]])
                    pend.append(fin)

            def down(e, blks):
                b = e % 2
                wd, wdB = wdS[b]
                for blk in blks:
                    yb, ybB = yR.next()
                    for ohf in range(2):
                        dp, dpB = dR.next()
                        pairs = [(actT[:, fc, blk * 128:(blk + 1) * 128], wd[:, fc, ohf * 512:(ohf + 1) * 512]) for fc in range(8)]
                        pairs.append((ones16[0:1, 0:128], bd16[0:1, b, ohf * 512:(ohf + 1) * 512]))
                        mmgroup(dp[:, :], dpB, pairs, [actTB[0 if blk < 3 else 1], bdB[b], ones16B] + wdB)
                        act(lambda eng: eng.copy(out=yb[:, ohf * 512:(ohf + 1) * 512], in_=dp[:, :]), [dpB], [ybB])
                    r0 = e * CAP + blk * 128
                    S.dma("sp", lambda eng: eng.dma_start(out=ys_d[r0:r0 + 128, :], in_=yb[:]), [ybB], ())

            load(0)
            transposes(0)
            for e in range(NE):
                if e + 1 < NE:
                    load(e + 1)
                gate_up(e, 0)
                gate_up(e, 1)
                flush_fin()
                down(e, (0, 1, 2))
                if e + 1 < NE:
                    transposes(e + 1)
                down(e, (3, 4))
            S.barrier()
            pbr.close()
            check_stop("B")

        with ExitStack() as pc:
            wpg, wpgB = alloc(pc, [128, 8, D], BF16, "wpg")
            wpp, wppB = alloc(pc, [128, 2, D], BF16, "wpp")
            gfin, gfinB = alloc(pc, [128, D], F32, "gfin")
            gpleT = cvec[:, 16:24]
            with ExitStack() as stc:
                wstg, wstgB = alloc(stc, [128, 8, D], F32, "wstg")
                S.dma("sp", lambda e: e.dma_start(out=wstg[:], in_=wpg_d.rearrange("(c p) o -> p c o", p=128)), (), [wstgB])
                for kc in range(8):
                    dve(lambda e: e.tensor_scalar_mul(out=wpg[:, kc, :], in0=wstg[:, kc, :], scalar1=gpleT[:, kc:kc + 1]), [wstgB, cvecB], [wpgB])
                S.barrier()
            S.dma("pool", lambda e: e.dma_start(out=wpp[:], in_=wpp_d.rearrange("(c p) o -> p c o", p=128)), (), [wppB])
            S.dma("sp", lambda e: e.dma_start(out=gfin[:], in_=rowp_d[:, 548:548 + D]), (), [gfinB])
            xcR = Ring([alloc(pc, [128, D], F32, "xc") for _ in range(6)])
            ygR = Ring([alloc(pc, [128, D], F32, "yg") for _ in range(16)])
            ptR_ = Ring([alloc(pc, [128, 256], F32, "pt32") for _ in range(4)])
            p16R = Ring([alloc(pc, [128, 256], BF16, "p16") for _ in range(2)])
            hpR = Ring([alloc(pc, [128, D], BF16, "hp") for _ in range(3)])
            hpTR = Ring([alloc(pc, [128, 8, 128], BF16, "hpT") for _ in range(3)])
            pTR = Ring([alloc(pc, [128, 2, 128], BF16, "pT") for _ in range(3)])
            sgR = Ring([alloc(pc, [128, D], F32, "sgc") for _ in range(3)])
            junkR = Ring([alloc(pc, [128, D], BF16, "junk") for _ in range(2)])
            tpR = Ring([palloc(pc, [128, 1024], BF16, "tpc") for _ in range(2)])
            fR = Ring([palloc(pc, [128, 512], F32, "fc") for _ in range(6)])
            cs = {}

            def stL(t):
                xc, xcB = xcR.next()
                S.dma("sp", lambda e: e.dma_start(out=xc[:], in_=x1_d[t * 128:(t + 1) * 128, :]), (), [xcB])
                p32, p32B = ptR_.next()
                S.dma("sp", lambda e: e.dma_start(out=p32[:], in_=p_d[t * 128:(t + 1) * 128, :]), (), [p32B])
                ygs = []
                for k in range(4):
                    yg, ygB = ygR.next()
                    S.dma("pool", lambda e: e.indirect_dma_start(
                        out=yg[:, :], out_offset=None, in_=ys_d,
                        in_offset=bass.IndirectOffsetOnAxis(ap=slots_all[:, t, k:k + 1], axis=0)), [slotB[t]], [ygB])
                    ygs.append((yg, ygB))
                cs[t] = dict(xc=xc, xcB=xcB, p32=p32, p32B=p32B, ygs=ygs)

            def stB(t):
                c = cs[t]
                xc, xcB = c["xc"], c["xcB"]
                for k in range(4):
                    yg, ygB = c["ygs"][k]
                    dve(lambda e: e.scalar_tensor_tensor(out=xc[:], in0=yg[:], scalar=gates_all[:, t, k:k + 1], in1=xc[:], op0=ALU.mult, op1=ALU.add),
                        [ygB, gateB[t], xcB], [xcB])
                hp, hpB = hpR.next()
                act(lambda e: e.copy(out=hp[:], in_=xc[:]), [xcB], [hpB])
                p16, p16B = p16R.next()
                act(lambda e: e.copy(out=p16[:], in_=c["p32"][:]), [c["p32B"]], [p16B])
                junk, junkB = junkR.next()
                sd3, sd3B = rms_sd(xc[:], xcB, junk[:], junkB)
                c.update(hp=hp, hpB=hpB, p16=p16, p16B=p16B, sd3=sd3, sd3B=sd3B)

            def stC(t):
                c = cs[t]
                hp, hpB, p16, p16B = c["hp"], c["hpB"], c["p16"], c["p16B"]
                c["rs3"], c["rs3B"] = rms_recip(c["sd3"], c["sd3B"])
                tp, tpB = tpR.next()
                for kc in range(8):
                    pe(lambda e: e.transpose(out=tp[:, kc * 128:(kc + 1) * 128], in_=hp[:, kc * 128:(kc + 1) * 128], identity=ident16[:]),
                       [hpB, ident16B], [tpB], inc=(kc == 7))
                hpT, hpTB = hpTR.next()
                act(lambda e: e.copy(out=hpT[:], in_=tp[:, :].rearrange("p (c t) -> p c t", c=8)), [tpB], [hpTB])
                tp2, tp2B = tpR.next()
                for cc in range(2):
                    pe(lambda e: e.transpose(out=tp2[:, cc * 128:(cc + 1) * 128], in_=p16[:, cc * 128:(cc + 1) * 128], identity=ident16[:]),
                       [p16B, ident16B], [tp2B], inc=(cc == 1))
                pT, pTB = pTR.next()
                act(lambda e: e.copy(out=pT[:], in_=tp2[:, 0:256].rearrange("p (c t) -> p c t", c=2)), [tp2B], [pTB])
                c.update(hpT=hpT, hpTB=hpTB, pT=pT, pTB=pTB)

            def stD(t):
                c = cs[t]
                xc, xcB = c["xc"], c["xcB"]
                hpT, hpTB, pT, pTB = c["hpT"], c["hpTB"], c["pT"], c["pTB"]
                sg, sgB = sgR.next()
                for half in range(2):
                    fg, fgB = fR.next()
                    mmgroup(fg[:, :], fgB, [(hpT[:, kc, :], wpg[:, kc, half * 512:(half + 1) * 512]) for kc in range(8)], [hpTB, wpgB])
                    fp, fpB = fR.next()
                    mmgroup(fp[:, :], fpB, [(pT[:, cc, :], wpp[:, cc, half * 512:(half + 1) * 512]) for cc in range(2)], [pTB, wppB])
                    act(lambda e: e.activation(out=sg[:, half * 512:(half + 1) * 512], in_=fg[:, :], func=AF.Sigmoid, scale=c["rs3"]), [fgB, c["rs3B"]], [sgB])
                    c["fp%d" % half] = (fp, fpB)
                c.update(sg=sg, sgB=sgB)

            def stD2(t):
                c = cs[t]
                xc, xcB, sg, sgB = c["xc"], c["xcB"], c["sg"], c["sgB"]
                for half in range(2):
                    fp, fpB = c["fp%d" % half]
                    dve(lambda e: e.tensor_tensor(out=sg[:, half * 512:(half + 1) * 512], in0=fp[:, :], in1=sg[:, half * 512:(half + 1) * 512], op=ALU.mult),
                        [fpB, sgB], [sgB])
                dve(lambda e: e.tensor_tensor(out=xc[:], in0=xc[:], in1=sg[:], op=ALU.add), [xcB, sgB], [xcB])
                junk, junkB = junkR.next()
                sd4, sd4B = rms_sd(xc[:], xcB, junk[:], junkB)
                c.update(sg=sg, sgB=sgB, sd4=sd4, sd4B=sd4B)

            def stE(t):
                c = cs[t]
                xc, xcB, sg, sgB = c["xc"], c["xcB"], c["sg"], c["sgB"]
                c["rs4"], c["rs4B"] = rms_recip(c["sd4"], c["sd4B"])
                dve(lambda e: e.scalar_tensor_tensor(out=sg[:], in0=xc[:], scalar=c["rs4"], in1=gfin[:], op0=ALU.mult, op1=ALU.mult), [xcB, c["rs4B"], gfinB], [sgB])
                S.dma("sp", lambda e: e.dma_start(out=out_d[t * 128:(t + 1) * 128, :], in_=sg[:]), [sgB], (), track=out_tokens)
                del cs[t]

            for i in range(-3, NT + 1):
                for fn, tt_ in ((stL, i + 3), (stC, i + 1), (stD, i), (stB, i + 2), (stD2, i), (stE, i - 1)):
                    if 0 <= tt_ < NT:
                        fn(tt_)
            S._wait("sp", out_tokens)
            S.barrier()


def _consts():
    ident = np.eye(128, dtype=np.float32)
    ar = np.arange(128)
    triU = (ar[:, None] < ar[None, :]).astype(np.float32)
    maskI = (ar[:, None] <= ar[None, :]).astype(np.float32)
    iota = np.broadcast_to(np.arange(32, dtype=np.float32), (128, 32))
    kaug = np.zeros((128, 2, 128), np.float32)
    kaug[0, 0, :] = ar - 128.0
    kaug[0, 1, :] = ar
    kaug[1, :, :] = 1.0
    qaug = np.zeros((128, 2, 4, 128), np.float32)
    for k in range(2):
        for g in range(4):
            slope = 2.0 ** (-8.0 * (k * 4 + g + 1) / 8.0)
            qaug[0, k, g, :] = 8.0 * slope
            qaug[1, k, g, :] = -8.0 * slope * ar
    mask01 = np.zeros((128, 2, 128), np.float32)
    mask01[:, 0, :] = (ar[:, None] > ar[None, :])
    mask01[:, 1, :] = (ar[:, None] <= ar[None, :])
    return np.ascontiguousarray(np.concatenate([ident, triU, maskI, iota, kaug.reshape(128, 256), qaug.reshape(128, 1024),
                                                mask01.reshape(128, 256)], axis=1))


_NC_CACHE = {}


def _prep(x, p, g_mix, w_in, attn_sinks, g_sgu, b_sgu, w_spatial, b_spatial, w_attn_proj, w_sgu_proj,
          w_out, g_ffn, w_router, b_router, w_gate, b_gate, w_up, b_up, w_down, b_down,
          g_ple, w_ple_gate, w_ple_proj, g_final):
    f = lambda a: np.ascontiguousarray(np.asarray(a, dtype=np.float32))
    x = f(x).reshape(NCORES, T, D)
    p = f(p)[0].reshape(NCORES, T, 256)
    w_in0 = f(w_in)[0]
    qcols = np.concatenate([np.arange((k * 4 + g) * 64, (k * 4 + g + 1) * 64) for g in range(4) for k in range(2)])
    w_in_p = np.ascontiguousarray(np.concatenate([w_in0[:, qcols], w_in0[:, 512:]], axis=1))
    colT = lambda v: f(v).reshape(-1, 128).T
    cvec = np.ascontiguousarray(np.concatenate([colT(g_mix[0]), colT(g_ffn[0]), colT(g_ple[0]), colT(g_sgu[0]), colT(b_sgu[0])], axis=1))
    bgT = np.ascontiguousarray(f(b_gate)[0].reshape(NE, 8, 128).transpose(2, 0, 1).reshape(128, NE * 8))
    buT = np.ascontiguousarray(f(b_up)[0].reshape(NE, 8, 128).transpose(2, 0, 1).reshape(128, NE * 8))
    sinks = f(attn_sinks)[0].reshape(2, 4)
    sink_rows = np.repeat(sinks, 64, axis=0)
    bc = lambda v: np.broadcast_to(f(v).reshape(1, -1), (128, f(v).size))
    rowp = np.ascontiguousarray(np.concatenate([bc(b_router[0]), sink_rows, bc(b_spatial[0]), bc(g_final)], axis=1))
    shared = {
        "w_in": w_in_p, "w_attn_proj": f(w_attn_proj)[0], "w_sgu_proj": f(w_sgu_proj)[0], "w_out": f(w_out)[0],
        "w_router": f(w_router)[0], "w_gate": f(w_gate)[0], "w_up": f(w_up)[0], "w_down": f(w_down)[0],
        "b_down": f(b_down)[0], "w_ple_gate": f(w_ple_gate)[0], "w_ple_proj": f(w_ple_proj)[0],
        "w_spatial": f(w_spatial)[0], "cvec": cvec, "bgT": bgT, "buT": buT, "rowp": rowp, "cst": _consts(),
    }
    return shared, x, p


def kernel(**inputs):
    shared, x, p = _prep(**inputs)
    if "nc" not in _NC_CACHE:
        _NC_CACHE["nc"] = build_nc()
    nc = _NC_CACHE["nc"]
    in_maps = []
    for c in range(NCORES):
        m = dict(shared)
        m["x"] = x[c]
        m["p"] = p[c]
        in_maps.append(m)
    res = run_bass_kernel_spmd(nc, in_maps, core_ids=list(range(NCORES)))
    out = np.stack([np.asarray(r["out"], dtype=np.float32) for r in res.results], axis=0)
    return out.reshape(16, 2048, D)
```

```python
from contextlib import ExitStack
import numpy as np
import concourse.bass as bass
import concourse.mybir as mybir
from concourse.bass_utils import run_bass_kernel_spmd

F32 = mybir.dt.float32
BF16 = mybir.dt.bfloat16
I32 = mybir.dt.int32
U32 = mybir.dt.uint32
AF = mybir.ActivationFunctionType
ALU = mybir.AluOpType

NCORES = 8
D = 1024
T = 4096
NT = 32
TPS = 16
NG = 8
NE = 32
CAP = 640
NB = CAP // 128
NSLOT = NE * CAP
BIGF = 1.0e6
QO, KO, VO, GUO, GVO, GAO, GBO = 0, 512, 640, 768, 1280, 1792, 2816
INW = 3840
RMS_EPS = 1e-6
LN_EPS = 1e-5
CSTW = 128 * 3 + 32 + 256 + 1024 + 256


class Buf:
    __slots__ = ("name", "w", "r")

    def __init__(self, name=""):
        self.name = name
        self.w = None
        self.r = []


class Sched:
    def __init__(self, nc, n_dma_sems=32):
        self.nc = nc
        self.engs = {"pe": nc.tensor, "act": nc.scalar, "dve": nc.vector, "pool": nc.gpsimd, "sp": nc.sync}
        self.sems = {}
        self.cnt = {}
        self._ctx = []
        for k in list(self.engs) + ["d%d" % i for i in range(n_dma_sems)]:
            cm = nc.semaphore("s_" + k)
            self.sems[k] = cm.__enter__()
            self._ctx.append(cm)
            self.cnt[k] = 0
        half = n_dma_sems // 2
        self.dma_keys = {"sp": ["d%d" % i for i in range(half)], "pool": ["d%d" % i for i in range(half, n_dma_sems)]}
        self.dma_rr = {"sp": 0, "pool": 0}
        self.seen = {e: {} for e in self.engs}
        self.pe_pending = False

    def close(self):
        for cm in reversed(self._ctx):
            cm.__exit__(None, None, None)

    def _wait(self, e, deps):
        need = {}
        for d in deps:
            if d is None:
                continue
            k, v = d
            if k == "pe" and e == "pe":
                continue
            if v > need.get(k, 0):
                need[k] = v
        for k, v in need.items():
            if self.seen[e].get(k, 0) >= v:
                continue
            assert v <= self.cnt[k], (e, k, v, self.cnt[k])
            self.engs[e].wait_ge(self.sems[k], v)
            self.seen[e][k] = v

    @staticmethod
    def _deps(reads, writes):
        deps = []
        for b in reads:
            deps.append(b.w)
        for b in writes:
            deps.append(b.w)
            deps.extend(b.r)
        return deps

    def _record(self, tok, reads, writes):
        for b in reads:
            b.r.append(tok)
            if len(b.r) > 64:
                best = {}
                for k, v in b.r:
                    if v > best.get(k, 0):
                        best[k] = v
                b.r = list(best.items())
        for b in writes:
            b.w = tok
            b.r = []

    def op(self, e, fn, reads=(), writes=(), inc=True):
        self._wait(e, self._deps(reads, writes))
        ins = fn(self.engs[e])
        if inc:
            self.cnt[e] += 1
            ins.then_inc(self.sems[e], 1)
            tok = (e, self.cnt[e])
            if e == "pe":
                self.pe_pending = False
        else:
            assert e == "pe"
            tok = (e, self.cnt[e] + 1)
            self.pe_pending = True
        self._record(tok, reads, writes)
        return tok

    def dma(self, q, fn, reads=(), writes=(), track=None):
        keys = self.dma_keys[q]
        k = keys[self.dma_rr[q] % len(keys)]
        self.dma_rr[q] += 1
        deps = self._deps(reads, writes)
        if self.cnt[k] > 0:
            deps.append((k, self.cnt[k]))
        self._wait(q, deps)
        ins = fn(self.engs[q])
        self.cnt[k] += 16
        ins.then_inc(self.sems[k], 16)
        tok = (k, self.cnt[k])
        self._record(tok, reads, writes)
        if track is not None:
            track.append(tok)
        return tok

    def barrier(self):
        assert not self.pe_pending
        for e in self.engs:
            self._wait(e, [(k, v) for k, v in self.cnt.items() if v > 0])


class _Stop(Exception):
    pass


def build_nc(stop=None, dumps=()):
    nc = bass.Bass("TRN2", target_bir_lowering=False)

    def din(name, shape, dt=F32):
        return nc.dram_tensor(name, list(shape), dt, kind="ExternalInput").ap()

    x_d = din("x", [T, D])
    p_d = din("p", [T, 256])
    win_d = din("w_in", [D, INW])
    wa_d = din("w_attn_proj", [512, D])
    wb_d = din("w_sgu_proj", [512, D])
    wout_d = din("w_out", [D, D])
    wr_d = din("w_router", [D, NE])
    wg_d = din("w_gate", [NE, D, D])
    wu_d = din("w_up", [NE, D, D])
    wd_d = din("w_down", [NE, D, D])
    bd_d = din("b_down", [NE, D])
    wpg_d = din("w_ple_gate", [D, D])
    wpp_d = din("w_ple_proj", [256, D])
    ws_d = din("w_spatial", [4, 128, 128])
    cvec_d = din("cvec", [128, 32])
    bgT_d = din("bgT", [128, NE * 8])
    buT_d = din("buT", [128, NE * 8])
    rowp_d = din("rowp", [128, 32 + 4 + 512 + 1024])
    cst_d = din("cst", [128, CSTW])
    out_d = nc.dram_tensor("out", [T, D], F32, kind="ExternalOutput").ap()
    x1_d = nc.dram_tensor("x1_scr", [T, D], F32, kind="Internal").ap()
    xs_d = nc.dram_tensor("xs_scr", [NSLOT + 128, D], BF16, kind="Internal").ap()
    ys_d = nc.dram_tensor("ys_scr", [NSLOT + 128, D], F32, kind="Internal").ap()

    S = Sched(nc)
    uid = [0]

    def alloc(es, shape, dt, name="t", side=None):
        uid[0] += 1
        if side is None:
            t = es.enter_context(nc.sbuf_tensor("%s_%d" % (name, uid[0]), list(shape), dt))
        else:
            t = es.enter_context(nc.sbuf_tensor("%s_%d" % (name, uid[0]), list(shape), dt, side=side))
        return t, Buf(name)

    def palloc(es, shape, dt, name="p"):
        uid[0] += 1
        t = es.enter_context(nc.psum_tensor("%s_%d" % (name, uid[0]), list(shape), dt))
        return t, Buf(name)

    class Ring:
        def __init__(self, items):
            self.items = items
            self.i = 0

        def next(self):
            it = self.items[self.i % len(self.items)]
            self.i += 1
            return it

    def pe(fn, r=(), w=(), inc=True):
        return S.op("pe", fn, r, w, inc=inc)

    def act(fn, r=(), w=()):
        return S.op("act", fn, r, w)

    def dve(fn, r=(), w=()):
        return S.op("dve", fn, r, w)

    def pool(fn, r=(), w=()):
        return S.op("pool", fn, r, w)

    def mmgroup(out_ap, outB, pairs, rB, first=True, last=True):
        n = len(pairs)
        for i, (l, r) in enumerate(pairs):
            st = first and i == 0
            sp_ = last and i == n - 1
            pe(lambda e: e.matmul(out_ap, lhsT=l, rhs=r, start=st, stop=sp_), rB, [outB], inc=(i == n - 1))

    out_tokens = []
    dump_names = []

    def dump(name, ap, B, cols):
        if name not in dumps:
            return
        dd = nc.dram_tensor("dbg_" + name, [128, cols], F32, kind="ExternalOutput").ap()
        dump_names.append(name)
        with nc.sbuf_tensor("dbgst_" + name, [128, cols], F32) as stg:
            sB = Buf("stg")
            dve(lambda e: e.tensor_copy(out=stg[:], in_=ap), [B], [sB])
            S.dma("sp", lambda e: e.dma_start(out=dd, in_=stg[:]), [sB], ())
            S.barrier()

    def check_stop(tag):
        if stop == tag:
            raise _Stop()

    try:
        _body(locals())
    except _Stop:
        pass
    S.barrier()
    S.close()
    return nc


def _body(L):
    globals_ = L
    nc = L["nc"]; S = L["S"]; alloc = L["alloc"]; palloc = L["palloc"]; Ring = L["Ring"]
    pe = L["pe"]; act = L["act"]; dve = L["dve"]; pool = L["pool"]; mmgroup = L["mmgroup"]
    out_tokens = L["out_tokens"]; dump = L["dump"]; check_stop = L["check_stop"]
    x_d = L["x_d"]; p_d = L["p_d"]; win_d = L["win_d"]; wa_d = L["wa_d"]; wb_d = L["wb_d"]; wout_d = L["wout_d"]
    wr_d = L["wr_d"]; wg_d = L["wg_d"]; wu_d = L["wu_d"]; wd_d = L["wd_d"]; bd_d = L["bd_d"]; wpg_d = L["wpg_d"]
    wpp_d = L["wpp_d"]; ws_d = L["ws_d"]; cvec_d = L["cvec_d"]; bgT_d = L["bgT_d"]; buT_d = L["buT_d"]
    rowp_d = L["rowp_d"]; cst_d = L["cst_d"]; out_d = L["out_d"]; x1_d = L["x1_d"]; xs_d = L["xs_d"]; ys_d = L["ys_d"]

    with ExitStack() as glob:
        ident16, ident16B = alloc(glob, [128, 128], BF16, "ident16")
        ident32, ident32B = alloc(glob, [128, 128], F32, "ident32")
        ones16, ones16B = alloc(glob, [128, 128], BF16, "ones16")
        cvec, cvecB = alloc(glob, [128, 32], F32, "cvec")
        bgT, bgTB = alloc(glob, [128, NE * 8], F32, "bgT")
        bu1T, bu1TB = alloc(glob, [128, NE * 8], F32, "bu1T")
        gates_all, _ = alloc(glob, [128, NT, 4], F32, "gates")
        slots_all, _ = alloc(glob, [128, NT, 4], I32, "slots")
        gateB = [Buf("gate%d" % t) for t in range(NT)]
        slotB = [Buf("slot%d" % t) for t in range(NT)]
        stat, _ = alloc(glob, [128, 128], F32, "stat")
        statR = Ring([(stat[:, i:i + 1], Buf("stat%d" % i)) for i in range(128)])

        S.dma("sp", lambda e: e.dma_start(out=cvec[:], in_=cvec_d), (), [cvecB])
        S.dma("sp", lambda e: e.dma_start(out=bgT[:], in_=bgT_d), (), [bgTB])
        S.dma("sp", lambda e: e.dma_start(out=bu1T[:], in_=buT_d), (), [bu1TB])
        dve(lambda e: e.tensor_scalar_add(out=bu1T[:], in0=bu1T[:], scalar1=1.0), [bu1TB], [bu1TB])
        pool(lambda e: e.memset(ones16[:], 1.0), (), [ones16B])
        mhalf, mhalfB = alloc(glob, [128, 1], F32, "mhalf")
        pool(lambda e: e.memset(mhalf[:], -0.5), (), [mhalfB])
        pool(lambda e: e.memset(slots_all[:], 0), (), slotB)
        pool(lambda e: e.memset(gates_all[:], 0.0), (), gateB)

        def rms_sd(src_ap, srcB, junk_ap, junkB):
            ss, ssB = statR.next()
            act(lambda e: e.activation(out=junk_ap, in_=src_ap, func=AF.Square, accum_out=ss), [srcB], [junkB, ssB])
            return ss, ssB

        def rms_sd_c(src_ap, srcB, junk_ap, junkB):
            ss, ssB = rms_sd(src_ap, srcB, junk_ap, junkB)
            sd, sdB = statR.next()
            act(lambda e: e.activation(out=sd, in_=ss, func=AF.Sqrt, bias=RMS_EPS, scale=1.0 / D), [ssB], [sdB])
            return sd, sdB

        def rms_recip_c(sd, sdB):
            rs, rsB = statR.next()
            dve(lambda e: e.reciprocal(out=rs, in_=sd), [sdB], [rsB])
            return rs, rsB

        def rms_recip(ss, ssB, use_pool=True):
            if not use_pool:
                sd, sdB = statR.next()
                act(lambda e: e.activation(out=sd, in_=ss, func=AF.Sqrt, bias=RMS_EPS, scale=1.0 / D), [ssB], [sdB])
                rs, rsB = statR.next()
                dve(lambda e: e.reciprocal(out=rs, in_=sd), [sdB], [rsB])
                return rs, rsB
            v, vB = statR.next()
            dve(lambda e: e.tensor_scalar(out=v, in0=ss, scalar1=1.0 / D, scalar2=RMS_EPS, op0=ALU.mult, op1=ALU.add), [ssB], [vB])
            rs, rsB = statR.next()
            pool(lambda e: e.tensor_tensor(out=rs, in0=v, in1=mhalf[:, 0:1], op=ALU.pow), [vB, mhalfB], [rsB])
            return rs, rsB

        def rms_rstd(src_ap, srcB, junk_ap, junkB):
            sd, sdB = rms_sd(src_ap, srcB, junk_ap, junkB)
            return rms_recip(sd, sdB)

        pbr = ExitStack()
        pre = {}
        with ExitStack() as pa:
            pw = ExitStack()
            win, _ = alloc(pw, [128, 8, INW], BF16, "win", side="right")
            WIN_GROUPS = [(GVO, 512), (QO, 512), (KO, 128), (GUO, 512), (VO, 128), (GAO, 512), (GBO, 512), (GAO + 512, 512), (GBO + 512, 512)]
            winG = [Buf("win_c%d" % c0) for (c0, _) in WIN_GROUPS]

            def winB_for(col0):
                for (c0, w), b in zip(WIN_GROUPS, winG):
                    if c0 <= col0 < c0 + w:
                        return [b]
                raise AssertionError(col0)
            wa, _ = alloc(pa, [128, 4, D], BF16, "wa")
            waB = [Buf("wa0"), Buf("wa1")]
            wb, wbB = alloc(pa, [128, 4, D], BF16, "wb")
            wout, woutB = alloc(pa, [128, 8, D], BF16, "wout")
            wr, wrB = alloc(pa, [128, 8, NE], F32, "wr")
            rowp, rowpB = alloc(pa, [128, 32 + 4 + 512], F32, "rowp")
            kaug, kaugB = alloc(pa, [2, 2, 128], BF16, "kaug")
            qaug, qaugB = alloc(pa, [2, 2, 512], BF16, "qaug")
            mask01, mask01B = alloc(pa, [128, 2, 128], BF16, "mask01")
            triU, triUB = alloc(pa, [128, 128], BF16, "triU")
            iota, iotaB = alloc(pa, [128, 32], F32, "iota")
            wsT, wsTB = alloc(pa, [128, 4, 128], BF16, "wsT")
            comb, combB = alloc(pa, [128, 4, 128], F32, "comb")
            cnt, cntB = alloc(pa, [128, 32], F32, "cnt")

            tpR = Ring([palloc(pa, [128, 1024], BF16, "tp") for _ in range(2)])
            fR = Ring([palloc(pa, [128, 512], F32, "f") for _ in range(6)])

            S.dma("sp", lambda e: e.dma_start(out=wr[:], in_=wr_d.rearrange("(c p) o -> p c o", p=128)), (), [wrB])
            S.dma("sp", lambda e: e.dma_start(out=rowp[:], in_=rowp_d[:, 0:32 + 4 + 512]), (), [rowpB])
            brt = rowp[:, 0:32]
            expsink = rowp[:, 32:36]
            bspat = rowp[:, 36:36 + 512].rearrange("p (g t) -> p g t", g=4)

            with ExitStack() as st:
                cst, cstB = alloc(st, [128, CSTW], F32, "cst")
                wsn, wsnB = alloc(st, [128, 4, 128], F32, "wsn")
                S.dma("sp", lambda e: e.dma_start(out=cst[:], in_=cst_d), (), [cstB])
                S.dma("sp", lambda e: e.dma_start(out=wsn[:], in_=ws_d.rearrange("g t s -> t g s")), (), [wsnB])
                pool(lambda e: e.memset(cnt[:], 0.0), (), [cntB])
                zf, zfB = alloc(st, [128, D], F32, "zf")
                pool(lambda e: e.memset(zf[:], 0.0), (), [zfB])
                S.dma("sp", lambda e: e.dma_start(out=ys_d[NSLOT:NSLOT + 128, :], in_=zf[:]), [zfB], ())
                dve(lambda e: e.tensor_copy(out=ident32[:], in_=cst[:, 0:128]), [cstB], [ident32B])
                dve(lambda e: e.tensor_copy(out=ident16[:], in_=cst[:, 0:128]), [cstB], [ident16B])
                dve(lambda e: e.tensor_copy(out=triU[:], in_=cst[:, 128:256]), [cstB], [triUB])
                dve(lambda e: e.tensor_copy(out=iota[:], in_=cst[:, 384:416]), [cstB], [iotaB])
                dve(lambda e: e.tensor_copy(out=kaug[:], in_=cst[0:2, 416:672].rearrange("p (j n) -> p j n", j=2)), [cstB], [kaugB])
                dve(lambda e: e.tensor_copy(out=qaug[:], in_=cst[0:2, 672:1696].rearrange("p (k n) -> p k n", k=2)), [cstB], [qaugB])
                dve(lambda e: e.tensor_copy(out=mask01[:], in_=cst[:, 1696:1952].rearrange("p (j n) -> p j n", j=2)), [cstB], [mask01B])
                act(lambda e: e.activation(out=expsink, in_=expsink, func=AF.Exp), [rowpB], [rowpB])
                for g in range(4):
                    f, fB = fR.next()
                    pe(lambda e: e.transpose(out=f[:, 0:128], in_=wsn[:, g, :], identity=ident32[:]), [wsnB, ident32B], [fB])
                    dve(lambda e: e.tensor_tensor(out=wsT[:, g, :], in0=f[:, 0:128], in1=cst[:, 256:384], op=ALU.mult), [fB, cstB], [wsTB])
                f, fB = fR.next()
                mmgroup(f[:, :], fB, [(ones16[:, :], wsT[:, :, :].rearrange("p g t -> p (g t)"))], [ones16B, wsTB])
                for g in range(4):
                    dve(lambda e: e.scalar_tensor_tensor(out=comb[:, g, :], in0=f[:, g * 128:(g + 1) * 128], scalar=cvec[:, 28 + g:29 + g],
                                                         in1=bspat[:, g, :], op0=ALU.mult, op1=ALU.add), [fB, cvecB, rowpB], [combB])
                S.barrier()
            check_stop("setup")
            for (c0, w), b in zip(WIN_GROUPS, winG):
                S.dma("pool", lambda e: e.dma_start(out=win[:, :, c0:c0 + w], in_=win_d[:, c0:c0 + w].rearrange("(kc p) f -> p kc f", p=128)), (), [b])
            for k in range(2):
                S.dma("pool", lambda e: e.dma_start(
                    out=wa[k * 64:(k + 1) * 64, :, :],
                    in_=wa_d[k * 256:(k + 1) * 256, :].rearrange("(g hd) o -> hd g o", g=4)), (), [waB[k]])
            S.dma("pool", lambda e: e.dma_start(out=wb[:], in_=wb_d.rearrange("(c p) o -> p c o", p=128)), (), [wbB])
            S.dma("pool", lambda e: e.dma_start(out=wout[:], in_=wout_d.rearrange("(c p) o -> p c o", p=128)), (), [woutB])
            xR = Ring([alloc(pa, [128, D], F32, "x") for _ in range(4)])
            xnR = Ring([alloc(pa, [128, D], BF16, "xn") for _ in range(2)])
            hT, hTB = alloc(pa, [128, 8, 512], BF16, "hT")
            qT, qTB = alloc(pa, [128, 4, 512], BF16, "qT")
            kT, kTB = alloc(pa, [128, 5 * 128], BF16, "kT")
            vS, vSB = alloc(pa, [128, 5, 128], BF16, "vS")
            guT, guTB = alloc(pa, [128, 4, 512], BF16, "guT")
            gvgR = Ring([alloc(pa, [128, 512], F32, "gvg") for _ in range(2)])
            vln, vlnB = alloc(pa, [128, 4, 512], BF16, "vln")
            bnR = Ring([alloc(pa, [128, 8], F32, "bnst") for _ in range(2)])
            ptR = Ring([alloc(pa, [128, 2, 2, 512], BF16, "pt") for _ in range(2)])
            attnT, attnTB = alloc(pa, [128, 4, 512], BF16, "attnT")
            rden, rdenB = alloc(pa, [128, 512], F32, "rden")
            sguT, sguTB = alloc(pa, [128, 4, 512], BF16, "sguT")
            sgtmp, sgtmpB = alloc(pa, [128, 512], F32, "sgtmp")
            sigAR = Ring([alloc(pa, [128, 512], F32, "sigA") for _ in range(1)])
            sigBR = Ring([alloc(pa, [128, 512], F32, "sigB") for _ in range(1)])
            mrgT, mrgTB = alloc(pa, [128, 8, 512], BF16, "mrgT")
            hn4 = [alloc(pa, [128, D], BF16, "hn") for _ in range(4)]
            xgT, xgTB = alloc(pa, [128, 8, 128], F32, "xgT")
            lg4, lg4B = alloc(pa, [128, 4, 32], F32, "lg4")
            mx8, mx8B = alloc(pa, [128, 4, 8], F32, "mx8")
            ix8, ix8B = alloc(pa, [128, 4, 8], U32, "ix8")
            ixf, ixfB = alloc(pa, [128, 4, 4], F32, "ixf")
            ex4, ex4B = alloc(pa, [128, 4, 4], F32, "ex4")
            sm4, sm4B = alloc(pa, [128, 4], F32, "sm4")
            oh, ohB = alloc(pa, [128, 4, 4, 32], F32, "oh")
            msk16, msk16B = alloc(pa, [128, 4, 32], BF16, "msk16")
            posf, posfB = alloc(pa, [128, 4, 32], F32, "posf")
            ohp, ohpB = alloc(pa, [128, 4, 4, 32], F32, "ohp")
            p4, p4B = alloc(pa, [128, 4, 4], F32, "p4")
            slf, slfB = alloc(pa, [128, 4, 4], F32, "slf")
            ovf, ovfB = alloc(pa, [128, 4, 4], F32, "ovf")

            gmixT = cvec[:, 0:8]
            gffnT = cvec[:, 8:16]

            pending_route = []
            s1st = {}

            def route_a(t0):
                tl = list(range(t0, t0 + 4))
                for i in range(4):
                    dve(lambda e: e.max(out=mx8[:, i, :], in_=lg4[:, i, :]), [lg4B], [mx8B])
                    dve(lambda e: e.max_index(out=ix8[:, i, :], in_max=mx8[:, i, :], in_values=lg4[:, i, :]), [lg4B, mx8B], [ix8B])
                dve(lambda e: e.tensor_copy(out=ixf[:], in_=ix8[:, :, 0:4]), [ix8B], [ixfB])
                dve(lambda e: e.tensor_tensor(out=ex4[:], in0=mx8[:, :, 0:4], in1=mx8[:, :, 0:1].to_broadcast([128, 4, 4]), op=ALU.subtract), [mx8B], [ex4B])
                act(lambda e: e.activation(out=ex4[:], in_=ex4[:], func=AF.Exp), [ex4B], [ex4B])
                dve(lambda e: e.reduce_sum(out=sm4[:], in_=ex4[:], axis=mybir.AxisListType.X), [ex4B], [sm4B])
                dve(lambda e: e.reciprocal(out=sm4[:], in_=sm4[:]), [sm4B], [sm4B])
                dve(lambda e: e.tensor_tensor(out=gates_all[:, t0:t0 + 4, :], in0=ex4[:], in1=sm4[:, :].unsqueeze(2).to_broadcast([128, 4, 4]), op=ALU.mult),
                    [ex4B, sm4B], [gateB[t] for t in tl])
                dve(lambda e: e.tensor_tensor(out=oh[:], in0=iota[:, :].unsqueeze(1).unsqueeze(1).to_broadcast([128, 4, 4, 32]),
                                              in1=ixf[:, :, :].unsqueeze(3).to_broadcast([128, 4, 4, 32]), op=ALU.is_equal), [iotaB, ixfB], [ohB])
                with nc.allow_low_precision(reason="0/1 mask sums are exact in bf16"):
                    dve(lambda e: e.tensor_reduce(out=msk16[:], in_=oh[:, :, :, :].rearrange("p i k e -> p i e k"), axis=mybir.AxisListType.X, op=ALU.add),
                        [ohB], [msk16B])

            def route_b(t0):
                tl = list(range(t0, t0 + 4))
                f, fB = fR.next()
                for i in range(4):
                    pairs = [(triU[:, :], msk16[:, i, :])] + [(ones16[:, :], msk16[:, i2, :]) for i2 in range(i)]
                    mmgroup(f[:, i * 32:(i + 1) * 32], fB, pairs, [triUB, ones16B, msk16B])
                mmgroup(f[:, 128:160], fB, [(ones16[:, :], msk16[:, i, :]) for i in range(4)], [ones16B, msk16B])
                dve(lambda e: e.tensor_tensor(out=posf[:], in0=f[:, 0:128].rearrange("p (i e) -> p i e", i=4),
                                              in1=cnt[:, :].unsqueeze(1).to_broadcast([128, 4, 32]), op=ALU.add), [fB, cntB], [posfB])
                dve(lambda e: e.tensor_tensor(out=cnt[:], in0=f[:, 128:160], in1=cnt[:], op=ALU.add), [fB, cntB, posfB], [cntB])
                dve(lambda e: e.tensor_tensor(out=ohp[:], in0=oh[:], in1=posf[:, :, :].unsqueeze(2).to_broadcast([128, 4, 4, 32]), op=ALU.mult), [ohB, posfB], [ohpB])
                dve(lambda e: e.reduce_sum(out=p4[:], in_=ohp[:], axis=mybir.AxisListType.X), [ohpB], [p4B])
                dve(lambda e: e.scalar_tensor_tensor(out=slf[:], in0=ixf[:], scalar=float(CAP), in1=p4[:], op0=ALU.mult, op1=ALU.add), [ixfB, p4B], [slfB])
                dve(lambda e: e.tensor_scalar(out=ovf[:], in0=p4[:], scalar1=float(CAP), scalar2=BIGF, op0=ALU.is_ge, op1=ALU.mult), [p4B], [ovfB])
                dve(lambda e: e.tensor_tensor(out=slf[:], in0=slf[:], in1=ovf[:], op=ALU.max), [slfB, ovfB], [slfB])
                dve(lambda e: e.tensor_scalar(out=slf[:], in0=slf[:], scalar1=float(NSLOT), scalar2=0.0, op0=ALU.min, op1=ALU.max), [slfB], [slfB])
                dve(lambda e: e.tensor_copy(out=slots_all[:, t0:t0 + 4, :], in_=slf[:]), [slfB], [slotB[t] for t in tl])
                if t0 == 0:
                    dump("lg", lg4[:, 0, :], lg4B, 32)
                    dump("gates", gates_all[:, 0, :], gateB[0], 4)
                    dump("slf", slf[:, 0, :], slfB, 4)
                    dump("posf", posf[:, 0, :], posfB, 32)
                for i in range(4):
                    hn, hnB = hn4[i]
                    for k in range(4):
                        S.dma("pool", lambda e: e.indirect_dma_start(
                            out=xs_d, out_offset=bass.IndirectOffsetOnAxis(ap=slots_all[:, t0 + i, k:k + 1], axis=0),
                            in_=hn[:, :], in_offset=None), [hnB, slotB[t0 + i]], ())

            for gi in range(NG):
                t0 = gi * 4
                def s1_load(tg, i):
                    t = tg * 4 + i
                    xt, xtB = xR.next()
                    S.dma("sp", lambda e: e.dma_start(out=xt[:], in_=x_d[t * 128:(t + 1) * 128, :]), (), [xtB])
                    s1st[(tg, i)] = dict(xt=xt, xtB=xtB)

                def s1a(tg, i):
                    c = s1st[(tg, i)]
                    xt, xtB = c["xt"], c["xtB"]
                    xn, xnB = xnR.next()
                    rs, rsB = rms_rstd(xt[:], xtB, xn[:], xnB)
                    dve(lambda e: e.tensor_scalar_mul(out=xn[:], in0=xt[:], scalar1=rs), [xtB, rsB], [xnB])
                    c.update(xn=xn, xnB=xnB)

                def s1b(tg, i):
                    c = s1st.pop((tg, i))
                    xn, xnB = c["xn"], c["xnB"]
                    tp, tpB = tpR.next()
                    for kc in range(8):
                        pe(lambda e: e.transpose(out=tp[:, kc * 128:(kc + 1) * 128], in_=xn[:, kc * 128:(kc + 1) * 128], identity=ident16[:]),
                           [xnB, ident16B], [tpB], inc=(kc == 7))
                    dve(lambda e: e.tensor_tensor(out=hT[:, :, i * 128:(i + 1) * 128], in0=tp[:, :].rearrange("p (c t) -> p c t", c=8),
                                                  in1=gmixT.unsqueeze(2).to_broadcast([128, 8, 128]), op=ALU.mult), [tpB, cvecB], [hTB])

                if gi == 0:
                    for i in range(4):
                        s1_load(0, i)
                        s1a(0, i)
                        s1b(0, i)
                if gi == 0:
                    for kc in range(8):
                        dump("hT%d" % kc, hT[:, kc, :], hTB, 512)
                    check_stop("g0s1")

                def inproj_fm(col0, evac):
                    f, fB = fR.next()
                    mmgroup(f[:, :], fB, [(win[:, kc, col0:col0 + 128], hT[:, kc, :]) for kc in range(8)], [hTB] + winB_for(col0))
                    evac(f, fB)

                for t0r in pending_route:
                    route_a(t0r)
                gst = {}

                def gv_mm(i):
                    f, fB = fR.next()
                    mmgroup(f[:, :], fB, [(hT[:, kc, i * 128:(i + 1) * 128], win[:, kc, GVO:GVO + 512]) for kc in range(8)], [hTB] + winB_for(GVO))
                    gvg, gvgB = gvgR.next()
                    act(lambda e: e.activation(out=gvg[:], in_=f[:, :], func=AF.Gelu_apprx_tanh), [fB], [gvgB])
                    gst[i] = (gvg, gvgB)

                def gv_ln(i):
                    gvg, gvgB = gst[i]
                    bn, bnB = bnR.next()
                    dve(lambda e: e.bn_stats(out=bn[:, 0:6], in_=gvg[:]), [gvgB], [bnB])
                    dve(lambda e: e.bn_aggr(out=bn[:, 6:8], in_=bn[:, 0:6]), [bnB], [bnB])
                    sd, sdB = statR.next()
                    dve(lambda e: e.tensor_scalar_add(out=sd, in0=bn[:, 7:8], scalar1=LN_EPS), [bnB], [sdB])
                    rs, rsB = statR.next()
                    pool(lambda e: e.tensor_tensor(out=rs, in0=sd, in1=mhalf[:, 0:1], op=ALU.pow), [sdB, mhalfB], [rsB])
                    dve(lambda e: e.tensor_scalar(out=vln[:, i, :], in0=gvg[:], scalar1=bn[:, 6:7], scalar2=rs, op0=ALU.subtract, op1=ALU.mult),
                        [gvgB, bnB, rsB], [vlnB])

                for i in range(5):
                    if i < 4:
                        gv_mm(i)
                    if i >= 1:
                        gv_ln(i - 1)

                for g in range(4):
                    inproj_fm(QO + g * 128, lambda f, fB: act(lambda e: e.copy(out=qT[:, g, :], in_=f[:, :]), [fB], [qTB]))
                inproj_fm(KO, lambda f, fB: act(lambda e: e.copy(out=kT[:, 128:640], in_=f[:, :]), [fB], [kTB]))
                while pending_route:
                    route_b(pending_route.pop(0))
                for c in range(4):
                    inproj_fm(GUO + c * 128, lambda f, fB: act(lambda e: e.activation(out=guT[:, c, :], in_=f[:, :], func=AF.Gelu_apprx_tanh), [fB], [guTB]))
                f, fB = fR.next()
                for i in range(4):
                    mmgroup(f[:, i * 128:(i + 1) * 128], fB, [(hT[:, kc, i * 128:(i + 1) * 128], win[:, kc, VO:VO + 128]) for kc in range(8)], [hTB] + winB_for(VO))
                act(lambda e: e.copy(out=vS[:, 1:5, :], in_=f[:, :].rearrange("p (i c) -> p i c", i=4)), [fB], [vSB])

                if gi == 0:
                    for g in range(4):
                        dump("qT%d" % g, qT[:, g, :], qTB, 512)
                        dump("guT%d" % g, guT[:, g, :], guTB, 512)
                        dump("vln%d" % g, vln[:, g, :], vlnB, 512)
                    dump("kT", kT[:, :], kTB, 640)
                    dump("vS", vS[:, :, :].rearrange("p a b -> p (a b)"), vSB, 640)
                    check_stop("g0s2")
                for cg in range(4):
                    f, fB = fR.next()
                    for i in range(4):
                        mmgroup(f[:, i * 128:(i + 1) * 128], fB, [(vln[:, i, cg * 128:(cg + 1) * 128], wsT[:, cg, :])], [vlnB, wsTB])
                    dve(lambda e: e.scalar_tensor_tensor(out=sgtmp[:, :].rearrange("p (i t) -> p i t", i=4), in0=f[:, :].rearrange("p (i t) -> p i t", i=4),
                                                         scalar=cvec[:, 24 + cg:25 + cg], in1=comb[:, cg, :].unsqueeze(1).to_broadcast([128, 4, 128]),
                                                         op0=ALU.mult, op1=ALU.add), [fB, cvecB, combB], [sgtmpB])
                    dve(lambda e: e.tensor_tensor(out=sguT[:, cg, :], in0=sgtmp[:], in1=guT[:, cg, :], op=ALU.mult), [sgtmpB, guTB], [sguTB])

                ast = {}

                def att_scores(i):
                    t = t0 + i
                    n = t % TPS
                    js = [(0, i), (1, i + 1)] if n > 0 else [(1, i + 1)]
                    pt, ptB = ptR.next()
                    for k in range(2):
                        for (j, slot) in js:
                            f, fB = fR.next()
                            mmgroup(f[:, :], fB, [(kT[k * 64:(k + 1) * 64, slot * 128:(slot + 1) * 128], qT[k * 64:(k + 1) * 64, :, i * 128:(i + 1) * 128]),
                                                  (kaug[0:2, j, :], qaug[0:2, k, :])], [kTB, qTB, kaugB, qaugB])
                            act(lambda e: e.activation(out=pt[:, k, j, :], in_=f[:, :], func=AF.Exp, scale=0.125), [fB], [ptB])
                            dve(lambda e: e.tensor_tensor(out=pt[:, k, j, :].rearrange("p (g q) -> p g q", g=4),
                                                           in0=pt[:, k, j, :].rearrange("p (g q) -> p g q", g=4),
                                                           in1=mask01[:, j, :].unsqueeze(1).to_broadcast([128, 4, 128]), op=ALU.mult), [ptB, mask01B], [ptB])
                    ast[i] = (js, pt, ptB)

                def att_pv(i):
                    js, pt, ptB = ast[i]
                    pv, pvB = fR.next()
                    dn, dnB = fR.next()
                    for k in range(2):
                        mmgroup(pv[k * 64:(k + 1) * 64, :], pvB, [(vS[:, slot, k * 64:(k + 1) * 64], pt[:, k, j, :]) for (j, slot) in js], [vSB, ptB])
                    for k in range(2):
                        mmgroup(dn[k * 64:(k + 1) * 64, :], dnB, [(ones16[:, 0:64], pt[:, k, j, :]) for (j, slot) in js], [ones16B, ptB])
                    dve(lambda e: e.tensor_tensor(out=rden[:, :].rearrange("p (g q) -> p g q", g=4), in0=dn[:, :].rearrange("p (g q) -> p g q", g=4),
                                                  in1=expsink.unsqueeze(2).to_broadcast([128, 4, 128]), op=ALU.add), [dnB, rowpB], [rdenB])
                    act(lambda e: e.activation(out=rden[:], in_=rden[:], func=AF.Ln), [rdenB], [rdenB])
                    act(lambda e: e.activation(out=rden[:], in_=rden[:], func=AF.Exp, scale=-1.0), [rdenB], [rdenB])
                    dve(lambda e: e.tensor_tensor(out=attnT[:, :, i * 128:(i + 1) * 128], in0=pv[:, :].rearrange("p (g q) -> p g q", g=4),
                                                  in1=rden[:, :].rearrange("p (g q) -> p g q", g=4), op=ALU.mult), [pvB, rdenB], [attnTB])

                for i in range(5):
                    if i < 4:
                        att_scores(i)
                    if i >= 1:
                        att_pv(i - 1)
                pool(lambda e: e.tensor_copy(out=kT[:, 0:128], in_=kT[:, 512:640]), [kTB], [kTB])
                pool(lambda e: e.tensor_copy(out=vS[:, 0, :], in_=vS[:, 4, :]), [vSB], [vSB])

                if gi == 0:
                    for g in range(4):
                        dump("attnT%d" % g, attnT[:, g, :], attnTB, 512)
                        dump("sguT%d" % g, sguT[:, g, :], sguTB, 512)
                    check_stop("g0s3")
                for oc in range(8):
                    sigA, sigAB = sigAR.next()
                    sigB_, sigBB = sigBR.next()
                    inproj_fm(GAO + oc * 128, lambda f, fB: act(lambda e: e.activation(out=sigA[:], in_=f[:, :], func=AF.Sigmoid), [fB], [sigAB]))
                    inproj_fm(GBO + oc * 128, lambda f, fB: act(lambda e: e.activation(out=sigB_[:], in_=f[:, :], func=AF.Sigmoid), [fB], [sigBB]))
                    f, fB = fR.next()
                    mmgroup(f[:, :], fB, [(wa[:, g, oc * 128:(oc + 1) * 128], attnT[:, g, :]) for g in range(4)], waB + [attnTB])
                    dve(lambda e: e.tensor_tensor(out=sigA[:], in0=f[:, :], in1=sigA[:], op=ALU.mult), [fB, sigAB], [sigAB])
                    f, fB = fR.next()
                    mmgroup(f[:, :], fB, [(wb[:, c, oc * 128:(oc + 1) * 128], sguT[:, c, :]) for c in range(4)], [wbB, sguTB])
                    dve(lambda e: e.tensor_tensor(out=sigB_[:], in0=f[:, :], in1=sigB_[:], op=ALU.mult), [fB, sigBB], [sigBB])
                    dve(lambda e: e.tensor_tensor(out=mrgT[:, oc, :], in0=sigA[:], in1=sigB_[:], op=ALU.add), [sigAB, sigBB], [mrgTB])

                if gi == 0:
                    for kc in range(8):
                        dump("mrgT%d" % kc, mrgT[:, kc, :], mrgTB, 512)
                    check_stop("g0s4")
                if gi == NG - 1:
                    pw.close()
                    for nm, src in (("wg", wg_d), ("wu", wu_d), ("wd", wd_d)):
                        t_, _b = alloc(pbr, [128, 8, D], BF16, nm + "0", side="right")
                        wB2 = [Buf(nm + "0h0"), Buf(nm + "0h1")]
                        for hh in range(2):
                            S.dma("pool", lambda eng: eng.dma_start(out=t_[:, hh * 4:(hh + 1) * 4, :],
                                                                    in_=src[0, hh * 512:(hh + 1) * 512, :].rearrange("(c p) o -> p c o", p=128)),
                                  (), [wB2[hh]] + winG)
                        pre[nm] = (t_, wB2)
                s5 = {}

                def s5_op(i):
                    t = t0 + i
                    xr, xrB = xR.next()
                    S.dma("sp", lambda e: e.dma_start(out=xr[:], in_=x_d[t * 128:(t + 1) * 128, :]), (), [xrB])
                    for half in range(2):
                        f, fB = fR.next()
                        mmgroup(f[:, :], fB, [(mrgT[:, kc, i * 128:(i + 1) * 128], wout[:, kc, half * 512:(half + 1) * 512]) for kc in range(8)], [mrgTB, woutB])
                        dve(lambda e: e.tensor_tensor(out=xr[:, half * 512:(half + 1) * 512], in0=f[:, :], in1=xr[:, half * 512:(half + 1) * 512], op=ALU.add),
                            [fB, xrB], [xrB])
                    S.dma("sp", lambda e: e.dma_start(out=x1_d[t * 128:(t + 1) * 128, :], in_=xr[:]), [xrB], ())
                    hn, hnB = hn4[i]
                    sd2, sd2B = rms_sd(xr[:], xrB, hn[:], hnB)
                    s5[i] = dict(xr=xr, xrB=xrB, hn=hn, hnB=hnB, sd2=sd2, sd2B=sd2B)

                def s5_tr(i):
                    c = s5[i]
                    xr, xrB, hn, hnB = c["xr"], c["xrB"], c["hn"], c["hnB"]
                    rs2, rs2B = rms_recip(c["sd2"], c["sd2B"])
                    c["rs2"], c["rs2B"] = rs2, rs2B
                    act(lambda e: e.activation(out=hn[:], in_=xr[:], func=AF.Copy, scale=rs2), [xrB, rs2B], [hnB])
                    for hh in range(2):
                        f, fB = fR.next()
                        for c4 in range(4):
                            kc = hh * 4 + c4
                            pe(lambda e: e.transpose(out=f[:, c4 * 128:(c4 + 1) * 128], in_=xr[:, kc * 128:(kc + 1) * 128], identity=ident32[:]),
                               [xrB, ident32B], [fB], inc=(c4 == 3))
                        dve(lambda e: e.tensor_tensor(out=xgT[:, hh * 4:(hh + 1) * 4, :], in0=f[:, :].rearrange("p (c t) -> p c t", c=4),
                                                      in1=gffnT[:, hh * 4:(hh + 1) * 4].unsqueeze(2).to_broadcast([128, 4, 128]), op=ALU.mult),
                            [fB, cvecB], [xgTB])

                def s5_lg(i):
                    c = s5[i]
                    f, fB = fR.next()
                    mmgroup(f[:, 0:32], fB, [(xgT[:, kc, :], wr[:, kc, :]) for kc in range(8)], [xgTB, wrB])
                    dve(lambda e: e.scalar_tensor_tensor(out=lg4[:, i, :], in0=f[:, 0:32], scalar=c["rs2"], in1=brt, op0=ALU.mult, op1=ALU.add),
                        [fB, c["rs2B"], rowpB], [lg4B])

                if gi + 1 < NG:
                    s1_load(gi + 1, 0)
                for j in range(6):
                    if j < 4:
                        s5_op(j)
                    if 2 <= j:
                        s5_lg(j - 2)
                    if 1 <= j <= 4:
                        s5_tr(j - 1)
                    if gi + 1 < NG:
                        if 1 <= j <= 4:
                            s1b(gi + 1, j - 1)
                        if j < 4:
                            s1a(gi + 1, j)
                        if j + 1 < 4:
                            s1_load(gi + 1, j + 1)
                pending_route.append(t0)

                if gi == 0:
                    check_stop("g0")
            while pending_route:
                t0r = pending_route.pop(0)
                route_a(t0r)
                route_b(t0r)
            S.barrier()
            dump("cnt", cnt[:], cntB, 32)
            check_stop("A")

        with ExitStack() as pb:
            def walloc(name):
                t_, _b = alloc(pb, [128, 8, D], BF16, name)
                return t_, [Buf(name + "h0"), Buf(name + "h1")]
            wgS = [pre["wg"], walloc("wg")]
            wuS = [pre["wu"], walloc("wu")]
            wdS = [pre["wd"], walloc("wd")]
            bd16, _ = alloc(pb, [1, 2, D], BF16, "bd16")
            bdB = [Buf("bd0"), Buf("bd1")]
            def xalloc():
                t_, _b = alloc(pb, [128, 8, CAP], BF16, "xT")
                return t_, [Buf("xT%d" % kc) for kc in range(8)]
            xTS = [xalloc() for _ in range(2)]
            actT, _ = alloc(pb, [128, 8, CAP], BF16, "actT")
            actTB = [Buf("actT0"), Buf("actT1")]
            acR = Ring([alloc(pb, [128, 384], F32, "ac") for _ in range(3)])
            sgR = Ring([alloc(pb, [128, 384], F32, "sg") for _ in range(3)])
            ttR = Ring([alloc(pb, [128, 384], F32, "tt") for _ in range(3)])
            b1R = Ring([alloc(pb, [128, 384], F32, "b1") for _ in range(3)])
            yR = Ring([alloc(pb, [128, D], F32, "yb") for _ in range(2)])
            gR = Ring([palloc(pb, [128, 512], F32, "gb") for _ in range(6)])
            dR = Ring([palloc(pb, [128, 512], F32, "db") for _ in range(2)])
            gffnT = cvec[:, 8:16]

            def load(e):
                b = e % 2
                for (ws, src) in ((wgS, wg_d), (wuS, wu_d), (wdS, wd_d)):
                    if e == 0:
                        break
                    w, wB = ws[b]
                    for hh in range(2):
                        S.dma("pool", lambda eng: eng.dma_start(out=w[:, hh * 4:(hh + 1) * 4, :],
                                                                in_=src[e, hh * 512:(hh + 1) * 512, :].rearrange("(c p) o -> p c o", p=128)), (), [wB[hh]])
                S.dma("pool", lambda eng: eng.dma_start(out=bd16[0:1, b, :], in_=bd_d[e:e + 1, :]), (), [bdB[b]])
                xT, xTB = xTS[b]
                for kc in range(8):
                    S.dma("sp", lambda eng: eng.dma_start_transpose(out=xT[:, kc, :], in_=xs_d[e * CAP:(e + 1) * CAP, kc * 128:(kc + 1) * 128]), (), [xTB[kc]])

            def transposes(e):
                b = e % 2
                xT, xTB = xTS[b]
                dve(lambda eng: eng.tensor_tensor(out=xT[:, :, :], in0=xT[:, :, :], in1=gffnT.unsqueeze(2).to_broadcast([128, 8, CAP]), op=ALU.mult),
                    xTB + [cvecB], xTB)

            HALVES = ((0, 384), (384, 256))
            pend = []

            def flush_fin():
                while pend:
                    pend.pop(0)()

            def gate_up(e, hf):
                b = e % 2
                wg, wgB = wgS[b]
                wu, wuB = wuS[b]
                xT, xTB = xTS[b]
                s0, sn = HALVES[hf]
                for fc in range(8):
                    gA, gAB = gR.next()
                    gB_, gBB = gR.next()
                    mmgroup(gA[:, 0:sn], gAB, [(wg[:, kc, fc * 128:(fc + 1) * 128], xT[:, kc, s0:s0 + sn]) for kc in range(8)], wgB + xTB)
                    mmgroup(gB_[:, 0:sn], gBB, [(wu[:, kc, fc * 128:(fc + 1) * 128], xT[:, kc, s0:s0 + sn]) for kc in range(8)], wuB + xTB)
                    ac, acB = acR.next()
                    sg, sgB = sgR.next()
                    tt, ttB = ttR.next()
                    b1, b1B = b1R.next()
                    col = e * 8 + fc
                    dve(lambda eng: eng.tensor_scalar(out=ac[:, 0:sn], in0=gA[:, 0:sn], scalar1=bgT[:, col:col + 1], scalar2=7.0, op0=ALU.add, op1=ALU.min),
                        [gAB, bgTB], [acB])
                    act(lambda eng: eng.activation(out=sg[:, 0:sn], in_=ac[:, 0:sn], func=AF.Sigmoid, scale=1.702), [acB], [sgB])
                    pool(lambda eng: eng.tensor_tensor(out=tt[:, 0:sn], in0=ac[:, 0:sn], in1=sg[:, 0:sn], op=ALU.mult), [acB, sgB], [ttB])
                    dve(lambda eng: eng.tensor_scalar(out=b1[:, 0:sn], in0=gB_[:, 0:sn], scalar1=bu1T[:, col:col + 1], scalar2=8.0, op0=ALU.add, op1=ALU.min),
                        [gBB, bu1TB], [b1B])
                    flush_fin()

                    def fin(fc=fc, s0=s0, sn=sn, b1=b1, b1B=b1B, tt=tt, ttB=ttB, hf=hf):
                        dve(lambda eng: eng.scalar_tensor_tensor(out=actT[:, fc, s0:s0 + sn], in0=b1[:, 0:sn], scalar=-6.0, in1=tt[:, 0:sn], op0=ALU.max, op1=ALU.mult),
                            [b1B, ttB], [actTB[hf]])
                    pend.append(fin)

            def down(e, blks):
                b = e % 2
                wd, wdB = wdS[b]
                for blk in blks:
                    yb, ybB = yR.next()
                    for ohf in range(2):
                        dp, dpB = dR.next()
                        pairs = [(actT[:, fc, blk * 128:(blk + 1) * 128], wd[:, fc, ohf * 512:(ohf + 1) * 512]) for fc in range(8)]
                        pairs.append((ones16[0:1, 0:128], bd16[0:1, b, ohf * 512:(ohf + 1) * 512]))
                        mmgroup(dp[:, :], dpB, pairs, [actTB[0 if blk < 3 else 1], bdB[b], ones16B] + wdB)
                        act(lambda eng: eng.copy(out=yb[:, ohf * 512:(ohf + 1) * 512], in_=dp[:, :]), [dpB], [ybB])
                    r0 = e * CAP + blk * 128
                    S.dma("sp", lambda eng: eng.dma_start(out=ys_d[r0:r0 + 128, :], in_=yb[:]), [ybB], ())

            load(0)
            transposes(0)
            for e in range(NE):
                if e + 1 < NE:
                    load(e + 1)
                gate_up(e, 0)
                gate_up(e, 1)
                flush_fin()
                down(e, (0, 1, 2))
                if e + 1 < NE:
                    transposes(e + 1)
                down(e, (3, 4))
            S.barrier()
            pbr.close()
            check_stop("B")

        with ExitStack() as pc:
            wpg, wpgB = alloc(pc, [128, 8, D], BF16, "wpg")
            wpp, wppB = alloc(pc, [128, 2, D], BF16, "wpp")
            gfin, gfinB = alloc(pc, [128, D], F32, "gfin")
            gpleT = cvec[:, 16:24]
            with ExitStack() as stc:
                wstg, wstgB = alloc(stc, [128, 8, D], F32, "wstg")
                S.dma("sp", lambda e: e.dma_start(out=wstg[:], in_=wpg_d.rearrange("(c p) o -> p c o", p=128)), (), [wstgB])
                for kc in range(8):
                    dve(lambda e: e.tensor_scalar_mul(out=wpg[:, kc, :], in0=wstg[:, kc, :], scalar1=gpleT[:, kc:kc + 1]), [wstgB, cvecB], [wpgB])
                S.barrier()
            S.dma("pool", lambda e: e.dma_start(out=wpp[:], in_=wpp_d.rearrange("(c p) o -> p c o", p=128)), (), [wppB])
            S.dma("sp", lambda e: e.dma_start(out=gfin[:], in_=rowp_d[:, 548:548 + D]), (), [gfinB])
            xcR = Ring([alloc(pc, [128, D], F32, "xc") for _ in range(6)])
            ygR = Ring([alloc(pc, [128, D], F32, "yg") for _ in range(16)])
            ptR_ = Ring([alloc(pc, [128, 256], F32, "pt32") for _ in range(4)])
            p16R = Ring([alloc(pc, [128, 256], BF16, "p16") for _ in range(2)])
            hpR = Ring([alloc(pc, [128, D], BF16, "hp") for _ in range(3)])
            hpTR = Ring([alloc(pc, [128, 8, 128], BF16, "hpT") for _ in range(3)])
            pTR = Ring([alloc(pc, [128, 2, 128], BF16, "pT") for _ in range(3)])
            sgR = Ring([alloc(pc, [128, D], F32, "sgc") for _ in range(3)])
            junkR = Ring([alloc(pc, [128, D], BF16, "junk") for _ in range(2)])
            tpR = Ring([palloc(pc, [128, 1024], BF16, "tpc") for _ in range(2)])
            fR = Ring([palloc(pc, [128, 512], F32, "fc") for _ in range(6)])
            cs = {}

            def stL(t):
                xc, xcB = xcR.next()
                S.dma("sp", lambda e: e.dma_start(out=xc[:], in_=x1_d[t * 128:(t + 1) * 128, :]), (), [xcB])
                p32, p32B = ptR_.next()
                S.dma("sp", lambda e: e.dma_start(out=p32[:], in_=p_d[t * 128:(t + 1) * 128, :]), (), [p32B])
                ygs = []
                for k in range(4):
                    yg, ygB = ygR.next()
                    S.dma("pool", lambda e: e.indirect_dma_start(
                        out=yg[:, :], out_offset=None, in_=ys_d,
                        in_offset=bass.IndirectOffsetOnAxis(ap=slots_all[:, t, k:k + 1], axis=0)), [slotB[t]], [ygB])
                    ygs.append((yg, ygB))
                cs[t] = dict(xc=xc, xcB=xcB, p32=p32, p32B=p32B, ygs=ygs)

            def stB(t):
                c = cs[t]
                xc, xcB = c["xc"], c["xcB"]
                for k in range(4):
                    yg, ygB = c["ygs"][k]
                    dve(lambda e: e.scalar_tensor_tensor(out=xc[:], in0=yg[:], scalar=gates_all[:, t, k:k + 1], in1=xc[:], op0=ALU.mult, op1=ALU.add),
                        [ygB, gateB[t], xcB], [xcB])
                hp, hpB = hpR.next()
                act(lambda e: e.copy(out=hp[:], in_=xc[:]), [xcB], [hpB])
                p16, p16B = p16R.next()
                act(lambda e: e.copy(out=p16[:], in_=c["p32"][:]), [c["p32B"]], [p16B])
                junk, junkB = junkR.next()
                sd3, sd3B = rms_sd_c(xc[:], xcB, junk[:], junkB)
                c.update(hp=hp, hpB=hpB, p16=p16, p16B=p16B, sd3=sd3, sd3B=sd3B)

            def stC(t):
                c = cs[t]
                hp, hpB, p16, p16B = c["hp"], c["hpB"], c["p16"], c["p16B"]
                c["rs3"], c["rs3B"] = rms_recip_c(c["sd3"], c["sd3B"])
                tp, tpB = tpR.next()
                for kc in range(8):
                    pe(lambda e: e.transpose(out=tp[:, kc * 128:(kc + 1) * 128], in_=hp[:, kc * 128:(kc + 1) * 128], identity=ident16[:]),
                       [hpB, ident16B], [tpB], inc=(kc == 7))
                hpT, hpTB = hpTR.next()
                act(lambda e: e.copy(out=hpT[:], in_=tp[:, :].rearrange("p (c t) -> p c t", c=8)), [tpB], [hpTB])
                tp2, tp2B = tpR.next()
                for cc in range(2):
                    pe(lambda e: e.transpose(out=tp2[:, cc * 128:(cc + 1) * 128], in_=p16[:, cc * 128:(cc + 1) * 128], identity=ident16[:]),
                       [p16B, ident16B], [tp2B], inc=(cc == 1))
                pT, pTB = pTR.next()
                act(lambda e: e.copy(out=pT[:], in_=tp2[:, 0:256].rearrange("p (c t) -> p c t", c=2)), [tp2B], [pTB])
                c.update(hpT=hpT, hpTB=hpTB, pT=pT, pTB=pTB)

            def stD(t):
                c = cs[t]
                xc, xcB = c["xc"], c["xcB"]
                hpT, hpTB, pT, pTB = c["hpT"], c["hpTB"], c["pT"], c["pTB"]
                sg, sgB = sgR.next()
                for half in range(2):
                    fg, fgB = fR.next()
                    mmgroup(fg[:, :], fgB, [(hpT[:, kc, :], wpg[:, kc, half * 512:(half + 1) * 512]) for kc in range(8)], [hpTB, wpgB])
                    fp, fpB = fR.next()
                    mmgroup(fp[:, :], fpB, [(pT[:, cc, :], wpp[:, cc, half * 512:(half + 1) * 512]) for cc in range(2)], [pTB, wppB])
                    act(lambda e: e.activation(out=sg[:, half * 512:(half + 1) * 512], in_=fg[:, :], func=AF.Sigmoid, scale=c["rs3"]), [fgB, c["rs3B"]], [sgB])
                    c["fp%d" % half] = (fp, fpB)
                c.update(sg=sg, sgB=sgB)

            def stD2(t):
                c = cs[t]
                xc, xcB, sg, sgB = c["xc"], c["xcB"], c["sg"], c["sgB"]
                for half in range(2):
                    fp, fpB = c["fp%d" % half]
                    dve(lambda e: e.tensor_tensor(out=sg[:, half * 512:(half + 1) * 512], in0=fp[:, :], in1=sg[:, half * 512:(half + 1) * 512], op=ALU.mult),
                        [fpB, sgB], [sgB])
                dve(lambda e: e.tensor_tensor(out=xc[:], in0=xc[:], in1=sg[:], op=ALU.add), [xcB, sgB], [xcB])
                junk, junkB = junkR.next()
                sd4, sd4B = rms_sd_c(xc[:], xcB, junk[:], junkB)
                c.update(sg=sg, sgB=sgB, sd4=sd4, sd4B=sd4B)

            def stE(t):
                c = cs[t]
                xc, xcB, sg, sgB = c["xc"], c["xcB"], c["sg"], c["sgB"]
                c["rs4"], c["rs4B"] = rms_recip_c(c["sd4"], c["sd4B"])
                dve(lambda e: e.scalar_tensor_tensor(out=sg[:], in0=xc[:], scalar=c["rs4"], in1=gfin[:], op0=ALU.mult, op1=ALU.mult), [xcB, c["rs4B"], gfinB], [sgB])
                S.dma("sp", lambda e: e.dma_start(out=out_d[t * 128:(t + 1) * 128, :], in_=sg[:]), [sgB], (), track=out_tokens)
                del cs[t]

            for i in range(-3, NT + 1):
                for fn, tt_ in ((stL, i + 3), (stC, i + 1), (stD, i), (stB, i + 2), (stD2, i), (stE, i - 1)):
                    if 0 <= tt_ < NT:
                        fn(tt_)
            S._wait("sp", out_tokens)
            S.barrier()


def _consts():
    ident = np.eye(128, dtype=np.float32)
    ar = np.arange(128)
    triU = (ar[:, None] < ar[None, :]).astype(np.float32)
    maskI = (ar[:, None] <= ar[None, :]).astype(np.float32)
    iota = np.broadcast_to(np.arange(32, dtype=np.float32), (128, 32))
    kaug = np.zeros((128, 2, 128), np.float32)
    kaug[0, 0, :] = ar - 128.0
    kaug[0, 1, :] = ar
    kaug[1, :, :] = 1.0
    qaug = np.zeros((128, 2, 4, 128), np.float32)
    for k in range(2):
        for g in range(4):
            slope = 2.0 ** (-8.0 * (k * 4 + g + 1) / 8.0)
            qaug[0, k, g, :] = 8.0 * slope
            qaug[1, k, g, :] = -8.0 * slope * ar
    mask01 = np.zeros((128, 2, 128), np.float32)
    mask01[:, 0, :] = (ar[:, None] > ar[None, :])
    mask01[:, 1, :] = (ar[:, None] <= ar[None, :])
    return np.ascontiguousarray(np.concatenate([ident, triU, maskI, iota, kaug.reshape(128, 256), qaug.reshape(128, 1024),
                                                mask01.reshape(128, 256)], axis=1))


_NC_CACHE = {}


def _prep(x, p, g_mix, w_in, attn_sinks, g_sgu, b_sgu, w_spatial, b_spatial, w_attn_proj, w_sgu_proj,
          w_out, g_ffn, w_router, b_router, w_gate, b_gate, w_up, b_up, w_down, b_down,
          g_ple, w_ple_gate, w_ple_proj, g_final):
    f = lambda a: np.ascontiguousarray(np.asarray(a, dtype=np.float32))
    x = f(x).reshape(NCORES, T, D)
    p = f(p)[0].reshape(NCORES, T, 256)
    w_in0 = f(w_in)[0]
    qcols = np.concatenate([np.arange((k * 4 + g) * 64, (k * 4 + g + 1) * 64) for g in range(4) for k in range(2)])
    w_in_p = np.ascontiguousarray(np.concatenate([w_in0[:, qcols], w_in0[:, 512:]], axis=1))
    colT = lambda v: f(v).reshape(-1, 128).T
    cvec = np.ascontiguousarray(np.concatenate([colT(g_mix[0]), colT(g_ffn[0]), colT(g_ple[0]), colT(g_sgu[0]), colT(b_sgu[0])], axis=1))
    bgT = np.ascontiguousarray(f(b_gate)[0].reshape(NE, 8, 128).transpose(2, 0, 1).reshape(128, NE * 8))
    buT = np.ascontiguousarray(f(b_up)[0].reshape(NE, 8, 128).transpose(2, 0, 1).reshape(128, NE * 8))
    sinks = f(attn_sinks)[0].reshape(2, 4)
    sink_rows = np.repeat(sinks, 64, axis=0)
    bc = lambda v: np.broadcast_to(f(v).reshape(1, -1), (128, f(v).size))
    rowp = np.ascontiguousarray(np.concatenate([bc(b_router[0]), sink_rows, bc(b_spatial[0]), bc(g_final)], axis=1))
    shared = {
        "w_in": w_in_p, "w_attn_proj": f(w_attn_proj)[0], "w_sgu_proj": f(w_sgu_proj)[0], "w_out": f(w_out)[0],
        "w_router": f(w_router)[0], "w_gate": f(w_gate)[0], "w_up": f(w_up)[0], "w_down": f(w_down)[0],
        "b_down": f(b_down)[0], "w_ple_gate": f(w_ple_gate)[0], "w_ple_proj": f(w_ple_proj)[0],
        "w_spatial": f(w_spatial)[0], "cvec": cvec, "bgT": bgT, "buT": buT, "rowp": rowp, "cst": _consts(),
    }
    return shared, x, p


def kernel(**inputs):
    shared, x, p = _prep(**inputs)
    if "nc" not in _NC_CACHE:
        _NC_CACHE["nc"] = build_nc()
    nc = _NC_CACHE["nc"]
    in_maps = []
    for c in range(NCORES):
        m = dict(shared)
        m["x"] = x[c]
        m["p"] = p[c]
        in_maps.append(m)
    res = run_bass_kernel_spmd(nc, in_maps, core_ids=list(range(NCORES)))
    out = np.stack([np.asarray(r["out"], dtype=np.float32) for r in res.results], axis=0)
    return out.reshape(16, 2048, D)
```

```python
from contextlib import ExitStack
import numpy as np
import concourse.bass as bass
import concourse.mybir as mybir
from concourse.bass_utils import run_bass_kernel_spmd

F32 = mybir.dt.float32
BF16 = mybir.dt.bfloat16
I32 = mybir.dt.int32
U32 = mybir.dt.uint32
AF = mybir.ActivationFunctionType
ALU = mybir.AluOpType

NCORES = 8
D = 1024
T = 4096
NT = 32
TPS = 16
NG = 8
NE = 32
CAP = 640
NB = CAP // 128
NSLOT = NE * CAP
BIGF = 1.0e6
QO, KO, VO, GUO, GVO, GAO, GBO = 0, 512, 640, 768, 1280, 1792, 2816
INW = 3840
RMS_EPS = 1e-6
LN_EPS = 1e-5
CSTW = 128 * 3 + 32 + 256 + 1024 + 256


class Buf:
    __slots__ = ("name", "w", "r")

    def __init__(self, name=""):
        self.name = name
        self.w = None
        self.r = []


class Sched:
    def __init__(self, nc, n_dma_sems=32):
        self.nc = nc
        self.engs = {"pe": nc.tensor, "act": nc.scalar, "dve": nc.vector, "pool": nc.gpsimd, "sp": nc.sync}
        self.sems = {}
        self.cnt = {}
        self._ctx = []
        for k in list(self.engs) + ["d%d" % i for i in range(n_dma_sems)]:
            cm = nc.semaphore("s_" + k)
            self.sems[k] = cm.__enter__()
            self._ctx.append(cm)
            self.cnt[k] = 0
        half = n_dma_sems // 2
        self.dma_keys = {"sp": ["d%d" % i for i in range(half)], "pool": ["d%d" % i for i in range(half, n_dma_sems)]}
        self.dma_rr = {"sp": 0, "pool": 0}
        self.seen = {e: {} for e in self.engs}
        self.pe_pending = False

    def close(self):
        for cm in reversed(self._ctx):
            cm.__exit__(None, None, None)

    def _wait(self, e, deps):
        need = {}
        for d in deps:
            if d is None:
                continue
            k, v = d
            if k == "pe" and e == "pe":
                continue
            if v > need.get(k, 0):
                need[k] = v
        for k, v in need.items():
            if self.seen[e].get(k, 0) >= v:
                continue
            assert v <= self.cnt[k], (e, k, v, self.cnt[k])
            self.engs[e].wait_ge(self.sems[k], v)
            self.seen[e][k] = v

    @staticmethod
    def _deps(reads, writes):
        deps = []
        for b in reads:
            deps.append(b.w)
        for b in writes:
            deps.append(b.w)
            deps.extend(b.r)
        return deps

    def _record(self, tok, reads, writes):
        for b in reads:
            b.r.append(tok)
            if len(b.r) > 64:
                best = {}
                for k, v in b.r:
                    if v > best.get(k, 0):
                        best[k] = v
                b.r = list(best.items())
        for b in writes:
            b.w = tok
            b.r = []

    def op(self, e, fn, reads=(), writes=(), inc=True):
        self._wait(e, self._deps(reads, writes))
        ins = fn(self.engs[e])
        if inc:
            self.cnt[e] += 1
            ins.then_inc(self.sems[e], 1)
            tok = (e, self.cnt[e])
            if e == "pe":
                self.pe_pending = False
        else:
            assert e == "pe"
            tok = (e, self.cnt[e] + 1)
            self.pe_pending = True
        self._record(tok, reads, writes)
        return tok

    def dma(self, q, fn, reads=(), writes=(), track=None):
        keys = self.dma_keys[q]
        k = keys[self.dma_rr[q] % len(keys)]
        self.dma_rr[q] += 1
        deps = self._deps(reads, writes)
        if self.cnt[k] > 0:
            deps.append((k, self.cnt[k]))
        self._wait(q, deps)
        ins = fn(self.engs[q])
        self.cnt[k] += 16
        ins.then_inc(self.sems[k], 16)
        tok = (k, self.cnt[k])
        self._record(tok, reads, writes)
        if track is not None:
            track.append(tok)
        return tok

    def barrier(self):
        assert not self.pe_pending
        for e in self.engs:
            self._wait(e, [(k, v) for k, v in self.cnt.items() if v > 0])


class _Stop(Exception):
    pass


def build_nc(stop=None, dumps=()):
    nc = bass.Bass("TRN2", target_bir_lowering=False)

    def din(name, shape, dt=F32):
        return nc.dram_tensor(name, list(shape), dt, kind="ExternalInput").ap()

    x_d = din("x", [T, D])
    p_d = din("p", [T, 256])
    win_d = din("w_in", [D, INW])
    wa_d = din("w_attn_proj", [512, D])
    wb_d = din("w_sgu_proj", [512, D])
    wout_d = din("w_out", [D, D])
    wr_d = din("w_router", [D, NE])
    wg_d = din("w_gate", [NE, D, D])
    wu_d = din("w_up", [NE, D, D])
    wd_d = din("w_down", [NE, D, D])
    bd_d = din("b_down", [NE, D])
    wpg_d = din("w_ple_gate", [D, D])
    wpp_d = din("w_ple_proj", [256, D])
    ws_d = din("w_spatial", [4, 128, 128])
    cvec_d = din("cvec", [128, 32])
    bgT_d = din("bgT", [128, NE * 8])
    buT_d = din("buT", [128, NE * 8])
    rowp_d = din("rowp", [128, 32 + 4 + 512 + 1024])
    cst_d = din("cst", [128, CSTW])
    out_d = nc.dram_tensor("out", [T, D], F32, kind="ExternalOutput").ap()
    x1_d = nc.dram_tensor("x1_scr", [T, D], F32, kind="Internal").ap()
    xs_d = nc.dram_tensor("xs_scr", [NSLOT + 128, D], BF16, kind="Internal").ap()
    ys_d = nc.dram_tensor("ys_scr", [NSLOT + 128, D], F32, kind="Internal").ap()

    S = Sched(nc)
    uid = [0]

    def alloc(es, shape, dt, name="t", side=None):
        uid[0] += 1
        if side is None:
            t = es.enter_context(nc.sbuf_tensor("%s_%d" % (name, uid[0]), list(shape), dt))
        else:
            t = es.enter_context(nc.sbuf_tensor("%s_%d" % (name, uid[0]), list(shape), dt, side=side))
        return t, Buf(name)

    def palloc(es, shape, dt, name="p"):
        uid[0] += 1
        t = es.enter_context(nc.psum_tensor("%s_%d" % (name, uid[0]), list(shape), dt))
        return t, Buf(name)

    class Ring:
        def __init__(self, items):
            self.items = items
            self.i = 0

        def next(self):
            it = self.items[self.i % len(self.items)]
            self.i += 1
            return it

    def pe(fn, r=(), w=(), inc=True):
        return S.op("pe", fn, r, w, inc=inc)

    def act(fn, r=(), w=()):
        return S.op("act", fn, r, w)

    def dve(fn, r=(), w=()):
        return S.op("dve", fn, r, w)

    def pool(fn, r=(), w=()):
        return S.op("pool", fn, r, w)

    def mmgroup(out_ap, outB, pairs, rB, first=True, last=True):
        n = len(pairs)
        for i, (l, r) in enumerate(pairs):
            st = first and i == 0
            sp_ = last and i == n - 1
            pe(lambda e: e.matmul(out_ap, lhsT=l, rhs=r, start=st, stop=sp_), rB, [outB], inc=(i == n - 1))

    out_tokens = []
    dump_names = []

    def dump(name, ap, B, cols):
        if name not in dumps:
            return
        dd = nc.dram_tensor("dbg_" + name, [128, cols], F32, kind="ExternalOutput").ap()
        dump_names.append(name)
        with nc.sbuf_tensor("dbgst_" + name, [128, cols], F32) as stg:
            sB = Buf("stg")
            dve(lambda e: e.tensor_copy(out=stg[:], in_=ap), [B], [sB])
            S.dma("sp", lambda e: e.dma_start(out=dd, in_=stg[:]), [sB], ())
            S.barrier()

    def check_stop(tag):
        if stop == tag:
            raise _Stop()

    try:
        _body(locals())
    except _Stop:
        pass
    S.barrier()
    S.close()
    return nc


def _body(L):
    globals_ = L
    nc = L["nc"]; S = L["S"]; alloc = L["alloc"]; palloc = L["palloc"]; Ring = L["Ring"]
    pe = L["pe"]; act = L["act"]; dve = L["dve"]; pool = L["pool"]; mmgroup = L["mmgroup"]
    out_tokens = L["out_tokens"]; dump = L["dump"]; check_stop = L["check_stop"]
    x_d = L["x_d"]; p_d = L["p_d"]; win_d = L["win_d"]; wa_d = L["wa_d"]; wb_d = L["wb_d"]; wout_d = L["wout_d"]
    wr_d = L["wr_d"]; wg_d = L["wg_d"]; wu_d = L["wu_d"]; wd_d = L["wd_d"]; bd_d = L["bd_d"]; wpg_d = L["wpg_d"]
    wpp_d = L["wpp_d"]; ws_d = L["ws_d"]; cvec_d = L["cvec_d"]; bgT_d = L["bgT_d"]; buT_d = L["buT_d"]
    rowp_d = L["rowp_d"]; cst_d = L["cst_d"]; out_d = L["out_d"]; x1_d = L["x1_d"]; xs_d = L["xs_d"]; ys_d = L["ys_d"]

    with ExitStack() as glob:
        ident16, ident16B = alloc(glob, [128, 128], BF16, "ident16")
        ident32, ident32B = alloc(glob, [128, 128], F32, "ident32")
        ones16, ones16B = alloc(glob, [128, 128], BF16, "ones16")
        cvec, cvecB = alloc(glob, [128, 32], F32, "cvec")
        bgT, bgTB = alloc(glob, [128, NE * 8], F32, "bgT")
        bu1T, bu1TB = alloc(glob, [128, NE * 8], F32, "bu1T")
        gates_all, _ = alloc(glob, [128, NT, 4], F32, "gates")
        slots_all, _ = alloc(glob, [128, NT, 4], I32, "slots")
        gateB = [Buf("gate%d" % t) for t in range(NT)]
        slotB = [Buf("slot%d" % t) for t in range(NT)]
        stat, _ = alloc(glob, [128, 128], F32, "stat")
        statR = Ring([(stat[:, i:i + 1], Buf("stat%d" % i)) for i in range(128)])

        S.dma("sp", lambda e: e.dma_start(out=cvec[:], in_=cvec_d), (), [cvecB])
        S.dma("sp", lambda e: e.dma_start(out=bgT[:], in_=bgT_d), (), [bgTB])
        S.dma("sp", lambda e: e.dma_start(out=bu1T[:], in_=buT_d), (), [bu1TB])
        dve(lambda e: e.tensor_scalar_add(out=bu1T[:], in0=bu1T[:], scalar1=1.0), [bu1TB], [bu1TB])
        pool(lambda e: e.memset(ones16[:], 1.0), (), [ones16B])
        mhalf, mhalfB = alloc(glob, [128, 1], F32, "mhalf")
        pool(lambda e: e.memset(mhalf[:], -0.5), (), [mhalfB])
        pool(lambda e: e.memset(slots_all[:], 0), (), slotB)
        pool(lambda e: e.memset(gates_all[:], 0.0), (), gateB)

        def rms_sd(src_ap, srcB, junk_ap, junkB):
            ss, ssB = statR.next()
            act(lambda e: e.activation(out=junk_ap, in_=src_ap, func=AF.Square, accum_out=ss), [srcB], [junkB, ssB])
            return ss, ssB

        def rms_sd_c(src_ap, srcB, junk_ap, junkB):
            ss, ssB = rms_sd(src_ap, srcB, junk_ap, junkB)
            sd, sdB = statR.next()
            act(lambda e: e.activation(out=sd, in_=ss, func=AF.Sqrt, bias=RMS_EPS, scale=1.0 / D), [ssB], [sdB])
            return sd, sdB

        def rms_recip_c(sd, sdB):
            rs, rsB = statR.next()
            dve(lambda e: e.reciprocal(out=rs, in_=sd), [sdB], [rsB])
            return rs, rsB

        def rms_recip(ss, ssB, use_pool=True):
            if not use_pool:
                sd, sdB = statR.next()
                act(lambda e: e.activation(out=sd, in_=ss, func=AF.Sqrt, bias=RMS_EPS, scale=1.0 / D), [ssB], [sdB])
                rs, rsB = statR.next()
                dve(lambda e: e.reciprocal(out=rs, in_=sd), [sdB], [rsB])
                return rs, rsB
            v, vB = statR.next()
            dve(lambda e: e.tensor_scalar(out=v, in0=ss, scalar1=1.0 / D, scalar2=RMS_EPS, op0=ALU.mult, op1=ALU.add), [ssB], [vB])
            rs, rsB = statR.next()
            pool(lambda e: e.tensor_tensor(out=rs, in0=v, in1=mhalf[:, 0:1], op=ALU.pow), [vB, mhalfB], [rsB])
            return rs, rsB

        def rms_rstd(src_ap, srcB, junk_ap, junkB):
            sd, sdB = rms_sd(src_ap, srcB, junk_ap, junkB)
            return rms_recip(sd, sdB)

        pbr = ExitStack()
        pre = {}
        with ExitStack() as pa:
            pw = ExitStack()
            win, _ = alloc(pw, [128, 8, INW], BF16, "win", side="right")
            WIN_GROUPS = [(GVO, 512), (QO, 512), (KO, 128), (GUO, 512), (VO, 128), (GAO, 512), (GBO, 512), (GAO + 512, 512), (GBO + 512, 512)]
            winG = [Buf("win_c%d" % c0) for (c0, _) in WIN_GROUPS]

            def winB_for(col0):
                for (c0, w), b in zip(WIN_GROUPS, winG):
                    if c0 <= col0 < c0 + w:
                        return [b]
                raise AssertionError(col0)
            wa, _ = alloc(pa, [128, 4, D], BF16, "wa")
            waB = [Buf("wa0"), Buf("wa1")]
            wb, wbB = alloc(pa, [128, 4, D], BF16, "wb")
            wout, woutB = alloc(pa, [128, 8, D], BF16, "wout")
            wr, wrB = alloc(pa, [128, 8, NE], F32, "wr")
            rowp, rowpB = alloc(pa, [128, 32 + 4 + 512], F32, "rowp")
            kaug, kaugB = alloc(pa, [2, 2, 128], BF16, "kaug")
            qaug, qaugB = alloc(pa, [2, 2, 512], BF16, "qaug")
            mask01, mask01B = alloc(pa, [128, 2, 128], BF16, "mask01")
            triU, triUB = alloc(pa, [128, 128], BF16, "triU")
            iota, iotaB = alloc(pa, [128, 32], F32, "iota")
            wsT, wsTB = alloc(pa, [128, 4, 128], BF16, "wsT")
            comb, combB = alloc(pa, [128, 4, 128], F32, "comb")
            cnt, cntB = alloc(pa, [128, 32], F32, "cnt")

            tpR = Ring([palloc(pa, [128, 1024], BF16, "tp") for _ in range(2)])
            fR = Ring([palloc(pa, [128, 512], F32, "f") for _ in range(6)])

            S.dma("sp", lambda e: e.dma_start(out=wr[:], in_=wr_d.rearrange("(c p) o -> p c o", p=128)), (), [wrB])
            S.dma("sp", lambda e: e.dma_start(out=rowp[:], in_=rowp_d[:, 0:32 + 4 + 512]), (), [rowpB])
            brt = rowp[:, 0:32]
            expsink = rowp[:, 32:36]
            bspat = rowp[:, 36:36 + 512].rearrange("p (g t) -> p g t", g=4)

            with ExitStack() as st:
                cst, cstB = alloc(st, [128, CSTW], F32, "cst")
                wsn, wsnB = alloc(st, [128, 4, 128], F32, "wsn")
                S.dma("sp", lambda e: e.dma_start(out=cst[:], in_=cst_d), (), [cstB])
                S.dma("sp", lambda e: e.dma_start(out=wsn[:], in_=ws_d.rearrange("g t s -> t g s")), (), [wsnB])
                pool(lambda e: e.memset(cnt[:], 0.0), (), [cntB])
                zf, zfB = alloc(st, [128, D], F32, "zf")
                pool(lambda e: e.memset(zf[:], 0.0), (), [zfB])
                S.dma("sp", lambda e: e.dma_start(out=ys_d[NSLOT:NSLOT + 128, :], in_=zf[:]), [zfB], ())
                dve(lambda e: e.tensor_copy(out=ident32[:], in_=cst[:, 0:128]), [cstB], [ident32B])
                dve(lambda e: e.tensor_copy(out=ident16[:], in_=cst[:, 0:128]), [cstB], [ident16B])
                dve(lambda e: e.tensor_copy(out=triU[:], in_=cst[:, 128:256]), [cstB], [triUB])
                dve(lambda e: e.tensor_copy(out=iota[:], in_=cst[:, 384:416]), [cstB], [iotaB])
                dve(lambda e: e.tensor_copy(out=kaug[:], in_=cst[0:2, 416:672].rearrange("p (j n) -> p j n", j=2)), [cstB], [kaugB])
                dve(lambda e: e.tensor_copy(out=qaug[:], in_=cst[0:2, 672:1696].rearrange("p (k n) -> p k n", k=2)), [cstB], [qaugB])
                dve(lambda e: e.tensor_copy(out=mask01[:], in_=cst[:, 1696:1952].rearrange("p (j n) -> p j n", j=2)), [cstB], [mask01B])
                act(lambda e: e.activation(out=expsink, in_=expsink, func=AF.Exp), [rowpB], [rowpB])
                for g in range(4):
                    f, fB = fR.next()
                    pe(lambda e: e.transpose(out=f[:, 0:128], in_=wsn[:, g, :], identity=ident32[:]), [wsnB, ident32B], [fB])
                    dve(lambda e: e.tensor_tensor(out=wsT[:, g, :], in0=f[:, 0:128], in1=cst[:, 256:384], op=ALU.mult), [fB, cstB], [wsTB])
                f, fB = fR.next()
                mmgroup(f[:, :], fB, [(ones16[:, :], wsT[:, :, :].rearrange("p g t -> p (g t)"))], [ones16B, wsTB])
                for g in range(4):
                    dve(lambda e: e.scalar_tensor_tensor(out=comb[:, g, :], in0=f[:, g * 128:(g + 1) * 128], scalar=cvec[:, 28 + g:29 + g],
                                                         in1=bspat[:, g, :], op0=ALU.mult, op1=ALU.add), [fB, cvecB, rowpB], [combB])
                S.barrier()
            check_stop("setup")
            for (c0, w), b in zip(WIN_GROUPS, winG):
                S.dma("pool", lambda e: e.dma_start(out=win[:, :, c0:c0 + w], in_=win_d[:, c0:c0 + w].rearrange("(kc p) f -> p kc f", p=128)), (), [b])
            for k in range(2):
                S.dma("pool", lambda e: e.dma_start(
                    out=wa[k * 64:(k + 1) * 64, :, :],
                    in_=wa_d[k * 256:(k + 1) * 256, :].rearrange("(g hd) o -> hd g o", g=4)), (), [waB[k]])
            S.dma("pool", lambda e: e.dma_start(out=wb[:], in_=wb_d.rearrange("(c p) o -> p c o", p=128)), (), [wbB])
            S.dma("pool", lambda e: e.dma_start(out=wout[:], in_=wout_d.rearrange("(c p) o -> p c o", p=128)), (), [woutB])
            xR = Ring([alloc(pa, [128, D], F32, "x") for _ in range(4)])
            xnR = Ring([alloc(pa, [128, D], BF16, "xn") for _ in range(2)])
            hT, hTB = alloc(pa, [128, 8, 512], BF16, "hT")
            qT, qTB = alloc(pa, [128, 4, 512], BF16, "qT")
            kT, kTB = alloc(pa, [128, 5 * 128], BF16, "kT")
            vS, vSB = alloc(pa, [128, 5, 128], BF16, "vS")
            guT, guTB = alloc(pa, [128, 4, 512], BF16, "guT")
            gvgR = Ring([alloc(pa, [128, 512], F32, "gvg") for _ in range(2)])
            vln, vlnB = alloc(pa, [128, 4, 512], BF16, "vln")
            bnR = Ring([alloc(pa, [128, 8], F32, "bnst") for _ in range(2)])
            ptR = Ring([alloc(pa, [128, 2, 2, 512], BF16, "pt") for _ in range(2)])
            attnT, attnTB = alloc(pa, [128, 4, 512], BF16, "attnT")
            rden, rdenB = alloc(pa, [128, 512], F32, "rden")
            sguT, sguTB = alloc(pa, [128, 4, 512], BF16, "sguT")
            sgtmp, sgtmpB = alloc(pa, [128, 512], F32, "sgtmp")
            sigAR = Ring([alloc(pa, [128, 512], F32, "sigA") for _ in range(1)])
            sigBR = Ring([alloc(pa, [128, 512], F32, "sigB") for _ in range(1)])
            mrgT, mrgTB = alloc(pa, [128, 8, 512], BF16, "mrgT")
            hn4 = [alloc(pa, [128, D], BF16, "hn") for _ in range(4)]
            xgT, xgTB = alloc(pa, [128, 8, 128], F32, "xgT")
            lg4, lg4B = alloc(pa, [128, 4, 32], F32, "lg4")
            mx8, mx8B = alloc(pa, [128, 4, 8], F32, "mx8")
            ix8, ix8B = alloc(pa, [128, 4, 8], U32, "ix8")
            ixf, ixfB = alloc(pa, [128, 4, 4], F32, "ixf")
            ex4, ex4B = alloc(pa, [128, 4, 4], F32, "ex4")
            sm4, sm4B = alloc(pa, [128, 4], F32, "sm4")
            oh, ohB = alloc(pa, [128, 4, 4, 32], F32, "oh")
            msk16, msk16B = alloc(pa, [128, 4, 32], BF16, "msk16")
            posf, posfB = alloc(pa, [128, 4, 32], F32, "posf")
            ohp, ohpB = alloc(pa, [128, 4, 4, 32], F32, "ohp")
            p4, p4B = alloc(pa, [128, 4, 4], F32, "p4")
            slf, slfB = alloc(pa, [128, 4, 4], F32, "slf")
            ovf, ovfB = alloc(pa, [128, 4, 4], F32, "ovf")

            gmixT = cvec[:, 0:8]
            gffnT = cvec[:, 8:16]

            pending_route = []
            s1st = {}

            def route_a(t0):
                tl = list(range(t0, t0 + 4))
                for i in range(4):
                    dve(lambda e: e.max(out=mx8[:, i, :], in_=lg4[:, i, :]), [lg4B], [mx8B])
                    dve(lambda e: e.max_index(out=ix8[:, i, :], in_max=mx8[:, i, :], in_values=lg4[:, i, :]), [lg4B, mx8B], [ix8B])
                dve(lambda e: e.tensor_copy(out=ixf[:], in_=ix8[:, :, 0:4]), [ix8B], [ixfB])
                dve(lambda e: e.tensor_tensor(out=ex4[:], in0=mx8[:, :, 0:4], in1=mx8[:, :, 0:1].to_broadcast([128, 4, 4]), op=ALU.subtract), [mx8B], [ex4B])
                act(lambda e: e.activation(out=ex4[:], in_=ex4[:], func=AF.Exp), [ex4B], [ex4B])
                dve(lambda e: e.reduce_sum(out=sm4[:], in_=ex4[:], axis=mybir.AxisListType.X), [ex4B], [sm4B])
                dve(lambda e: e.reciprocal(out=sm4[:], in_=sm4[:]), [sm4B], [sm4B])
                dve(lambda e: e.tensor_tensor(out=gates_all[:, t0:t0 + 4, :], in0=ex4[:], in1=sm4[:, :].unsqueeze(2).to_broadcast([128, 4, 4]), op=ALU.mult),
                    [ex4B, sm4B], [gateB[t] for t in tl])
                dve(lambda e: e.tensor_tensor(out=oh[:], in0=iota[:, :].unsqueeze(1).unsqueeze(1).to_broadcast([128, 4, 4, 32]),
                                              in1=ixf[:, :, :].unsqueeze(3).to_broadcast([128, 4, 4, 32]), op=ALU.is_equal), [iotaB, ixfB], [ohB])
                with nc.allow_low_precision(reason="0/1 mask sums are exact in bf16"):
                    dve(lambda e: e.tensor_reduce(out=msk16[:], in_=oh[:, :, :, :].rearrange("p i k e -> p i e k"), axis=mybir.AxisListType.X, op=ALU.add),
                        [ohB], [msk16B])

            def route_b(t0):
                tl = list(range(t0, t0 + 4))
                f, fB = fR.next()
                for i in range(4):
                    pairs = [(triU[:, :], msk16[:, i, :])] + [(ones16[:, :], msk16[:, i2, :]) for i2 in range(i)]
                    mmgroup(f[:, i * 32:(i + 1) * 32], fB, pairs, [triUB, ones16B, msk16B])
                mmgroup(f[:, 128:160], fB, [(ones16[:, :], msk16[:, i, :]) for i in range(4)], [ones16B, msk16B])
                dve(lambda e: e.tensor_tensor(out=posf[:], in0=f[:, 0:128].rearrange("p (i e) -> p i e", i=4),
                                              in1=cnt[:, :].unsqueeze(1).to_broadcast([128, 4, 32]), op=ALU.add), [fB, cntB], [posfB])
                dve(lambda e: e.tensor_tensor(out=cnt[:], in0=f[:, 128:160], in1=cnt[:], op=ALU.add), [fB, cntB, posfB], [cntB])
                dve(lambda e: e.tensor_tensor(out=ohp[:], in0=oh[:], in1=posf[:, :, :].unsqueeze(2).to_broadcast([128, 4, 4, 32]), op=ALU.mult), [ohB, posfB], [ohpB])
                dve(lambda e: e.reduce_sum(out=p4[:], in_=ohp[:], axis=mybir.AxisListType.X), [ohpB], [p4B])
                dve(lambda e: e.scalar_tensor_tensor(out=slf[:], in0=ixf[:], scalar=float(CAP), in1=p4[:], op0=ALU.mult, op1=ALU.add), [ixfB, p4B], [slfB])
                dve(lambda e: e.tensor_scalar(out=ovf[:], in0=p4[:], scalar1=float(CAP), scalar2=BIGF, op0=ALU.is_ge, op1=ALU.mult), [p4B], [ovfB])
                dve(lambda e: e.tensor_tensor(out=slf[:], in0=slf[:], in1=ovf[:], op=ALU.max), [slfB, ovfB], [slfB])
                dve(lambda e: e.tensor_scalar(out=slf[:], in0=slf[:], scalar1=float(NSLOT), scalar2=0.0, op0=ALU.min, op1=ALU.max), [slfB], [slfB])
                dve(lambda e: e.tensor_copy(out=slots_all[:, t0:t0 + 4, :], in_=slf[:]), [slfB], [slotB[t] for t in tl])
                dve(lambda e: e.tensor_copy(out=ovf[:], in_=slots_all[:, t0:t0 + 4, :]), [slotB[t] for t in tl], [ovfB])
                if t0 == 0:
                    dump("lg", lg4[:, 0, :], lg4B, 32)
                    dump("gates", gates_all[:, 0, :], gateB[0], 4)
                    dump("slf", slf[:, 0, :], slfB, 4)
                    dump("posf", posf[:, 0, :], posfB, 32)
                for i in range(4):
                    hn, hnB = hn4[i]
                    for k in range(4):
                        S.dma("pool", lambda e: e.indirect_dma_start(
                            out=xs_d, out_offset=bass.IndirectOffsetOnAxis(ap=slots_all[:, t0 + i, k:k + 1], axis=0),
                            in_=hn[:, :], in_offset=None), [hnB, slotB[t0 + i], ovfB], ())

            for gi in range(NG):
                t0 = gi * 4
                def s1_load(tg, i):
                    t = tg * 4 + i
                    xt, xtB = xR.next()
                    S.dma("sp", lambda e: e.dma_start(out=xt[:], in_=x_d[t * 128:(t + 1) * 128, :]), (), [xtB])
                    s1st[(tg, i)] = dict(xt=xt, xtB=xtB)

                def s1a(tg, i):
                    c = s1st[(tg, i)]
                    xt, xtB = c["xt"], c["xtB"]
                    xn, xnB = xnR.next()
                    rs, rsB = rms_rstd(xt[:], xtB, xn[:], xnB)
                    dve(lambda e: e.tensor_scalar_mul(out=xn[:], in0=xt[:], scalar1=rs), [xtB, rsB], [xnB])
                    c.update(xn=xn, xnB=xnB)

                def s1b(tg, i):
                    c = s1st.pop((tg, i))
                    xn, xnB = c["xn"], c["xnB"]
                    tp, tpB = tpR.next()
                    for kc in range(8):
                        pe(lambda e: e.transpose(out=tp[:, kc * 128:(kc + 1) * 128], in_=xn[:, kc * 128:(kc + 1) * 128], identity=ident16[:]),
                           [xnB, ident16B], [tpB], inc=(kc == 7))
                    dve(lambda e: e.tensor_tensor(out=hT[:, :, i * 128:(i + 1) * 128], in0=tp[:, :].rearrange("p (c t) -> p c t", c=8),
                                                  in1=gmixT.unsqueeze(2).to_broadcast([128, 8, 128]), op=ALU.mult), [tpB, cvecB], [hTB])

                if gi == 0:
                    for i in range(4):
                        s1_load(0, i)
                        s1a(0, i)
                        s1b(0, i)
                if gi == 0:
                    for kc in range(8):
                        dump("hT%d" % kc, hT[:, kc, :], hTB, 512)
                    check_stop("g0s1")

                def inproj_fm(col0, evac):
                    f, fB = fR.next()
                    mmgroup(f[:, :], fB, [(win[:, kc, col0:col0 + 128], hT[:, kc, :]) for kc in range(8)], [hTB] + winB_for(col0))
                    evac(f, fB)

                for t0r in pending_route:
                    route_a(t0r)
                gst = {}

                def gv_mm(i):
                    f, fB = fR.next()
                    mmgroup(f[:, :], fB, [(hT[:, kc, i * 128:(i + 1) * 128], win[:, kc, GVO:GVO + 512]) for kc in range(8)], [hTB] + winB_for(GVO))
                    gvg, gvgB = gvgR.next()
                    act(lambda e: e.activation(out=gvg[:], in_=f[:, :], func=AF.Gelu_apprx_tanh), [fB], [gvgB])
                    gst[i] = (gvg, gvgB)

                def gv_ln(i):
                    gvg, gvgB = gst[i]
                    bn, bnB = bnR.next()
                    dve(lambda e: e.bn_stats(out=bn[:, 0:6], in_=gvg[:]), [gvgB], [bnB])
                    dve(lambda e: e.bn_aggr(out=bn[:, 6:8], in_=bn[:, 0:6]), [bnB], [bnB])
                    sd, sdB = statR.next()
                    dve(lambda e: e.tensor_scalar_add(out=sd, in0=bn[:, 7:8], scalar1=LN_EPS), [bnB], [sdB])
                    rs, rsB = statR.next()
                    pool(lambda e: e.tensor_tensor(out=rs, in0=sd, in1=mhalf[:, 0:1], op=ALU.pow), [sdB, mhalfB], [rsB])
                    dve(lambda e: e.tensor_scalar(out=vln[:, i, :], in0=gvg[:], scalar1=bn[:, 6:7], scalar2=rs, op0=ALU.subtract, op1=ALU.mult),
                        [gvgB, bnB, rsB], [vlnB])

                for i in range(5):
                    if i < 4:
                        gv_mm(i)
                    if i >= 1:
                        gv_ln(i - 1)

                for g in range(4):
                    inproj_fm(QO + g * 128, lambda f, fB: act(lambda e: e.copy(out=qT[:, g, :], in_=f[:, :]), [fB], [qTB]))
                inproj_fm(KO, lambda f, fB: act(lambda e: e.copy(out=kT[:, 128:640], in_=f[:, :]), [fB], [kTB]))
                while pending_route:
                    route_b(pending_route.pop(0))
                for c in range(4):
                    inproj_fm(GUO + c * 128, lambda f, fB: act(lambda e: e.activation(out=guT[:, c, :], in_=f[:, :], func=AF.Gelu_apprx_tanh), [fB], [guTB]))
                f, fB = fR.next()
                for i in range(4):
                    mmgroup(f[:, i * 128:(i + 1) * 128], fB, [(hT[:, kc, i * 128:(i + 1) * 128], win[:, kc, VO:VO + 128]) for kc in range(8)], [hTB] + winB_for(VO))
                act(lambda e: e.copy(out=vS[:, 1:5, :], in_=f[:, :].rearrange("p (i c) -> p i c", i=4)), [fB], [vSB])

                if gi == 0:
                    for g in range(4):
                        dump("qT%d" % g, qT[:, g, :], qTB, 512)
                        dump("guT%d" % g, guT[:, g, :], guTB, 512)
                        dump("vln%d" % g, vln[:, g, :], vlnB, 512)
                    dump("kT", kT[:, :], kTB, 640)
                    dump("vS", vS[:, :, :].rearrange("p a b -> p (a b)"), vSB, 640)
                    check_stop("g0s2")
                for cg in range(4):
                    f, fB = fR.next()
                    for i in range(4):
                        mmgroup(f[:, i * 128:(i + 1) * 128], fB, [(vln[:, i, cg * 128:(cg + 1) * 128], wsT[:, cg, :])], [vlnB, wsTB])
                    dve(lambda e: e.scalar_tensor_tensor(out=sgtmp[:, :].rearrange("p (i t) -> p i t", i=4), in0=f[:, :].rearrange("p (i t) -> p i t", i=4),
                                                         scalar=cvec[:, 24 + cg:25 + cg], in1=comb[:, cg, :].unsqueeze(1).to_broadcast([128, 4, 128]),
                                                         op0=ALU.mult, op1=ALU.add), [fB, cvecB, combB], [sgtmpB])
                    dve(lambda e: e.tensor_tensor(out=sguT[:, cg, :], in0=sgtmp[:], in1=guT[:, cg, :], op=ALU.mult), [sgtmpB, guTB], [sguTB])

                ast = {}

                def att_scores(i):
                    t = t0 + i
                    n = t % TPS
                    js = [(0, i), (1, i + 1)] if n > 0 else [(1, i + 1)]
                    pt, ptB = ptR.next()
                    for k in range(2):
                        for (j, slot) in js:
                            f, fB = fR.next()
                            mmgroup(f[:, :], fB, [(kT[k * 64:(k + 1) * 64, slot * 128:(slot + 1) * 128], qT[k * 64:(k + 1) * 64, :, i * 128:(i + 1) * 128]),
                                                  (kaug[0:2, j, :], qaug[0:2, k, :])], [kTB, qTB, kaugB, qaugB])
                            act(lambda e: e.activation(out=pt[:, k, j, :], in_=f[:, :], func=AF.Exp, scale=0.125), [fB], [ptB])
                            dve(lambda e: e.tensor_tensor(out=pt[:, k, j, :].rearrange("p (g q) -> p g q", g=4),
                                                           in0=pt[:, k, j, :].rearrange("p (g q) -> p g q", g=4),
                                                           in1=mask01[:, j, :].unsqueeze(1).to_broadcast([128, 4, 128]), op=ALU.mult), [ptB, mask01B], [ptB])
                    ast[i] = (js, pt, ptB)

                def att_pv(i):
                    js, pt, ptB = ast[i]
                    pv, pvB = fR.next()
                    dn, dnB = fR.next()
                    for k in range(2):
                        mmgroup(pv[k * 64:(k + 1) * 64, :], pvB, [(vS[:, slot, k * 64:(k + 1) * 64], pt[:, k, j, :]) for (j, slot) in js], [vSB, ptB])
                    for k in range(2):
                        mmgroup(dn[k * 64:(k + 1) * 64, :], dnB, [(ones16[:, 0:64], pt[:, k, j, :]) for (j, slot) in js], [ones16B, ptB])
                    dve(lambda e: e.tensor_tensor(out=rden[:, :].rearrange("p (g q) -> p g q", g=4), in0=dn[:, :].rearrange("p (g q) -> p g q", g=4),
                                                  in1=expsink.unsqueeze(2).to_broadcast([128, 4, 128]), op=ALU.add), [dnB, rowpB], [rdenB])
                    act(lambda e: e.activation(out=rden[:], in_=rden[:], func=AF.Ln), [rdenB], [rdenB])
                    act(lambda e: e.activation(out=rden[:], in_=rden[:], func=AF.Exp, scale=-1.0), [rdenB], [rdenB])
                    dve(lambda e: e.tensor_tensor(out=attnT[:, :, i * 128:(i + 1) * 128], in0=pv[:, :].rearrange("p (g q) -> p g q", g=4),
                                                  in1=rden[:, :].rearrange("p (g q) -> p g q", g=4), op=ALU.mult), [pvB, rdenB], [attnTB])

                for i in range(5):
                    if i < 4:
                        att_scores(i)
                    if i >= 1:
                        att_pv(i - 1)
                pool(lambda e: e.tensor_copy(out=kT[:, 0:128], in_=kT[:, 512:640]), [kTB], [kTB])
                pool(lambda e: e.tensor_copy(out=vS[:, 0, :], in_=vS[:, 4, :]), [vSB], [vSB])

                if gi == 0:
                    for g in range(4):
                        dump("attnT%d" % g, attnT[:, g, :], attnTB, 512)
                        dump("sguT%d" % g, sguT[:, g, :], sguTB, 512)
                    check_stop("g0s3")
                for oc in range(8):
                    sigA, sigAB = sigAR.next()
                    sigB_, sigBB = sigBR.next()
                    inproj_fm(GAO + oc * 128, lambda f, fB: act(lambda e: e.activation(out=sigA[:], in_=f[:, :], func=AF.Sigmoid), [fB], [sigAB]))
                    inproj_fm(GBO + oc * 128, lambda f, fB: act(lambda e: e.activation(out=sigB_[:], in_=f[:, :], func=AF.Sigmoid), [fB], [sigBB]))
                    f, fB = fR.next()
                    mmgroup(f[:, :], fB, [(wa[:, g, oc * 128:(oc + 1) * 128], attnT[:, g, :]) for g in range(4)], waB + [attnTB])
                    dve(lambda e: e.tensor_tensor(out=sigA[:], in0=f[:, :], in1=sigA[:], op=ALU.mult), [fB, sigAB], [sigAB])
                    f, fB = fR.next()
                    mmgroup(f[:, :], fB, [(wb[:, c, oc * 128:(oc + 1) * 128], sguT[:, c, :]) for c in range(4)], [wbB, sguTB])
                    dve(lambda e: e.tensor_tensor(out=sigB_[:], in0=f[:, :], in1=sigB_[:], op=ALU.mult), [fB, sigBB], [sigBB])
                    dve(lambda e: e.tensor_tensor(out=mrgT[:, oc, :], in0=sigA[:], in1=sigB_[:], op=ALU.add), [sigAB, sigBB], [mrgTB])

                if gi == 0:
                    for kc in range(8):
                        dump("mrgT%d" % kc, mrgT[:, kc, :], mrgTB, 512)
                    check_stop("g0s4")
                if gi == NG - 1:
                    pw.close()
                    for nm, src in (("wg", wg_d), ("wu", wu_d), ("wd", wd_d)):
                        t_, _b = alloc(pbr, [128, 8, D], BF16, nm + "0", side="right")
                        wB2 = [Buf(nm + "0h0"), Buf(nm + "0h1")]
                        for hh in range(2):
                            S.dma("pool", lambda eng: eng.dma_start(out=t_[:, hh * 4:(hh + 1) * 4, :],
                                                                    in_=src[0, hh * 512:(hh + 1) * 512, :].rearrange("(c p) o -> p c o", p=128)),
                                  (), [wB2[hh]] + winG)
                        pre[nm] = (t_, wB2)
                s5 = {}

                def s5_op(i):
                    t = t0 + i
                    xr, xrB = xR.next()
                    S.dma("sp", lambda e: e.dma_start(out=xr[:], in_=x_d[t * 128:(t + 1) * 128, :]), (), [xrB])
                    for half in range(2):
                        f, fB = fR.next()
                        mmgroup(f[:, :], fB, [(mrgT[:, kc, i * 128:(i + 1) * 128], wout[:, kc, half * 512:(half + 1) * 512]) for kc in range(8)], [mrgTB, woutB])
                        dve(lambda e: e.tensor_tensor(out=xr[:, half * 512:(half + 1) * 512], in0=f[:, :], in1=xr[:, half * 512:(half + 1) * 512], op=ALU.add),
                            [fB, xrB], [xrB])
                    S.dma("sp", lambda e: e.dma_start(out=x1_d[t * 128:(t + 1) * 128, :], in_=xr[:]), [xrB], ())
                    hn, hnB = hn4[i]
                    sd2, sd2B = rms_sd(xr[:], xrB, hn[:], hnB)
                    s5[i] = dict(xr=xr, xrB=xrB, hn=hn, hnB=hnB, sd2=sd2, sd2B=sd2B)

                def s5_tr(i):
                    c = s5[i]
                    xr, xrB, hn, hnB = c["xr"], c["xrB"], c["hn"], c["hnB"]
                    rs2, rs2B = rms_recip(c["sd2"], c["sd2B"])
                    c["rs2"], c["rs2B"] = rs2, rs2B
                    act(lambda e: e.activation(out=hn[:], in_=xr[:], func=AF.Copy, scale=rs2), [xrB, rs2B], [hnB])
                    for hh in range(2):
                        f, fB = fR.next()
                        for c4 in range(4):
                            kc = hh * 4 + c4
                            pe(lambda e: e.transpose(out=f[:, c4 * 128:(c4 + 1) * 128], in_=xr[:, kc * 128:(kc + 1) * 128], identity=ident32[:]),
                               [xrB, ident32B], [fB], inc=(c4 == 3))
                        dve(lambda e: e.tensor_tensor(out=xgT[:, hh * 4:(hh + 1) * 4, :], in0=f[:, :].rearrange("p (c t) -> p c t", c=4),
                                                      in1=gffnT[:, hh * 4:(hh + 1) * 4].unsqueeze(2).to_broadcast([128, 4, 128]), op=ALU.mult),
                            [fB, cvecB], [xgTB])

                def s5_lg(i):
                    c = s5[i]
                    f, fB = fR.next()
                    mmgroup(f[:, 0:32], fB, [(xgT[:, kc, :], wr[:, kc, :]) for kc in range(8)], [xgTB, wrB])
                    dve(lambda e: e.scalar_tensor_tensor(out=lg4[:, i, :], in0=f[:, 0:32], scalar=c["rs2"], in1=brt, op0=ALU.mult, op1=ALU.add),
                        [fB, c["rs2B"], rowpB], [lg4B])

                if gi + 1 < NG:
                    s1_load(gi + 1, 0)
                for j in range(6):
                    if j < 4:
                        s5_op(j)
                    if 2 <= j:
                        s5_lg(j - 2)
                    if 1 <= j <= 4:
                        s5_tr(j - 1)
                    if gi + 1 < NG:
                        if 1 <= j <= 4:
                            s1b(gi + 1, j - 1)
                        if j < 4:
                            s1a(gi + 1, j)
                        if j + 1 < 4:
                            s1_load(gi + 1, j + 1)
                pending_route.append(t0)

                if gi == 0:
                    check_stop("g0")
            while pending_route:
                t0r = pending_route.pop(0)
                route_a(t0r)
                route_b(t0r)
            S.barrier()
            dump("cnt", cnt[:], cntB, 32)
            check_stop("A")

        with ExitStack() as pb:
            def walloc(name):
                t_, _b = alloc(pb, [128, 8, D], BF16, name)
                return t_, [Buf(name + "h0"), Buf(name + "h1")]
            wgS = [pre["wg"], walloc("wg")]
            wuS = [pre["wu"], walloc("wu")]
            wdS = [pre["wd"], walloc("wd")]
            bd16, _ = alloc(pb, [1, 2, D], BF16, "bd16")
            bdB = [Buf("bd0"), Buf("bd1")]
            def xalloc():
                t_, _b = alloc(pb, [128, 8, CAP], BF16, "xT")
                return t_, [Buf("xT%d" % kc) for kc in range(8)]
            xTS = [xalloc() for _ in range(2)]
            actT, _ = alloc(pb, [128, 8, CAP], BF16, "actT")
            actTB = [Buf("actT0"), Buf("actT1")]
            acR = Ring([alloc(pb, [128, 384], F32, "ac") for _ in range(3)])
            sgR = Ring([alloc(pb, [128, 384], F32, "sg") for _ in range(3)])
            ttR = Ring([alloc(pb, [128, 384], F32, "tt") for _ in range(3)])
            b1R = Ring([alloc(pb, [128, 384], F32, "b1") for _ in range(3)])
            yR = Ring([alloc(pb, [128, D], F32, "yb") for _ in range(2)])
            gR = Ring([palloc(pb, [128, 512], F32, "gb") for _ in range(6)])
            dR = Ring([palloc(pb, [128, 512], F32, "db") for _ in range(2)])
            gffnT = cvec[:, 8:16]

            def load(e):
                b = e % 2
                for (ws, src) in ((wgS, wg_d), (wuS, wu_d), (wdS, wd_d)):
                    if e == 0:
                        break
                    w, wB = ws[b]
                    for hh in range(2):
                        S.dma("pool", lambda eng: eng.dma_start(out=w[:, hh * 4:(hh + 1) * 4, :],
                                                                in_=src[e, hh * 512:(hh + 1) * 512, :].rearrange("(c p) o -> p c o", p=128)), (), [wB[hh]])
                S.dma("pool", lambda eng: eng.dma_start(out=bd16[0:1, b, :], in_=bd_d[e:e + 1, :]), (), [bdB[b]])
                xT, xTB = xTS[b]
                for kc in range(8):
                    S.dma("sp", lambda eng: eng.dma_start_transpose(out=xT[:, kc, :], in_=xs_d[e * CAP:(e + 1) * CAP, kc * 128:(kc + 1) * 128]), (), [xTB[kc]])

            def transposes(e):
                b = e % 2
                xT, xTB = xTS[b]
                dve(lambda eng: eng.tensor_tensor(out=xT[:, :, :], in0=xT[:, :, :], in1=gffnT.unsqueeze(2).to_broadcast([128, 8, CAP]), op=ALU.mult),
                    xTB + [cvecB], xTB)

            HALVES = ((0, 384), (384, 256))
            pend = []

            def flush_fin():
                while pend:
                    pend.pop(0)()

            def gate_up(e, hf):
                b = e % 2
                wg, wgB = wgS[b]
                wu, wuB = wuS[b]
                xT, xTB = xTS[b]
                s0, sn = HALVES[hf]
                for fc in range(8):
                    gA, gAB = gR.next()
                    gB_, gBB = gR.next()
                    mmgroup(gA[:, 0:sn], gAB, [(wg[:, kc, fc * 128:(fc + 1) * 128], xT[:, kc, s0:s0 + sn]) for kc in range(8)], wgB + xTB)
                    mmgroup(gB_[:, 0:sn], gBB, [(wu[:, kc, fc * 128:(fc + 1) * 128], xT[:, kc, s0:s0 + sn]) for kc in range(8)], wuB + xTB)
                    ac, acB = acR.next()
                    sg, sgB = sgR.next()
                    tt, ttB = ttR.next()
                    b1, b1B = b1R.next()
                    col = e * 8 + fc
                    dve(lambda eng: eng.tensor_scalar(out=ac[:, 0:sn], in0=gA[:, 0:sn], scalar1=bgT[:, col:col + 1], scalar2=7.0, op0=ALU.add, op1=ALU.min),
                        [gAB, bgTB], [acB])
                    act(lambda eng: eng.activation(out=sg[:, 0:sn], in_=ac[:, 0:sn], func=AF.Sigmoid, scale=1.702), [acB], [sgB])
                    pool(lambda eng: eng.tensor_tensor(out=tt[:, 0:sn], in0=ac[:, 0:sn], in1=sg[:, 0:sn], op=ALU.mult), [acB, sgB], [ttB])
                    dve(lambda eng: eng.tensor_scalar(out=b1[:, 0:sn], in0=gB_[:, 0:sn], scalar1=bu1T[:, col:col + 1], scalar2=8.0, op0=ALU.add, op1=ALU.min),
                        [gBB, bu1TB], [b1B])
                    flush_fin()

                    def fin(fc=fc, s0=s0, sn=sn, b1=b1, b1B=b1B, tt=tt, ttB=ttB, hf=hf):
                        dve(lambda eng: eng.scalar_tensor_tensor(out=actT[:, fc, s0:s0 + sn], in0=b1[:, 0:sn], scalar=-6.0, in1=tt[:, 0:sn], op0=ALU.max, op1=ALU.mult),
                            [b1B, ttB], [actTB[hf]])
                    pend.append(fin)

            def down(e, blks):
                b = e % 2
                wd, wdB = wdS[b]
                for blk in blks:
                    yb, ybB = yR.next()
                    for ohf in range(2):
                        dp, dpB = dR.next()
                        pairs = [(actT[:, fc, blk * 128:(blk + 1) * 128], wd[:, fc, ohf * 512:(ohf + 1) * 512]) for fc in range(8)]
                        pairs.append((ones16[0:1, 0:128], bd16[0:1, b, ohf * 512:(ohf + 1) * 512]))
                        mmgroup(dp[:, :], dpB, pairs, [actTB[0 if blk < 3 else 1], bdB[b], ones16B] + wdB)
                        act(lambda eng: eng.copy(out=yb[:, ohf * 512:(ohf + 1) * 512], in_=dp[:, :]), [dpB], [ybB])
                    r0 = e * CAP + blk * 128
                    S.dma("sp", lambda eng: eng.dma_start(out=ys_d[r0:r0 + 128, :], in_=yb[:]), [ybB], ())

            load(0)
            transposes(0)
            for e in range(NE):
                if e + 1 < NE:
                    load(e + 1)
                gate_up(e, 0)
                gate_up(e, 1)
                flush_fin()
                down(e, (0, 1, 2))
                if e + 1 < NE:
                    transposes(e + 1)
                down(e, (3, 4))
            S.barrier()
            pbr.close()
            check_stop("B")

        with ExitStack() as pc:
            wpg, wpgB = alloc(pc, [128, 8, D], BF16, "wpg")
            wpp, wppB = alloc(pc, [128, 2, D], BF16, "wpp")
            gfin, gfinB = alloc(pc, [128, D], F32, "gfin")
            gpleT = cvec[:, 16:24]
            with ExitStack() as stc:
                wstg, wstgB = alloc(stc, [128, 8, D], F32, "wstg")
                S.dma("sp", lambda e: e.dma_start(out=wstg[:], in_=wpg_d.rearrange("(c p) o -> p c o", p=128)), (), [wstgB])
                for kc in range(8):
                    dve(lambda e: e.tensor_scalar_mul(out=wpg[:, kc, :], in0=wstg[:, kc, :], scalar1=gpleT[:, kc:kc + 1]), [wstgB, cvecB], [wpgB])
                S.barrier()
            S.dma("pool", lambda e: e.dma_start(out=wpp[:], in_=wpp_d.rearrange("(c p) o -> p c o", p=128)), (), [wppB])
            S.dma("sp", lambda e: e.dma_start(out=gfin[:], in_=rowp_d[:, 548:548 + D]), (), [gfinB])
            xcR = Ring([alloc(pc, [128, D], F32, "xc") for _ in range(6)])
            ygR = Ring([alloc(pc, [128, D], F32, "yg") for _ in range(16)])
            ptR_ = Ring([alloc(pc, [128, 256], F32, "pt32") for _ in range(4)])
            p16R = Ring([alloc(pc, [128, 256], BF16, "p16") for _ in range(2)])
            hpR = Ring([alloc(pc, [128, D], BF16, "hp") for _ in range(3)])
            hpTR = Ring([alloc(pc, [128, 8, 128], BF16, "hpT") for _ in range(3)])
            pTR = Ring([alloc(pc, [128, 2, 128], BF16, "pT") for _ in range(3)])
            sgR = Ring([alloc(pc, [128, D], F32, "sgc") for _ in range(3)])
            junkR = Ring([alloc(pc, [128, D], BF16, "junk") for _ in range(2)])
            tpR = Ring([palloc(pc, [128, 1024], BF16, "tpc") for _ in range(2)])
            fR = Ring([palloc(pc, [128, 512], F32, "fc") for _ in range(6)])
            cs = {}

            def stL(t):
                xc, xcB = xcR.next()
                S.dma("sp", lambda e: e.dma_start(out=xc[:], in_=x1_d[t * 128:(t + 1) * 128, :]), (), [xcB])
                p32, p32B = ptR_.next()
                S.dma("sp", lambda e: e.dma_start(out=p32[:], in_=p_d[t * 128:(t + 1) * 128, :]), (), [p32B])
                ygs = []
                for k in range(4):
                    yg, ygB = ygR.next()
                    S.dma("pool", lambda e: e.indirect_dma_start(
                        out=yg[:, :], out_offset=None, in_=ys_d,
                        in_offset=bass.IndirectOffsetOnAxis(ap=slots_all[:, t, k:k + 1], axis=0)), [slotB[t]], [ygB])
                    ygs.append((yg, ygB))
                cs[t] = dict(xc=xc, xcB=xcB, p32=p32, p32B=p32B, ygs=ygs)

            def stB(t):
                c = cs[t]
                xc, xcB = c["xc"], c["xcB"]
                for k in range(4):
                    yg, ygB = c["ygs"][k]
                    dve(lambda e: e.scalar_tensor_tensor(out=xc[:], in0=yg[:], scalar=gates_all[:, t, k:k + 1], in1=xc[:], op0=ALU.mult, op1=ALU.add),
                        [ygB, gateB[t], xcB], [xcB])
                hp, hpB = hpR.next()
                act(lambda e: e.copy(out=hp[:], in_=xc[:]), [xcB], [hpB])
                p16, p16B = p16R.next()
                act(lambda e: e.copy(out=p16[:], in_=c["p32"][:]), [c["p32B"]], [p16B])
                junk, junkB = junkR.next()
                sd3, sd3B = rms_sd_c(xc[:], xcB, junk[:], junkB)
                c.update(hp=hp, hpB=hpB, p16=p16, p16B=p16B, sd3=sd3, sd3B=sd3B)

            def stC(t):
                c = cs[t]
                hp, hpB, p16, p16B = c["hp"], c["hpB"], c["p16"], c["p16B"]
                c["rs3"], c["rs3B"] = rms_recip_c(c["sd3"], c["sd3B"])
                tp, tpB = tpR.next()
                for kc in range(8):
                    pe(lambda e: e.transpose(out=tp[:, kc * 128:(kc + 1) * 128], in_=hp[:, kc * 128:(kc + 1) * 128], identity=ident16[:]),
                       [hpB, ident16B], [tpB], inc=(kc == 7))
                hpT, hpTB = hpTR.next()
                act(lambda e: e.copy(out=hpT[:], in_=tp[:, :].rearrange("p (c t) -> p c t", c=8)), [tpB], [hpTB])
                tp2, tp2B = tpR.next()
                for cc in range(2):
                    pe(lambda e: e.transpose(out=tp2[:, cc * 128:(cc + 1) * 128], in_=p16[:, cc * 128:(cc + 1) * 128], identity=ident16[:]),
                       [p16B, ident16B], [tp2B], inc=(cc == 1))
                pT, pTB = pTR.next()
                act(lambda e: e.copy(out=pT[:], in_=tp2[:, 0:256].rearrange("p (c t) -> p c t", c=2)), [tp2B], [pTB])
                c.update(hpT=hpT, hpTB=hpTB, pT=pT, pTB=pTB)

            def stD(t):
                c = cs[t]
                xc, xcB = c["xc"], c["xcB"]
                hpT, hpTB, pT, pTB = c["hpT"], c["hpTB"], c["pT"], c["pTB"]
                sg, sgB = sgR.next()
                for half in range(2):
                    fg, fgB = fR.next()
                    mmgroup(fg[:, :], fgB, [(hpT[:, kc, :], wpg[:, kc, half * 512:(half + 1) * 512]) for kc in range(8)], [hpTB, wpgB])
                    fp, fpB = fR.next()
                    mmgroup(fp[:, :], fpB, [(pT[:, cc, :], wpp[:, cc, half * 512:(half + 1) * 512]) for cc in range(2)], [pTB, wppB])
                    act(lambda e: e.activation(out=sg[:, half * 512:(half + 1) * 512], in_=fg[:, :], func=AF.Sigmoid, scale=c["rs3"]), [fgB, c["rs3B"]], [sgB])
                    c["fp%d" % half] = (fp, fpB)
                c.update(sg=sg, sgB=sgB)

            def stD2(t):
                c = cs[t]
                xc, xcB, sg, sgB = c["xc"], c["xcB"], c["sg"], c["sgB"]
                for half in range(2):
                    fp, fpB = c["fp%d" % half]
                    dve(lambda e: e.tensor_tensor(out=sg[:, half * 512:(half + 1) * 512], in0=fp[:, :], in1=sg[:, half * 512:(half + 1) * 512], op=ALU.mult),
                        [fpB, sgB], [sgB])
                dve(lambda e: e.tensor_tensor(out=xc[:], in0=xc[:], in1=sg[:], op=ALU.add), [xcB, sgB], [xcB])
                junk, junkB = junkR.next()
                sd4, sd4B = rms_sd_c(xc[:], xcB, junk[:], junkB)
                c.update(sg=sg, sgB=sgB, sd4=sd4, sd4B=sd4B)

            def stE(t):
                c = cs[t]
                xc, xcB, sg, sgB = c["xc"], c["xcB"], c["sg"], c["sgB"]
                c["rs4"], c["rs4B"] = rms_recip_c(c["sd4"], c["sd4B"])
                dve(lambda e: e.scalar_tensor_tensor(out=sg[:], in0=xc[:], scalar=c["rs4"], in1=gfin[:], op0=ALU.mult, op1=ALU.mult), [xcB, c["rs4B"], gfinB], [sgB])
                S.dma("sp", lambda e: e.dma_start(out=out_d[t * 128:(t + 1) * 128, :], in_=sg[:]), [sgB], (), track=out_tokens)
                del cs[t]

            for i in range(-3, NT + 1):
                for fn, tt_ in ((stL, i + 3), (stC, i + 1), (stD, i), (stB, i + 2), (stD2, i), (stE, i - 1)):
                    if 0 <= tt_ < NT:
                        fn(tt_)
            S._wait("sp", out_tokens)
            S.barrier()


def _consts():
    ident = np.eye(128, dtype=np.float32)
    ar = np.arange(128)
    triU = (ar[:, None] < ar[None, :]).astype(np.float32)
    maskI = (ar[:, None] <= ar[None, :]).astype(np.float32)
    iota = np.broadcast_to(np.arange(32, dtype=np.float32), (128, 32))
    kaug = np.zeros((128, 2, 128), np.float32)
    kaug[0, 0, :] = ar - 128.0
    kaug[0, 1, :] = ar
    kaug[1, :, :] = 1.0
    qaug = np.zeros((128, 2, 4, 128), np.float32)
    for k in range(2):
        for g in range(4):
            slope = 2.0 ** (-8.0 * (k * 4 + g + 1) / 8.0)
            qaug[0, k, g, :] = 8.0 * slope
            qaug[1, k, g, :] = -8.0 * slope * ar
    mask01 = np.zeros((128, 2, 128), np.float32)
    mask01[:, 0, :] = (ar[:, None] > ar[None, :])
    mask01[:, 1, :] = (ar[:, None] <= ar[None, :])
    return np.ascontiguousarray(np.concatenate([ident, triU, maskI, iota, kaug.reshape(128, 256), qaug.reshape(128, 1024),
                                                mask01.reshape(128, 256)], axis=1))


_NC_CACHE = {}


def _prep(x, p, g_mix, w_in, attn_sinks, g_sgu, b_sgu, w_spatial, b_spatial, w_attn_proj, w_sgu_proj,
          w_out, g_ffn, w_router, b_router, w_gate, b_gate, w_up, b_up, w_down, b_down,
          g_ple, w_ple_gate, w_ple_proj, g_final):
    f = lambda a: np.ascontiguousarray(np.asarray(a, dtype=np.float32))
    x = f(x).reshape(NCORES, T, D)
    p = f(p)[0].reshape(NCORES, T, 256)
    w_in0 = f(w_in)[0]
    qcols = np.concatenate([np.arange((k * 4 + g) * 64, (k * 4 + g + 1) * 64) for g in range(4) for k in range(2)])
    w_in_p = np.ascontiguousarray(np.concatenate([w_in0[:, qcols], w_in0[:, 512:]], axis=1))
    colT = lambda v: f(v).reshape(-1, 128).T
    cvec = np.ascontiguousarray(np.concatenate([colT(g_mix[0]), colT(g_ffn[0]), colT(g_ple[0]), colT(g_sgu[0]), colT(b_sgu[0])], axis=1))
    bgT = np.ascontiguousarray(f(b_gate)[0].reshape(NE, 8, 128).transpose(2, 0, 1).reshape(128, NE * 8))
    buT = np.ascontiguousarray(f(b_up)[0].reshape(NE, 8, 128).transpose(2, 0, 1).reshape(128, NE * 8))
    sinks = f(attn_sinks)[0].reshape(2, 4)
    sink_rows = np.repeat(sinks, 64, axis=0)
    bc = lambda v: np.broadcast_to(f(v).reshape(1, -1), (128, f(v).size))
    rowp = np.ascontiguousarray(np.concatenate([bc(b_router[0]), sink_rows, bc(b_spatial[0]), bc(g_final)], axis=1))
    shared = {
        "w_in": w_in_p, "w_attn_proj": f(w_attn_proj)[0], "w_sgu_proj": f(w_sgu_proj)[0], "w_out": f(w_out)[0],
        "w_router": f(w_router)[0], "w_gate": f(w_gate)[0], "w_up": f(w_up)[0], "w_down": f(w_down)[0],
        "b_down": f(b_down)[0], "w_ple_gate": f(w_ple_gate)[0], "w_ple_proj": f(w_ple_proj)[0],
        "w_spatial": f(w_spatial)[0], "cvec": cvec, "bgT": bgT, "buT": buT, "rowp": rowp, "cst": _consts(),
    }
    return shared, x, p


def kernel(**inputs):
    shared, x, p = _prep(**inputs)
    if "nc" not in _NC_CACHE:
        _NC_CACHE["nc"] = build_nc()
    nc = _NC_CACHE["nc"]
    in_maps = []
    for c in range(NCORES):
        m = dict(shared)
        m["x"] = x[c]
        m["p"] = p[c]
        in_maps.append(m)
    res = run_bass_kernel_spmd(nc, in_maps, core_ids=list(range(NCORES)))
    out = np.stack([np.asarray(r["out"], dtype=np.float32) for r in res.results], axis=0)
    return out.reshape(16, 2048, D)
```

```python
from contextlib import ExitStack
import numpy as np
import concourse.bass as bass
import concourse.mybir as mybir
from concourse.bass_utils import run_bass_kernel_spmd

F32 = mybir.dt.float32
BF16 = mybir.dt.bfloat16
I32 = mybir.dt.int32
U32 = mybir.dt.uint32
AF = mybir.ActivationFunctionType
ALU = mybir.AluOpType

NCORES = 8
D = 1024
T = 4096
NT = 32
TPS = 16
NG = 8
NE = 32
CAP = 640
NB = CAP // 128
NSLOT = NE * CAP
BIGF = 1.0e6
QO, KO, VO, GUO, GVO, GAO, GBO = 0, 512, 640, 768, 1280, 1792, 2816
INW = 3840
RMS_EPS = 1e-6
LN_EPS = 1e-5
CSTW = 128 * 3 + 32 + 256 + 1024 + 256


class Buf:
    __slots__ = ("name", "w", "r")

    def __init__(self, name=""):
        self.name = name
        self.w = None
        self.r = []


class Sched:
    def __init__(self, nc, n_dma_sems=32):
        self.nc = nc
        self.engs = {"pe": nc.tensor, "act": nc.scalar, "dve": nc.vector, "pool": nc.gpsimd, "sp": nc.sync}
        self.sems = {}
        self.cnt = {}
        self._ctx = []
        for k in list(self.engs) + ["d%d" % i for i in range(n_dma_sems)]:
            cm = nc.semaphore("s_" + k)
            self.sems[k] = cm.__enter__()
            self._ctx.append(cm)
            self.cnt[k] = 0
        half = n_dma_sems // 2
        self.dma_keys = {"sp": ["d%d" % i for i in range(half)], "pool": ["d%d" % i for i in range(half, n_dma_sems)]}
        self.dma_rr = {"sp": 0, "pool": 0}
        self.seen = {e: {} for e in self.engs}
        self.pe_pending = False

    def close(self):
        for cm in reversed(self._ctx):
            cm.__exit__(None, None, None)

    def _wait(self, e, deps):
        need = {}
        for d in deps:
            if d is None:
                continue
            k, v = d
            if k == "pe" and e == "pe":
                continue
            if v > need.get(k, 0):
                need[k] = v
        for k, v in need.items():
            if self.seen[e].get(k, 0) >= v:
                continue
            assert v <= self.cnt[k], (e, k, v, self.cnt[k])
            self.engs[e].wait_ge(self.sems[k], v)
            self.seen[e][k] = v

    @staticmethod
    def _deps(reads, writes):
        deps = []
        for b in reads:
            deps.append(b.w)
        for b in writes:
            deps.append(b.w)
            deps.extend(b.r)
        return deps

    def _record(self, tok, reads, writes):
        for b in reads:
            b.r.append(tok)
            if len(b.r) > 64:
                best = {}
                for k, v in b.r:
                    if v > best.get(k, 0):
                        best[k] = v
                b.r = list(best.items())
        for b in writes:
            b.w = tok
            b.r = []

    def op(self, e, fn, reads=(), writes=(), inc=True):
        self._wait(e, self._deps(reads, writes))
        ins = fn(self.engs[e])
        if inc:
            self.cnt[e] += 1
            ins.then_inc(self.sems[e], 1)
            tok = (e, self.cnt[e])
            if e == "pe":
                self.pe_pending = False
        else:
            assert e == "pe"
            tok = (e, self.cnt[e] + 1)
            self.pe_pending = True
        self._record(tok, reads, writes)
        return tok

    def dma(self, q, fn, reads=(), writes=(), track=None):
        keys = self.dma_keys[q]
        k = keys[self.dma_rr[q] % len(keys)]
        self.dma_rr[q] += 1
        deps = self._deps(reads, writes)
        if self.cnt[k] > 0:
            deps.append((k, self.cnt[k]))
        self._wait(q, deps)
        ins = fn(self.engs[q])
        self.cnt[k] += 16
        ins.then_inc(self.sems[k], 16)
        tok = (k, self.cnt[k])
        self._record(tok, reads, writes)
        if track is not None:
            track.append(tok)
        return tok

    def barrier(self):
        assert not self.pe_pending
        for e in self.engs:
            self._wait(e, [(k, v) for k, v in self.cnt.items() if v > 0])


class _Stop(Exception):
    pass


def build_nc(stop=None, dumps=()):
    nc = bass.Bass("TRN2", target_bir_lowering=False)

    def din(name, shape, dt=F32):
        return nc.dram_tensor(name, list(shape), dt, kind="ExternalInput").ap()

    x_d = din("x", [T, D])
    p_d = din("p", [T, 256])
    win_d = din("w_in", [D, INW])
    wa_d = din("w_attn_proj", [512, D])
    wb_d = din("w_sgu_proj", [512, D])
    wout_d = din("w_out", [D, D])
    wr_d = din("w_router", [D, NE])
    wg_d = din("w_gate", [NE, D, D])
    wu_d = din("w_up", [NE, D, D])
    wd_d = din("w_down", [NE, D, D])
    bd_d = din("b_down", [NE, D])
    wpg_d = din("w_ple_gate", [D, D])
    wpp_d = din("w_ple_proj", [256, D])
    ws_d = din("w_spatial", [4, 128, 128])
    cvec_d = din("cvec", [128, 32])
    bgT_d = din("bgT", [128, NE * 8])
    buT_d = din("buT", [128, NE * 8])
    rowp_d = din("rowp", [128, 32 + 4 + 512 + 1024])
    cst_d = din("cst", [128, CSTW])
    out_d = nc.dram_tensor("out", [T, D], F32, kind="ExternalOutput").ap()
    x1_d = nc.dram_tensor("x1_scr", [T, D], F32, kind="Internal").ap()
    xs_d = nc.dram_tensor("xs_scr", [NSLOT + 128, D], BF16, kind="Internal").ap()
    ys_d = nc.dram_tensor("ys_scr", [NSLOT + 128, D], F32, kind="Internal").ap()

    S = Sched(nc)
    uid = [0]

    def alloc(es, shape, dt, name="t", side=None):
        uid[0] += 1
        if side is None:
            t = es.enter_context(nc.sbuf_tensor("%s_%d" % (name, uid[0]), list(shape), dt))
        else:
            t = es.enter_context(nc.sbuf_tensor("%s_%d" % (name, uid[0]), list(shape), dt, side=side))
        return t, Buf(name)

    def palloc(es, shape, dt, name="p"):
        uid[0] += 1
        t = es.enter_context(nc.psum_tensor("%s_%d" % (name, uid[0]), list(shape), dt))
        return t, Buf(name)

    class Ring:
        def __init__(self, items):
            self.items = items
            self.i = 0

        def next(self):
            it = self.items[self.i % len(self.items)]
            self.i += 1
            return it

    def pe(fn, r=(), w=(), inc=True):
        return S.op("pe", fn, r, w, inc=inc)

    def act(fn, r=(), w=()):
        return S.op("act", fn, r, w)

    def dve(fn, r=(), w=()):
        return S.op("dve", fn, r, w)

    def pool(fn, r=(), w=()):
        return S.op("pool", fn, r, w)

    def mmgroup(out_ap, outB, pairs, rB, first=True, last=True):
        n = len(pairs)
        for i, (l, r) in enumerate(pairs):
            st = first and i == 0
            sp_ = last and i == n - 1
            pe(lambda e: e.matmul(out_ap, lhsT=l, rhs=r, start=st, stop=sp_), rB, [outB], inc=(i == n - 1))

    out_tokens = []
    dump_names = []

    def dump(name, ap, B, cols):
        if name not in dumps:
            return
        dd = nc.dram_tensor("dbg_" + name, [128, cols], F32, kind="ExternalOutput").ap()
        dump_names.append(name)
        with nc.sbuf_tensor("dbgst_" + name, [128, cols], F32) as stg:
            sB = Buf("stg")
            dve(lambda e: e.tensor_copy(out=stg[:], in_=ap), [B], [sB])
            S.dma("sp", lambda e: e.dma_start(out=dd, in_=stg[:]), [sB], ())
            S.barrier()

    def check_stop(tag):
        if stop == tag:
            raise _Stop()

    try:
        _body(locals())
    except _Stop:
        pass
    S.barrier()
    S.close()
    return nc


def _body(L):
    globals_ = L
    nc = L["nc"]; S = L["S"]; alloc = L["alloc"]; palloc = L["palloc"]; Ring = L["Ring"]
    pe = L["pe"]; act = L["act"]; dve = L["dve"]; pool = L["pool"]; mmgroup = L["mmgroup"]
    out_tokens = L["out_tokens"]; dump = L["dump"]; check_stop = L["check_stop"]
    x_d = L["x_d"]; p_d = L["p_d"]; win_d = L["win_d"]; wa_d = L["wa_d"]; wb_d = L["wb_d"]; wout_d = L["wout_d"]
    wr_d = L["wr_d"]; wg_d = L["wg_d"]; wu_d = L["wu_d"]; wd_d = L["wd_d"]; bd_d = L["bd_d"]; wpg_d = L["wpg_d"]
    wpp_d = L["wpp_d"]; ws_d = L["ws_d"]; cvec_d = L["cvec_d"]; bgT_d = L["bgT_d"]; buT_d = L["buT_d"]
    rowp_d = L["rowp_d"]; cst_d = L["cst_d"]; out_d = L["out_d"]; x1_d = L["x1_d"]; xs_d = L["xs_d"]; ys_d = L["ys_d"]

    with ExitStack() as glob:
        ident16, ident16B = alloc(glob, [128, 128], BF16, "ident16")
        ident32, ident32B = alloc(glob, [128, 128], F32, "ident32")
        ones16, ones16B = alloc(glob, [128, 128], BF16, "ones16")
        cvec, cvecB = alloc(glob, [128, 32], F32, "cvec")
        bgT, bgTB = alloc(glob, [128, NE * 8], F32, "bgT")
        bu1T, bu1TB = alloc(glob, [128, NE * 8], F32, "bu1T")
        gates_all, _ = alloc(glob, [128, NT, 4], F32, "gates")
        slots_all, _ = alloc(glob, [128, NT, 4], I32, "slots")
        gateB = [Buf("gate%d" % t) for t in range(NT)]
        slotB = [Buf("slot%d" % t) for t in range(NT)]
        stat, _ = alloc(glob, [128, 128], F32, "stat")
        statR = Ring([(stat[:, i:i + 1], Buf("stat%d" % i)) for i in range(128)])

        S.dma("sp", lambda e: e.dma_start(out=cvec[:], in_=cvec_d), (), [cvecB])
        S.dma("sp", lambda e: e.dma_start(out=bgT[:], in_=bgT_d), (), [bgTB])
        S.dma("sp", lambda e: e.dma_start(out=bu1T[:], in_=buT_d), (), [bu1TB])
        dve(lambda e: e.tensor_scalar_add(out=bu1T[:], in0=bu1T[:], scalar1=1.0), [bu1TB], [bu1TB])
        pool(lambda e: e.memset(ones16[:], 1.0), (), [ones16B])
        mhalf, mhalfB = alloc(glob, [128, 1], F32, "mhalf")
        pool(lambda e: e.memset(mhalf[:], -0.5), (), [mhalfB])
        pool(lambda e: e.memset(slots_all[:], 0), (), slotB)
        pool(lambda e: e.memset(gates_all[:], 0.0), (), gateB)

        def rms_sd(src_ap, srcB, junk_ap, junkB):
            ss, ssB = statR.next()
            act(lambda e: e.activation(out=junk_ap, in_=src_ap, func=AF.Square, accum_out=ss), [srcB], [junkB, ssB])
            return ss, ssB

        def rms_sd_c(src_ap, srcB, junk_ap, junkB):
            ss, ssB = rms_sd(src_ap, srcB, junk_ap, junkB)
            sd, sdB = statR.next()
            act(lambda e: e.activation(out=sd, in_=ss, func=AF.Sqrt, bias=RMS_EPS, scale=1.0 / D), [ssB], [sdB])
            return sd, sdB

        def rms_recip_c(sd, sdB):
            rs, rsB = statR.next()
            dve(lambda e: e.reciprocal(out=rs, in_=sd), [sdB], [rsB])
            return rs, rsB

        def rms_recip(ss, ssB, use_pool=True):
            if not use_pool:
                sd, sdB = statR.next()
                act(lambda e: e.activation(out=sd, in_=ss, func=AF.Sqrt, bias=RMS_EPS, scale=1.0 / D), [ssB], [sdB])
                rs, rsB = statR.next()
                dve(lambda e: e.reciprocal(out=rs, in_=sd), [sdB], [rsB])
                return rs, rsB
            v, vB = statR.next()
            dve(lambda e: e.tensor_scalar(out=v, in0=ss, scalar1=1.0 / D, scalar2=RMS_EPS, op0=ALU.mult, op1=ALU.add), [ssB], [vB])
            rs, rsB = statR.next()
            pool(lambda e: e.tensor_tensor(out=rs, in0=v, in1=mhalf[:, 0:1], op=ALU.pow), [vB, mhalfB], [rsB])
            return rs, rsB

        def rms_rstd(src_ap, srcB, junk_ap, junkB):
            sd, sdB = rms_sd(src_ap, srcB, junk_ap, junkB)
            return rms_recip(sd, sdB)

        pbr = ExitStack()
        pre = {}
        with ExitStack() as pa:
            pw = ExitStack()
            win, _ = alloc(pw, [128, 8, INW], BF16, "win", side="right")
            WIN_GROUPS = [(GVO, 512), (QO, 512), (KO, 128), (GUO, 512), (VO, 128), (GAO, 512), (GBO, 512), (GAO + 512, 512), (GBO + 512, 512)]
            winG = [Buf("win_c%d" % c0) for (c0, _) in WIN_GROUPS]

            def winB_for(col0):
                for (c0, w), b in zip(WIN_GROUPS, winG):
                    if c0 <= col0 < c0 + w:
                        return [b]
                raise AssertionError(col0)
            wa, _ = alloc(pa, [128, 4, D], BF16, "wa")
            waB = [Buf("wa0"), Buf("wa1")]
            wb, wbB = alloc(pa, [128, 4, D], BF16, "wb")
            wout, woutB = alloc(pa, [128, 8, D], BF16, "wout")
            wr, wrB = alloc(pa, [128, 8, NE], F32, "wr")
            rowp, rowpB = alloc(pa, [128, 32 + 4 + 512], F32, "rowp")
            kaug, kaugB = alloc(pa, [2, 2, 128], BF16, "kaug")
            qaug, qaugB = alloc(pa, [2, 2, 512], BF16, "qaug")
            mask01, mask01B = alloc(pa, [128, 2, 128], BF16, "mask01")
            triU, triUB = alloc(pa, [128, 128], BF16, "triU")
            iota, iotaB = alloc(pa, [128, 32], F32, "iota")
            wsT, wsTB = alloc(pa, [128, 4, 128], BF16, "wsT")
            comb, combB = alloc(pa, [128, 4, 128], F32, "comb")
            cnt, cntB = alloc(pa, [128, 32], F32, "cnt")

            tpR = Ring([palloc(pa, [128, 1024], BF16, "tp") for _ in range(2)])
            fR = Ring([palloc(pa, [128, 512], F32, "f") for _ in range(6)])

            S.dma("sp", lambda e: e.dma_start(out=wr[:], in_=wr_d.rearrange("(c p) o -> p c o", p=128)), (), [wrB])
            S.dma("sp", lambda e: e.dma_start(out=rowp[:], in_=rowp_d[:, 0:32 + 4 + 512]), (), [rowpB])
            brt = rowp[:, 0:32]
            expsink = rowp[:, 32:36]
            bspat = rowp[:, 36:36 + 512].rearrange("p (g t) -> p g t", g=4)

            with ExitStack() as st:
                cst, cstB = alloc(st, [128, CSTW], F32, "cst")
                wsn, wsnB = alloc(st, [128, 4, 128], F32, "wsn")
                S.dma("sp", lambda e: e.dma_start(out=cst[:], in_=cst_d), (), [cstB])
                S.dma("sp", lambda e: e.dma_start(out=wsn[:], in_=ws_d.rearrange("g t s -> t g s")), (), [wsnB])
                pool(lambda e: e.memset(cnt[:], 0.0), (), [cntB])
                zf, zfB = alloc(st, [128, D], F32, "zf")
                pool(lambda e: e.memset(zf[:], 0.0), (), [zfB])
                S.dma("sp", lambda e: e.dma_start(out=ys_d[NSLOT:NSLOT + 128, :], in_=zf[:]), [zfB], ())
                dve(lambda e: e.tensor_copy(out=ident32[:], in_=cst[:, 0:128]), [cstB], [ident32B])
                dve(lambda e: e.tensor_copy(out=ident16[:], in_=cst[:, 0:128]), [cstB], [ident16B])
                dve(lambda e: e.tensor_copy(out=triU[:], in_=cst[:, 128:256]), [cstB], [triUB])
                dve(lambda e: e.tensor_copy(out=iota[:], in_=cst[:, 384:416]), [cstB], [iotaB])
                dve(lambda e: e.tensor_copy(out=kaug[:], in_=cst[0:2, 416:672].rearrange("p (j n) -> p j n", j=2)), [cstB], [kaugB])
                dve(lambda e: e.tensor_copy(out=qaug[:], in_=cst[0:2, 672:1696].rearrange("p (k n) -> p k n", k=2)), [cstB], [qaugB])
                dve(lambda e: e.tensor_copy(out=mask01[:], in_=cst[:, 1696:1952].rearrange("p (j n) -> p j n", j=2)), [cstB], [mask01B])
                act(lambda e: e.activation(out=expsink, in_=expsink, func=AF.Exp), [rowpB], [rowpB])
                for g in range(4):
                    f, fB = fR.next()
                    pe(lambda e: e.transpose(out=f[:, 0:128], in_=wsn[:, g, :], identity=ident32[:]), [wsnB, ident32B], [fB])
                    dve(lambda e: e.tensor_tensor(out=wsT[:, g, :], in0=f[:, 0:128], in1=cst[:, 256:384], op=ALU.mult), [fB, cstB], [wsTB])
                f, fB = fR.next()
                mmgroup(f[:, :], fB, [(ones16[:, :], wsT[:, :, :].rearrange("p g t -> p (g t)"))], [ones16B, wsTB])
                for g in range(4):
                    dve(lambda e: e.scalar_tensor_tensor(out=comb[:, g, :], in0=f[:, g * 128:(g + 1) * 128], scalar=cvec[:, 28 + g:29 + g],
                                                         in1=bspat[:, g, :], op0=ALU.mult, op1=ALU.add), [fB, cvecB, rowpB], [combB])
                S.barrier()
            check_stop("setup")
            for (c0, w), b in zip(WIN_GROUPS, winG):
                S.dma("pool", lambda e: e.dma_start(out=win[:, :, c0:c0 + w], in_=win_d[:, c0:c0 + w].rearrange("(kc p) f -> p kc f", p=128)), (), [b])
            for k in range(2):
                S.dma("pool", lambda e: e.dma_start(
                    out=wa[k * 64:(k + 1) * 64, :, :],
                    in_=wa_d[k * 256:(k + 1) * 256, :].rearrange("(g hd) o -> hd g o", g=4)), (), [waB[k]])
            S.dma("pool", lambda e: e.dma_start(out=wb[:], in_=wb_d.rearrange("(c p) o -> p c o", p=128)), (), [wbB])
            S.dma("pool", lambda e: e.dma_start(out=wout[:], in_=wout_d.rearrange("(c p) o -> p c o", p=128)), (), [woutB])
            xR = Ring([alloc(pa, [128, D], F32, "x") for _ in range(4)])
            xnR = Ring([alloc(pa, [128, D], BF16, "xn") for _ in range(2)])
            hT, hTB = alloc(pa, [128, 8, 512], BF16, "hT")
            qT, qTB = alloc(pa, [128, 4, 512], BF16, "qT")
            kT, kTB = alloc(pa, [128, 5 * 128], BF16, "kT")
            vS, vSB = alloc(pa, [128, 5, 128], BF16, "vS")
            guT, guTB = alloc(pa, [128, 4, 512], BF16, "guT")
            gvgR = Ring([alloc(pa, [128, 512], F32, "gvg") for _ in range(2)])
            vln, vlnB = alloc(pa, [128, 4, 512], BF16, "vln")
            bnR = Ring([alloc(pa, [128, 8], F32, "bnst") for _ in range(2)])
            ptR = Ring([alloc(pa, [128, 2, 2, 512], BF16, "pt") for _ in range(2)])
            attnT, attnTB = alloc(pa, [128, 4, 512], BF16, "attnT")
            rden, rdenB = alloc(pa, [128, 512], F32, "rden")
            sguT, sguTB = alloc(pa, [128, 4, 512], BF16, "sguT")
            sgtmp, sgtmpB = alloc(pa, [128, 512], F32, "sgtmp")
            sigAR = Ring([alloc(pa, [128, 512], F32, "sigA") for _ in range(1)])
            sigBR = Ring([alloc(pa, [128, 512], F32, "sigB") for _ in range(1)])
            mrgT, mrgTB = alloc(pa, [128, 8, 512], BF16, "mrgT")
            hn4 = [alloc(pa, [128, D], BF16, "hn") for _ in range(4)]
            xgT, xgTB = alloc(pa, [128, 8, 128], F32, "xgT")
            lg4, lg4B = alloc(pa, [128, 4, 32], F32, "lg4")
            mx8, mx8B = alloc(pa, [128, 4, 8], F32, "mx8")
            ix8, ix8B = alloc(pa, [128, 4, 8], U32, "ix8")
            ixf, ixfB = alloc(pa, [128, 4, 4], F32, "ixf")
            ex4, ex4B = alloc(pa, [128, 4, 4], F32, "ex4")
            sm4, sm4B = alloc(pa, [128, 4], F32, "sm4")
            oh, ohB = alloc(pa, [128, 4, 4, 32], F32, "oh")
            msk16, msk16B = alloc(pa, [128, 4, 32], BF16, "msk16")
            posf, posfB = alloc(pa, [128, 4, 32], F32, "posf")
            ohp, ohpB = alloc(pa, [128, 4, 4, 32], F32, "ohp")
            p4, p4B = alloc(pa, [128, 4, 4], F32, "p4")
            slf, slfB = alloc(pa, [128, 4, 4], F32, "slf")
            ovf, ovfB = alloc(pa, [128, 4, 4], F32, "ovf")

            gmixT = cvec[:, 0:8]
            gffnT = cvec[:, 8:16]

            pending_route = []
            s1st = {}

            def route_a(t0, i0=0, n=4):
                tl = list(range(t0 + i0, t0 + i0 + n))
                sl = slice(i0, i0 + n)
                for i in range(i0, i0 + n):
                    dve(lambda e: e.max(out=mx8[:, i, :], in_=lg4[:, i, :]), [lg4B], [mx8B])
                    dve(lambda e: e.max_index(out=ix8[:, i, :], in_max=mx8[:, i, :], in_values=lg4[:, i, :]), [lg4B, mx8B], [ix8B])
                dve(lambda e: e.tensor_copy(out=ixf[:, sl, :], in_=ix8[:, sl, 0:4]), [ix8B], [ixfB])
                dve(lambda e: e.tensor_tensor(out=ex4[:, sl, :], in0=mx8[:, sl, 0:4], in1=mx8[:, sl, 0:1].to_broadcast([128, n, 4]), op=ALU.subtract), [mx8B], [ex4B])
                act(lambda e: e.activation(out=ex4[:, sl, :], in_=ex4[:, sl, :], func=AF.Exp), [ex4B], [ex4B])
                dve(lambda e: e.reduce_sum(out=sm4[:, sl], in_=ex4[:, sl, :], axis=mybir.AxisListType.X), [ex4B], [sm4B])
                dve(lambda e: e.reciprocal(out=sm4[:, sl], in_=sm4[:, sl]), [sm4B], [sm4B])
                dve(lambda e: e.tensor_tensor(out=gates_all[:, t0 + i0:t0 + i0 + n, :], in0=ex4[:, sl, :], in1=sm4[:, sl].unsqueeze(2).to_broadcast([128, n, 4]), op=ALU.mult),
                    [ex4B, sm4B], [gateB[t] for t in tl])
                dve(lambda e: e.tensor_tensor(out=oh[:, sl, :, :], in0=iota[:, :].unsqueeze(1).unsqueeze(1).to_broadcast([128, n, 4, 32]),
                                              in1=ixf[:, sl, :].unsqueeze(3).to_broadcast([128, n, 4, 32]), op=ALU.is_equal), [iotaB, ixfB], [ohB])
                with nc.allow_low_precision(reason="0/1 mask sums are exact in bf16"):
                    dve(lambda e: e.tensor_reduce(out=msk16[:, sl, :], in_=oh[:, sl, :, :].rearrange("p i k e -> p i e k"), axis=mybir.AxisListType.X, op=ALU.add),
                        [ohB], [msk16B])

            def route_b(t0, i0=0, n=4):
                tl = list(range(t0 + i0, t0 + i0 + n))
                sl = slice(i0, i0 + n)
                f, fB = fR.next()
                for ii in range(n):
                    i = i0 + ii
                    pairs = [(triU[:, :], msk16[:, i, :])] + [(ones16[:, :], msk16[:, i2, :]) for i2 in range(i0, i)]
                    mmgroup(f[:, ii * 32:(ii + 1) * 32], fB, pairs, [triUB, ones16B, msk16B])
                mmgroup(f[:, 128:160], fB, [(ones16[:, :], msk16[:, i, :]) for i in range(i0, i0 + n)], [ones16B, msk16B])
                dve(lambda e: e.tensor_tensor(out=posf[:, sl, :], in0=f[:, 0:n * 32].rearrange("p (i e) -> p i e", i=n),
                                              in1=cnt[:, :].unsqueeze(1).to_broadcast([128, n, 32]), op=ALU.add), [fB, cntB], [posfB])
                dve(lambda e: e.tensor_tensor(out=cnt[:], in0=f[:, 128:160], in1=cnt[:], op=ALU.add), [fB, cntB, posfB], [cntB])
                dve(lambda e: e.tensor_tensor(out=ohp[:, sl, :, :], in0=oh[:, sl, :, :], in1=posf[:, sl, :].unsqueeze(2).to_broadcast([128, n, 4, 32]), op=ALU.mult), [ohB, posfB], [ohpB])
                dve(lambda e: e.reduce_sum(out=p4[:, sl, :], in_=ohp[:, sl, :, :], axis=mybir.AxisListType.X), [ohpB], [p4B])
                dve(lambda e: e.scalar_tensor_tensor(out=slf[:, sl, :], in0=ixf[:, sl, :], scalar=float(CAP), in1=p4[:, sl, :], op0=ALU.mult, op1=ALU.add), [ixfB, p4B], [slfB])
                dve(lambda e: e.tensor_scalar(out=ovf[:, sl, :], in0=p4[:, sl, :], scalar1=float(CAP), scalar2=BIGF, op0=ALU.is_ge, op1=ALU.mult), [p4B], [ovfB])
                dve(lambda e: e.tensor_tensor(out=slf[:, sl, :], in0=slf[:, sl, :], in1=ovf[:, sl, :], op=ALU.max), [slfB, ovfB], [slfB])
                dve(lambda e: e.tensor_scalar(out=slf[:, sl, :], in0=slf[:, sl, :], scalar1=float(NSLOT), scalar2=0.0, op0=ALU.min, op1=ALU.max), [slfB], [slfB])
                dve(lambda e: e.tensor_copy(out=slots_all[:, t0 + i0:t0 + i0 + n, :], in_=slf[:, sl, :]), [slfB], [slotB[t] for t in tl])
                dve(lambda e: e.tensor_copy(out=ovf[:, sl, :], in_=slots_all[:, t0 + i0:t0 + i0 + n, :]), [slotB[t] for t in tl], [ovfB])
                if t0 == 0 and i0 == 0:
                    dump("lg", lg4[:, 0, :], lg4B, 32)
                    dump("gates", gates_all[:, 0, :], gateB[0], 4)
                    dump("slf", slf[:, 0, :], slfB, 4)
                    dump("posf", posf[:, 0, :], posfB, 32)
                for i in range(i0, i0 + n):
                    hn, hnB = hn4[i]
                    for k in range(4):
                        S.dma("pool", lambda e: e.indirect_dma_start(
                            out=xs_d, out_offset=bass.IndirectOffsetOnAxis(ap=slots_all[:, t0 + i, k:k + 1], axis=0),
                            in_=hn[:, :], in_offset=None), [hnB, slotB[t0 + i], ovfB], ())

            for gi in range(NG):
                t0 = gi * 4
                def s1_load(tg, i):
                    t = tg * 4 + i
                    xt, xtB = xR.next()
                    S.dma("sp", lambda e: e.dma_start(out=xt[:], in_=x_d[t * 128:(t + 1) * 128, :]), (), [xtB])
                    s1st[(tg, i)] = dict(xt=xt, xtB=xtB)

                def s1a(tg, i):
                    c = s1st[(tg, i)]
                    xt, xtB = c["xt"], c["xtB"]
                    xn, xnB = xnR.next()
                    rs, rsB = rms_rstd(xt[:], xtB, xn[:], xnB)
                    dve(lambda e: e.tensor_scalar_mul(out=xn[:], in0=xt[:], scalar1=rs), [xtB, rsB], [xnB])
                    c.update(xn=xn, xnB=xnB)

                def s1b(tg, i):
                    c = s1st.pop((tg, i))
                    xn, xnB = c["xn"], c["xnB"]
                    tp, tpB = tpR.next()
                    for kc in range(8):
                        pe(lambda e: e.transpose(out=tp[:, kc * 128:(kc + 1) * 128], in_=xn[:, kc * 128:(kc + 1) * 128], identity=ident16[:]),
                           [xnB, ident16B], [tpB], inc=(kc == 7))
                    dve(lambda e: e.tensor_tensor(out=hT[:, :, i * 128:(i + 1) * 128], in0=tp[:, :].rearrange("p (c t) -> p c t", c=8),
                                                  in1=gmixT.unsqueeze(2).to_broadcast([128, 8, 128]), op=ALU.mult), [tpB, cvecB], [hTB])

                if gi == 0:
                    for i in range(4):
                        s1_load(0, i)
                        s1a(0, i)
                        s1b(0, i)
                if gi == 0:
                    for kc in range(8):
                        dump("hT%d" % kc, hT[:, kc, :], hTB, 512)
                    check_stop("g0s1")

                def inproj_fm(col0, evac):
                    f, fB = fR.next()
                    mmgroup(f[:, :], fB, [(win[:, kc, col0:col0 + 128], hT[:, kc, :]) for kc in range(8)], [hTB] + winB_for(col0))
                    evac(f, fB)

                for t0r in pending_route:
                    route_a(t0r)
                gst = {}

                def gv_mm(i):
                    f, fB = fR.next()
                    mmgroup(f[:, :], fB, [(hT[:, kc, i * 128:(i + 1) * 128], win[:, kc, GVO:GVO + 512]) for kc in range(8)], [hTB] + winB_for(GVO))
                    gvg, gvgB = gvgR.next()
                    act(lambda e: e.activation(out=gvg[:], in_=f[:, :], func=AF.Gelu_apprx_tanh), [fB], [gvgB])
                    gst[i] = (gvg, gvgB)

                def gv_ln(i):
                    gvg, gvgB = gst[i]
                    bn, bnB = bnR.next()
                    dve(lambda e: e.bn_stats(out=bn[:, 0:6], in_=gvg[:]), [gvgB], [bnB])
                    dve(lambda e: e.bn_aggr(out=bn[:, 6:8], in_=bn[:, 0:6]), [bnB], [bnB])
                    sd, sdB = statR.next()
                    dve(lambda e: e.tensor_scalar_add(out=sd, in0=bn[:, 7:8], scalar1=LN_EPS), [bnB], [sdB])
                    rs, rsB = statR.next()
                    pool(lambda e: e.tensor_tensor(out=rs, in0=sd, in1=mhalf[:, 0:1], op=ALU.pow), [sdB, mhalfB], [rsB])
                    dve(lambda e: e.tensor_scalar(out=vln[:, i, :], in0=gvg[:], scalar1=bn[:, 6:7], scalar2=rs, op0=ALU.subtract, op1=ALU.mult),
                        [gvgB, bnB, rsB], [vlnB])

                for i in range(5):
                    if i < 4:
                        gv_mm(i)
                    if i >= 1:
                        gv_ln(i - 1)

                for g in range(4):
                    inproj_fm(QO + g * 128, lambda f, fB: act(lambda e: e.copy(out=qT[:, g, :], in_=f[:, :]), [fB], [qTB]))
                inproj_fm(KO, lambda f, fB: act(lambda e: e.copy(out=kT[:, 128:640], in_=f[:, :]), [fB], [kTB]))
                while pending_route:
                    route_b(pending_route.pop(0))
                for c in range(4):
                    inproj_fm(GUO + c * 128, lambda f, fB: act(lambda e: e.activation(out=guT[:, c, :], in_=f[:, :], func=AF.Gelu_apprx_tanh), [fB], [guTB]))
                f, fB = fR.next()
                for i in range(4):
                    mmgroup(f[:, i * 128:(i + 1) * 128], fB, [(hT[:, kc, i * 128:(i + 1) * 128], win[:, kc, VO:VO + 128]) for kc in range(8)], [hTB] + winB_for(VO))
                act(lambda e: e.copy(out=vS[:, 1:5, :], in_=f[:, :].rearrange("p (i c) -> p i c", i=4)), [fB], [vSB])

                if gi == 0:
                    for g in range(4):
                        dump("qT%d" % g, qT[:, g, :], qTB, 512)
                        dump("guT%d" % g, guT[:, g, :], guTB, 512)
                        dump("vln%d" % g, vln[:, g, :], vlnB, 512)
                    dump("kT", kT[:, :], kTB, 640)
                    dump("vS", vS[:, :, :].rearrange("p a b -> p (a b)"), vSB, 640)
                    check_stop("g0s2")
                for cg in range(4):
                    f, fB = fR.next()
                    for i in range(4):
                        mmgroup(f[:, i * 128:(i + 1) * 128], fB, [(vln[:, i, cg * 128:(cg + 1) * 128], wsT[:, cg, :])], [vlnB, wsTB])
                    dve(lambda e: e.scalar_tensor_tensor(out=sgtmp[:, :].rearrange("p (i t) -> p i t", i=4), in0=f[:, :].rearrange("p (i t) -> p i t", i=4),
                                                         scalar=cvec[:, 24 + cg:25 + cg], in1=comb[:, cg, :].unsqueeze(1).to_broadcast([128, 4, 128]),
                                                         op0=ALU.mult, op1=ALU.add), [fB, cvecB, combB], [sgtmpB])
                    dve(lambda e: e.tensor_tensor(out=sguT[:, cg, :], in0=sgtmp[:], in1=guT[:, cg, :], op=ALU.mult), [sgtmpB, guTB], [sguTB])

                ast = {}

                def att_scores(i):
                    t = t0 + i
                    n = t % TPS
                    js = [(0, i), (1, i + 1)] if n > 0 else [(1, i + 1)]
                    pt, ptB = ptR.next()
                    for k in range(2):
                        for (j, slot) in js:
                            f, fB = fR.next()
                            mmgroup(f[:, :], fB, [(kT[k * 64:(k + 1) * 64, slot * 128:(slot + 1) * 128], qT[k * 64:(k + 1) * 64, :, i * 128:(i + 1) * 128]),
                                                  (kaug[0:2, j, :], qaug[0:2, k, :])], [kTB, qTB, kaugB, qaugB])
                            act(lambda e: e.activation(out=pt[:, k, j, :], in_=f[:, :], func=AF.Exp, scale=0.125), [fB], [ptB])
                            dve(lambda e: e.tensor_tensor(out=pt[:, k, j, :].rearrange("p (g q) -> p g q", g=4),
                                                           in0=pt[:, k, j, :].rearrange("p (g q) -> p g q", g=4),
                                                           in1=mask01[:, j, :].unsqueeze(1).to_broadcast([128, 4, 128]), op=ALU.mult), [ptB, mask01B], [ptB])
                    ast[i] = (js, pt, ptB)

                def att_pv(i):
                    js, pt, ptB = ast[i]
                    pv, pvB = fR.next()
                    dn, dnB = fR.next()
                    for k in range(2):
                        mmgroup(pv[k * 64:(k + 1) * 64, :], pvB, [(vS[:, slot, k * 64:(k + 1) * 64], pt[:, k, j, :]) for (j, slot) in js], [vSB, ptB])
                    for k in range(2):
                        mmgroup(dn[k * 64:(k + 1) * 64, :], dnB, [(ones16[:, 0:64], pt[:, k, j, :]) for (j, slot) in js], [ones16B, ptB])
                    dve(lambda e: e.tensor_tensor(out=rden[:, :].rearrange("p (g q) -> p g q", g=4), in0=dn[:, :].rearrange("p (g q) -> p g q", g=4),
                                                  in1=expsink.unsqueeze(2).to_broadcast([128, 4, 128]), op=ALU.add), [dnB, rowpB], [rdenB])
                    act(lambda e: e.activation(out=rden[:], in_=rden[:], func=AF.Ln), [rdenB], [rdenB])
                    act(lambda e: e.activation(out=rden[:], in_=rden[:], func=AF.Exp, scale=-1.0), [rdenB], [rdenB])
                    dve(lambda e: e.tensor_tensor(out=attnT[:, :, i * 128:(i + 1) * 128], in0=pv[:, :].rearrange("p (g q) -> p g q", g=4),
                                                  in1=rden[:, :].rearrange("p (g q) -> p g q", g=4), op=ALU.mult), [pvB, rdenB], [attnTB])

                for i in range(5):
                    if i < 4:
                        att_scores(i)
                    if i >= 1:
                        att_pv(i - 1)
                pool(lambda e: e.tensor_copy(out=kT[:, 0:128], in_=kT[:, 512:640]), [kTB], [kTB])
                pool(lambda e: e.tensor_copy(out=vS[:, 0, :], in_=vS[:, 4, :]), [vSB], [vSB])

                if gi == 0:
                    for g in range(4):
                        dump("attnT%d" % g, attnT[:, g, :], attnTB, 512)
                        dump("sguT%d" % g, sguT[:, g, :], sguTB, 512)
                    check_stop("g0s3")
                for oc in range(8):
                    sigA, sigAB = sigAR.next()
                    sigB_, sigBB = sigBR.next()
                    inproj_fm(GAO + oc * 128, lambda f, fB: act(lambda e: e.activation(out=sigA[:], in_=f[:, :], func=AF.Sigmoid), [fB], [sigAB]))
                    inproj_fm(GBO + oc * 128, lambda f, fB: act(lambda e: e.activation(out=sigB_[:], in_=f[:, :], func=AF.Sigmoid), [fB], [sigBB]))
                    f, fB = fR.next()
                    mmgroup(f[:, :], fB, [(wa[:, g, oc * 128:(oc + 1) * 128], attnT[:, g, :]) for g in range(4)], waB + [attnTB])
                    dve(lambda e: e.tensor_tensor(out=sigA[:], in0=f[:, :], in1=sigA[:], op=ALU.mult), [fB, sigAB], [sigAB])
                    f, fB = fR.next()
                    mmgroup(f[:, :], fB, [(wb[:, c, oc * 128:(oc + 1) * 128], sguT[:, c, :]) for c in range(4)], [wbB, sguTB])
                    dve(lambda e: e.tensor_tensor(out=sigB_[:], in0=f[:, :], in1=sigB_[:], op=ALU.mult), [fB, sigBB], [sigBB])
                    dve(lambda e: e.tensor_tensor(out=mrgT[:, oc, :], in0=sigA[:], in1=sigB_[:], op=ALU.add), [sigAB, sigBB], [mrgTB])

                if gi == 0:
                    for kc in range(8):
                        dump("mrgT%d" % kc, mrgT[:, kc, :], mrgTB, 512)
                    check_stop("g0s4")
                if gi == NG - 1:
                    pw.close()
                    for nm, src in (("wg", wg_d), ("wu", wu_d), ("wd", wd_d)):
                        t_, _b = alloc(pbr, [128, 8, D], BF16, nm + "0", side="right")
                        wB2 = [Buf(nm + "0h0"), Buf(nm + "0h1")]
                        for hh in range(2):
                            S.dma("pool", lambda eng: eng.dma_start(out=t_[:, hh * 4:(hh + 1) * 4, :],
                                                                    in_=src[0, hh * 512:(hh + 1) * 512, :].rearrange("(c p) o -> p c o", p=128)),
                                  (), [wB2[hh]] + winG)
                        pre[nm] = (t_, wB2)
                s5 = {}

                def s5_op(i):
                    t = t0 + i
                    xr, xrB = xR.next()
                    S.dma("sp", lambda e: e.dma_start(out=xr[:], in_=x_d[t * 128:(t + 1) * 128, :]), (), [xrB])
                    for half in range(2):
                        f, fB = fR.next()
                        mmgroup(f[:, :], fB, [(mrgT[:, kc, i * 128:(i + 1) * 128], wout[:, kc, half * 512:(half + 1) * 512]) for kc in range(8)], [mrgTB, woutB])
                        dve(lambda e: e.tensor_tensor(out=xr[:, half * 512:(half + 1) * 512], in0=f[:, :], in1=xr[:, half * 512:(half + 1) * 512], op=ALU.add),
                            [fB, xrB], [xrB])
                    S.dma("sp", lambda e: e.dma_start(out=x1_d[t * 128:(t + 1) * 128, :], in_=xr[:]), [xrB], ())
                    hn, hnB = hn4[i]
                    sd2, sd2B = rms_sd(xr[:], xrB, hn[:], hnB)
                    s5[i] = dict(xr=xr, xrB=xrB, hn=hn, hnB=hnB, sd2=sd2, sd2B=sd2B)

                def s5_tr(i):
                    c = s5[i]
                    xr, xrB, hn, hnB = c["xr"], c["xrB"], c["hn"], c["hnB"]
                    rs2, rs2B = rms_recip(c["sd2"], c["sd2B"])
                    c["rs2"], c["rs2B"] = rs2, rs2B
                    act(lambda e: e.activation(out=hn[:], in_=xr[:], func=AF.Copy, scale=rs2), [xrB, rs2B], [hnB])
                    for hh in range(2):
                        f, fB = fR.next()
                        for c4 in range(4):
                            kc = hh * 4 + c4
                            pe(lambda e: e.transpose(out=f[:, c4 * 128:(c4 + 1) * 128], in_=xr[:, kc * 128:(kc + 1) * 128], identity=ident32[:]),
                               [xrB, ident32B], [fB], inc=(c4 == 3))
                        dve(lambda e: e.tensor_tensor(out=xgT[:, hh * 4:(hh + 1) * 4, :], in0=f[:, :].rearrange("p (c t) -> p c t", c=4),
                                                      in1=gffnT[:, hh * 4:(hh + 1) * 4].unsqueeze(2).to_broadcast([128, 4, 128]), op=ALU.mult),
                            [fB, cvecB], [xgTB])

                def s5_lg(i):
                    c = s5[i]
                    f, fB = fR.next()
                    mmgroup(f[:, 0:32], fB, [(xgT[:, kc, :], wr[:, kc, :]) for kc in range(8)], [xgTB, wrB])
                    dve(lambda e: e.scalar_tensor_tensor(out=lg4[:, i, :], in0=f[:, 0:32], scalar=c["rs2"], in1=brt, op0=ALU.mult, op1=ALU.add),
                        [fB, c["rs2B"], rowpB], [lg4B])

                if gi + 1 < NG:
                    s1_load(gi + 1, 0)
                for j in range(6):
                    if j < 4:
                        s5_op(j)
                    if 2 <= j:
                        s5_lg(j - 2)
                    if 1 <= j <= 4:
                        s5_tr(j - 1)
                    if gi == NG - 1 and j == 3:
                        route_a(t0, 0, 2)
                        route_b(t0, 0, 2)
                    if gi + 1 < NG:
                        if 1 <= j <= 4:
                            s1b(gi + 1, j - 1)
                        if j < 4:
                            s1a(gi + 1, j)
                        if j + 1 < 4:
                            s1_load(gi + 1, j + 1)
                if gi == NG - 1:
                    route_a(t0, 2, 2)
                    route_b(t0, 2, 2)
                else:
                    pending_route.append(t0)

                if gi == 0:
                    check_stop("g0")
            while pending_route:
                t0r = pending_route.pop(0)
                route_a(t0r)
                route_b(t0r)
            S.barrier()
            dump("cnt", cnt[:], cntB, 32)
            check_stop("A")

        with ExitStack() as pb:
            def walloc(name):
                t_, _b = alloc(pb, [128, 8, D], BF16, name)
                return t_, [Buf(name + "h0"), Buf(name + "h1")]
            wgS = [pre["wg"], walloc("wg")]
            wuS = [pre["wu"], walloc("wu")]
            wdS = [pre["wd"], walloc("wd")]
            bd16, _ = alloc(pb, [1, 2, D], BF16, "bd16")
            bdB = [Buf("bd0"), Buf("bd1")]
            def xalloc():
                t_, _b = alloc(pb, [128, 8, CAP], BF16, "xT")
                return t_, [Buf("xT%d" % kc) for kc in range(8)]
            xTS = [xalloc() for _ in range(2)]
            actT, _ = alloc(pb, [128, 8, CAP], BF16, "actT")
            actTB = [Buf("actT0"), Buf("actT1")]
            acR = Ring([alloc(pb, [128, 384], F32, "ac") for _ in range(3)])
            sgR = Ring([alloc(pb, [128, 384], F32, "sg") for _ in range(3)])
            ttR = Ring([alloc(pb, [128, 384], F32, "tt") for _ in range(3)])
            b1R = Ring([alloc(pb, [128, 384], F32, "b1") for _ in range(3)])
            yR = Ring([alloc(pb, [128, D], F32, "yb") for _ in range(2)])
            gR = Ring([palloc(pb, [128, 512], F32, "gb") for _ in range(6)])
            dR = Ring([palloc(pb, [128, 512], F32, "db") for _ in range(2)])
            gffnT = cvec[:, 8:16]

            def load(e):
                b = e % 2
                for (ws, src) in ((wgS, wg_d), (wuS, wu_d), (wdS, wd_d)):
                    if e == 0:
                        break
                    w, wB = ws[b]
                    for hh in range(2):
                        S.dma("pool", lambda eng: eng.dma_start(out=w[:, hh * 4:(hh + 1) * 4, :],
                                                                in_=src[e, hh * 512:(hh + 1) * 512, :].rearrange("(c p) o -> p c o", p=128)), (), [wB[hh]])
                S.dma("pool", lambda eng: eng.dma_start(out=bd16[0:1, b, :], in_=bd_d[e:e + 1, :]), (), [bdB[b]])
                xT, xTB = xTS[b]
                for kc in range(8):
                    S.dma("sp", lambda eng: eng.dma_start_transpose(out=xT[:, kc, :], in_=xs_d[e * CAP:(e + 1) * CAP, kc * 128:(kc + 1) * 128]), (), [xTB[kc]])

            def transposes(e):
                b = e % 2
                xT, xTB = xTS[b]
                dve(lambda eng: eng.tensor_tensor(out=xT[:, :, :], in0=xT[:, :, :], in1=gffnT.unsqueeze(2).to_broadcast([128, 8, CAP]), op=ALU.mult),
                    xTB + [cvecB], xTB)

            HALVES = ((0, 384), (384, 256))
            pend = []

            def flush_fin():
                while pend:
                    pend.pop(0)()

            def gate_up(e, hf):
                b = e % 2
                wg, wgB = wgS[b]
                wu, wuB = wuS[b]
                xT, xTB = xTS[b]
                s0, sn = HALVES[hf]
                for fc in range(8):
                    gA, gAB = gR.next()
                    gB_, gBB = gR.next()
                    mmgroup(gA[:, 0:sn], gAB, [(wg[:, kc, fc * 128:(fc + 1) * 128], xT[:, kc, s0:s0 + sn]) for kc in range(8)], wgB + xTB)
                    mmgroup(gB_[:, 0:sn], gBB, [(wu[:, kc, fc * 128:(fc + 1) * 128], xT[:, kc, s0:s0 + sn]) for kc in range(8)], wuB + xTB)
                    ac, acB = acR.next()
                    sg, sgB = sgR.next()
                    tt, ttB = ttR.next()
                    b1, b1B = b1R.next()
                    col = e * 8 + fc
                    dve(lambda eng: eng.tensor_scalar(out=ac[:, 0:sn], in0=gA[:, 0:sn], scalar1=bgT[:, col:col + 1], scalar2=7.0, op0=ALU.add, op1=ALU.min),
                        [gAB, bgTB], [acB])
                    act(lambda eng: eng.activation(out=sg[:, 0:sn], in_=ac[:, 0:sn], func=AF.Sigmoid, scale=1.702), [acB], [sgB])
                    pool(lambda eng: eng.tensor_tensor(out=tt[:, 0:sn], in0=ac[:, 0:sn], in1=sg[:, 0:sn], op=ALU.mult), [acB, sgB], [ttB])
                    dve(lambda eng: eng.tensor_scalar(out=b1[:, 0:sn], in0=gB_[:, 0:sn], scalar1=bu1T[:, col:col + 1], scalar2=8.0, op0=ALU.add, op1=ALU.min),
                        [gBB, bu1TB], [b1B])
                    flush_fin()

                    def fin(fc=fc, s0=s0, sn=sn, b1=b1, b1B=b1B, tt=tt, ttB=ttB, hf=hf):
                        dve(lambda eng: eng.scalar_tensor_tensor(out=actT[:, fc, s0:s0 + sn], in0=b1[:, 0:sn], scalar=-6.0, in1=tt[:, 0:sn], op0=ALU.max, op1=ALU.mult),
                            [b1B, ttB], [actTB[hf]])
                    pend.append(fin)

            def down(e, blks):
                b = e % 2
                wd, wdB = wdS[b]
                for blk in blks:
                    yb, ybB = yR.next()
                    for ohf in range(2):
                        dp, dpB = dR.next()
                        pairs = [(actT[:, fc, blk * 128:(blk + 1) * 128], wd[:, fc, ohf * 512:(ohf + 1) * 512]) for fc in range(8)]
                        pairs.append((ones16[0:1, 0:128], bd16[0:1, b, ohf * 512:(ohf + 1) * 512]))
                        mmgroup(dp[:, :], dpB, pairs, [actTB[0 if blk < 3 else 1], bdB[b], ones16B] + wdB)
                        act(lambda eng: eng.copy(out=yb[:, ohf * 512:(ohf + 1) * 512], in_=dp[:, :]), [dpB], [ybB])
                    r0 = e * CAP + blk * 128
                    S.dma("sp", lambda eng: eng.dma_start(out=ys_d[r0:r0 + 128, :], in_=yb[:]), [ybB], ())

            load(0)
            transposes(0)
            for e in range(NE):
                if e + 1 < NE:
                    load(e + 1)
                gate_up(e, 0)
                gate_up(e, 1)
                flush_fin()
                down(e, (0, 1, 2))
                if e + 1 < NE:
                    transposes(e + 1)
                down(e, (3, 4))
            S.barrier()
            pbr.close()
            check_stop("B")

        with ExitStack() as pc:
            wpg, wpgB = alloc(pc, [128, 8, D], BF16, "wpg")
            wpp, wppB = alloc(pc, [128, 2, D], BF16, "wpp")
            gfin, gfinB = alloc(pc, [128, D], F32, "gfin")
            gpleT = cvec[:, 16:24]
            with ExitStack() as stc:
                wstg, wstgB = alloc(stc, [128, 8, D], F32, "wstg")
                S.dma("sp", lambda e: e.dma_start(out=wstg[:], in_=wpg_d.rearrange("(c p) o -> p c o", p=128)), (), [wstgB])
                for kc in range(8):
                    dve(lambda e: e.tensor_scalar_mul(out=wpg[:, kc, :], in0=wstg[:, kc, :], scalar1=gpleT[:, kc:kc + 1]), [wstgB, cvecB], [wpgB])
                S.barrier()
            S.dma("pool", lambda e: e.dma_start(out=wpp[:], in_=wpp_d.rearrange("(c p) o -> p c o", p=128)), (), [wppB])
            S.dma("sp", lambda e: e.dma_start(out=gfin[:], in_=rowp_d[:, 548:548 + D]), (), [gfinB])
            xcR = Ring([alloc(pc, [128, D], F32, "xc") for _ in range(6)])
            ygR = Ring([alloc(pc, [128, D], F32, "yg") for _ in range(16)])
            ptR_ = Ring([alloc(pc, [128, 256], F32, "pt32") for _ in range(4)])
            p16R = Ring([alloc(pc, [128, 256], BF16, "p16") for _ in range(2)])
            hpR = Ring([alloc(pc, [128, D], BF16, "hp") for _ in range(3)])
            hpTR = Ring([alloc(pc, [128, 8, 128], BF16, "hpT") for _ in range(3)])
            pTR = Ring([alloc(pc, [128, 2, 128], BF16, "pT") for _ in range(3)])
            sgR = Ring([alloc(pc, [128, D], F32, "sgc") for _ in range(3)])
            junkR = Ring([alloc(pc, [128, D], BF16, "junk") for _ in range(2)])
            tpR = Ring([palloc(pc, [128, 1024], BF16, "tpc") for _ in range(2)])
            fR = Ring([palloc(pc, [128, 512], F32, "fc") for _ in range(6)])
            cs = {}

            def stL(t):
                xc, xcB = xcR.next()
                S.dma("sp", lambda e: e.dma_start(out=xc[:], in_=x1_d[t * 128:(t + 1) * 128, :]), (), [xcB])
                p32, p32B = ptR_.next()
                S.dma("sp", lambda e: e.dma_start(out=p32[:], in_=p_d[t * 128:(t + 1) * 128, :]), (), [p32B])
                ygs = []
                for k in range(4):
                    yg, ygB = ygR.next()
                    S.dma("pool", lambda e: e.indirect_dma_start(
                        out=yg[:, :], out_offset=None, in_=ys_d,
                        in_offset=bass.IndirectOffsetOnAxis(ap=slots_all[:, t, k:k + 1], axis=0)), [slotB[t]], [ygB])
                    ygs.append((yg, ygB))
                cs[t] = dict(xc=xc, xcB=xcB, p32=p32, p32B=p32B, ygs=ygs)

            def stB(t):
                c = cs[t]
                xc, xcB = c["xc"], c["xcB"]
                for k in range(4):
                    yg, ygB = c["ygs"][k]
                    dve(lambda e: e.scalar_tensor_tensor(out=xc[:], in0=yg[:], scalar=gates_all[:, t, k:k + 1], in1=xc[:], op0=ALU.mult, op1=ALU.add),
                        [ygB, gateB[t], xcB], [xcB])
                hp, hpB = hpR.next()
                act(lambda e: e.copy(out=hp[:], in_=xc[:]), [xcB], [hpB])
                p16, p16B = p16R.next()
                act(lambda e: e.copy(out=p16[:], in_=c["p32"][:]), [c["p32B"]], [p16B])
                junk, junkB = junkR.next()
                sd3, sd3B = rms_sd_c(xc[:], xcB, junk[:], junkB)
                c.update(hp=hp, hpB=hpB, p16=p16, p16B=p16B, sd3=sd3, sd3B=sd3B)

            def stC(t):
                c = cs[t]
                hp, hpB, p16, p16B = c["hp"], c["hpB"], c["p16"], c["p16B"]
                c["rs3"], c["rs3B"] = rms_recip_c(c["sd3"], c["sd3B"])
                tp, tpB = tpR.next()
                for kc in range(8):
                    pe(lambda e: e.transpose(out=tp[:, kc * 128:(kc + 1) * 128], in_=hp[:, kc * 128:(kc + 1) * 128], identity=ident16[:]),
                       [hpB, ident16B], [tpB], inc=(kc == 7))
                hpT, hpTB = hpTR.next()
                act(lambda e: e.copy(out=hpT[:], in_=tp[:, :].rearrange("p (c t) -> p c t", c=8)), [tpB], [hpTB])
                tp2, tp2B = tpR.next()
                for cc in range(2):
                    pe(lambda e: e.transpose(out=tp2[:, cc * 128:(cc + 1) * 128], in_=p16[:, cc * 128:(cc + 1) * 128], identity=ident16[:]),
                       [p16B, ident16B], [tp2B], inc=(cc == 1))
                pT, pTB = pTR.next()
                act(lambda e: e.copy(out=pT[:], in_=tp2[:, 0:256].rearrange("p (c t) -> p c t", c=2)), [tp2B], [pTB])
                c.update(hpT=hpT, hpTB=hpTB, pT=pT, pTB=pTB)

            def stD(t):
                c = cs[t]
                xc, xcB = c["xc"], c["xcB"]
                hpT, hpTB, pT, pTB = c["hpT"], c["hpTB"], c["pT"], c["pTB"]
                sg, sgB = sgR.next()
                for half in range(2):
                    fg, fgB = fR.next()
                    mmgroup(fg[:, :], fgB, [(hpT[:, kc, :], wpg[:, kc, half * 512:(half + 1) * 512]) for kc in range(8)], [hpTB, wpgB])
                    fp, fpB = fR.next()
                    mmgroup(fp[:, :], fpB, [(pT[:, cc, :], wpp[:, cc, half * 512:(half + 1) * 512]) for cc in range(2)], [pTB, wppB])
                    act(lambda e: e.activation(out=sg[:, half * 512:(half + 1) * 512], in_=fg[:, :], func=AF.Sigmoid, scale=c["rs3"]), [fgB, c["rs3B"]], [sgB])
                    c["fp%d" % half] = (fp, fpB)
                c.update(sg=sg, sgB=sgB)

            def stD2(t):
                c = cs[t]
                xc, xcB, sg, sgB = c["xc"], c["xcB"], c["sg"], c["sgB"]
                for half in range(2):
                    fp, fpB = c["fp%d" % half]
                    dve(lambda e: e.tensor_tensor(out=sg[:, half * 512:(half + 1) * 512], in0=fp[:, :], in1=sg[:, half * 512:(half + 1) * 512], op=ALU.mult),
                        [fpB, sgB], [sgB])
                dve(lambda e: e.tensor_tensor(out=xc[:], in0=xc[:], in1=sg[:], op=ALU.add), [xcB, sgB], [xcB])
                junk, junkB = junkR.next()
                sd4, sd4B = rms_sd_c(xc[:], xcB, junk[:], junkB)
                c.update(sg=sg, sgB=sgB, sd4=sd4, sd4B=sd4B)

            def stE(t):
                c = cs[t]
                xc, xcB, sg, sgB = c["xc"], c["xcB"], c["sg"], c["sgB"]
                c["rs4"], c["rs4B"] = rms_recip_c(c["sd4"], c["sd4B"])
                dve(lambda e: e.scalar_tensor_tensor(out=sg[:], in0=xc[:], scalar=c["rs4"], in1=gfin[:], op0=ALU.mult, op1=ALU.mult), [xcB, c["rs4B"], gfinB], [sgB])
                S.dma("sp", lambda e: e.dma_start(out=out_d[t * 128:(t + 1) * 128, :], in_=sg[:]), [sgB], (), track=out_tokens)
                del cs[t]

            for i in range(-3, NT + 1):
                for fn, tt_ in ((stL, i + 3), (stC, i + 1), (stD, i), (stB, i + 2), (stD2, i), (stE, i - 1)):
                    if 0 <= tt_ < NT:
                        fn(tt_)
            S._wait("sp", out_tokens)
            S.barrier()


def _consts():
    ident = np.eye(128, dtype=np.float32)
    ar = np.arange(128)
    triU = (ar[:, None] < ar[None, :]).astype(np.float32)
    maskI = (ar[:, None] <= ar[None, :]).astype(np.float32)
    iota = np.broadcast_to(np.arange(32, dtype=np.float32), (128, 32))
    kaug = np.zeros((128, 2, 128), np.float32)
    kaug[0, 0, :] = ar - 128.0
    kaug[0, 1, :] = ar
    kaug[1, :, :] = 1.0
    qaug = np.zeros((128, 2, 4, 128), np.float32)
    for k in range(2):
        for g in range(4):
            slope = 2.0 ** (-8.0 * (k * 4 + g + 1) / 8.0)
            qaug[0, k, g, :] = 8.0 * slope
            qaug[1, k, g, :] = -8.0 * slope * ar
    mask01 = np.zeros((128, 2, 128), np.float32)
    mask01[:, 0, :] = (ar[:, None] > ar[None, :])
    mask01[:, 1, :] = (ar[:, None] <= ar[None, :])
    return np.ascontiguousarray(np.concatenate([ident, triU, maskI, iota, kaug.reshape(128, 256), qaug.reshape(128, 1024),
                                                mask01.reshape(128, 256)], axis=1))


_NC_CACHE = {}


def _prep(x, p, g_mix, w_in, attn_sinks, g_sgu, b_sgu, w_spatial, b_spatial, w_attn_proj, w_sgu_proj,
          w_out, g_ffn, w_router, b_router, w_gate, b_gate, w_up, b_up, w_down, b_down,
          g_ple, w_ple_gate, w_ple_proj, g_final):
    f = lambda a: np.ascontiguousarray(np.asarray(a, dtype=np.float32))
    x = f(x).reshape(NCORES, T, D)
    p = f(p)[0].reshape(NCORES, T, 256)
    w_in0 = f(w_in)[0]
    qcols = np.concatenate([np.arange((k * 4 + g) * 64, (k * 4 + g + 1) * 64) for g in range(4) for k in range(2)])
    w_in_p = np.ascontiguousarray(np.concatenate([w_in0[:, qcols], w_in0[:, 512:]], axis=1))
    colT = lambda v: f(v).reshape(-1, 128).T
    cvec = np.ascontiguousarray(np.concatenate([colT(g_mix[0]), colT(g_ffn[0]), colT(g_ple[0]), colT(g_sgu[0]), colT(b_sgu[0])], axis=1))
    bgT = np.ascontiguousarray(f(b_gate)[0].reshape(NE, 8, 128).transpose(2, 0, 1).reshape(128, NE * 8))
    buT = np.ascontiguousarray(f(b_up)[0].reshape(NE, 8, 128).transpose(2, 0, 1).reshape(128, NE * 8))
    sinks = f(attn_sinks)[0].reshape(2, 4)
    sink_rows = np.repeat(sinks, 64, axis=0)
    bc = lambda v: np.broadcast_to(f(v).reshape(1, -1), (128, f(v).size))
    rowp = np.ascontiguousarray(np.concatenate([bc(b_router[0]), sink_rows, bc(b_spatial[0]), bc(g_final)], axis=1))
    shared = {
        "w_in": w_in_p, "w_attn_proj": f(w_attn_proj)[0], "w_sgu_proj": f(w_sgu_proj)[0], "w_out": f(w_out)[0],
        "w_router": f(w_router)[0], "w_gate": f(w_gate)[0], "w_up": f(w_up)[0], "w_down": f(w_down)[0],
        "b_down": f(b_down)[0], "w_ple_gate": f(w_ple_gate)[0], "w_ple_proj": f(w_ple_proj)[0],
        "w_spatial": f(w_spatial)[0], "cvec": cvec, "bgT": bgT, "buT": buT, "rowp": rowp, "cst": _consts(),
    }
    return shared, x, p


def kernel(**inputs):
    shared, x, p = _prep(**inputs)
    if "nc" not in _NC_CACHE:
        _NC_CACHE["nc"] = build_nc()
    nc = _NC_CACHE["nc"]
    in_maps = []
    for c in range(NCORES):
        m = dict(shared)
        m["x"] = x[c]
        m["p"] = p[c]
        in_maps.append(m)
    res = run_bass_kernel_spmd(nc, in_maps, core_ids=list(range(NCORES)))
    out = np.stack([np.asarray(r["out"], dtype=np.float32) for r in res.results], axis=0)
    return out.reshape(16, 2048, D)
```
